# Optimizing a Trainium2 kernel written in Bass

```python
import jax, jax.numpy as jnp
from jax import lax
import numpy as np

D_MODEL = 1024
BATCH = 8
SEQ = 8192
DEPTH = 1

D_FF = 2816
MACARON_WEIGHT = 0.5
NORM_EPS = 1e-6
GLA_HEADS = 4
GLA_DK = 64
GLA_DV = 128
GLA_GATE_RANK = 16
GLA_GATE_NORMALIZER = 16.0
GLA_CHUNK = 64
RWKV_HEADS = 8
RWKV_N = 64
RWKV_DECAY_RANK = 64
RWKV_AAA_RANK = 64
RWKV_GATE_RANK = 128
RWKV_GN_EPS = 64e-5
GLA_WIDTH = GLA_HEADS * GLA_DV
RWKV_WIDTH = RWKV_HEADS * RWKV_N
MIX_WIDTH = GLA_WIDTH + RWKV_WIDTH
GLA_COLS = (GLA_HEADS * GLA_DK, GLA_HEADS * GLA_DK, GLA_WIDTH, GLA_WIDTH, GLA_GATE_RANK)
RWKV_COLS = (RWKV_WIDTH, RWKV_WIDTH, RWKV_WIDTH, RWKV_DECAY_RANK, RWKV_AAA_RANK, RWKV_GATE_RANK)
GLA_PROJ = 2 * GLA_HEADS * GLA_DK + 2 * GLA_WIDTH + GLA_GATE_RANK
RWKV_PROJ = 3 * RWKV_WIDTH + RWKV_DECAY_RANK + RWKV_AAA_RANK + RWKV_GATE_RANK
PROJ_WIDTH = GLA_PROJ + RWKV_PROJ

kernel_name = "hybrid_gla_rwkv7_macaron_block"


def _rms_norm(x, g):
    xf = x.astype(jnp.float32)
    y = xf * lax.rsqrt(jnp.mean(xf * xf, axis=-1, keepdims=True) + NORM_EPS)
    return (y * g.astype(jnp.float32)).astype(x.dtype)


def _swiglu(h, w_gate, w_up, w_down):
    return (jax.nn.silu(h @ w_gate) * (h @ w_up)) @ w_down


def _split_cols(t, sizes):
    return jnp.split(t, np.cumsum(sizes)[:-1].tolist(), axis=-1)


def _gla_mixer(q, k, v, g_out, a_lr, alpha_w2, alpha_b, norm_w):
    f32 = jnp.float32
    bsz, seq, _ = q.shape
    n_chunks = seq // GLA_CHUNK
    log_alpha = jax.nn.log_sigmoid((a_lr @ alpha_w2 + alpha_b).astype(f32)) / GLA_GATE_NORMALIZER

    def to_chunks(t, d):
        return t.astype(f32).reshape(bsz, n_chunks, GLA_CHUNK, GLA_HEADS, d).transpose(1, 0, 3, 2, 4)

    qc = to_chunks(q, GLA_DK) * (GLA_DK ** -0.5)
    kc = to_chunks(k, GLA_DK)
    vc = to_chunks(v, GLA_DV)
    gc = to_chunks(log_alpha, GLA_DK)
    causal = jnp.tril(jnp.ones((GLA_CHUNK, GLA_CHUNK), dtype=bool))[:, :, None]

    def chunk_step(state, inp):
        qb, kb, vb, gb = inp
        cum = jnp.cumsum(gb, axis=2)
        o_inter = jnp.einsum('bhcd,bhdv->bhcv', qb * jnp.exp(cum), state)
        rel = jnp.exp(jnp.where(causal, cum[:, :, :, None, :] - cum[:, :, None, :, :], -jnp.inf))
        scores = jnp.einsum('bhid,bhjd,bhijd->bhij', qb, kb, rel)
        o_intra = jnp.einsum('bhij,bhjv->bhiv', scores, vb)
        last = cum[:, :, -1, :]
        new_state = jnp.exp(last)[..., None] * state + jnp.einsum(
            'bhcd,bhcv->bhdv', kb * jnp.exp(last[:, :, None, :] - cum), vb)
        return new_state, o_inter + o_intra

    state0 = jnp.zeros((bsz, GLA_HEADS, GLA_DK, GLA_DV), f32)
    _, oc = lax.scan(chunk_step, state0, (qc, kc, vc, gc))
    o = oc.transpose(1, 0, 3, 2, 4).reshape(bsz, seq, GLA_HEADS, GLA_DV)
    o = o * lax.rsqrt(jnp.mean(o * o, axis=-1, keepdims=True) + NORM_EPS) * norm_w
    o = o * jax.nn.silu(g_out.astype(f32).reshape(bsz, seq, GLA_HEADS, GLA_DV))
    return o.reshape(bsz, seq, GLA_WIDTH)


def _rwkv7_mixer(p, mu, w0, w2, a0, a2, g2, k_k, k_a, r_k, ln_w, ln_b):
    f32 = jnp.float32
    bsz, seq, _ = p.shape
    p = p.astype(f32)
    p_prev = jnp.pad(p[:, :-1], ((0, 0), (1, 0), (0, 0)))
    p = p + mu * (p_prev - p)
    r, k, v, w_lr, a_lr, g_lr = _split_cols(p, RWKV_COLS)
    w = -jax.nn.softplus(-(w0 + jnp.tanh(w_lr) @ w2)) - 0.5
    decay = jnp.exp(-jnp.exp(w))
    a = jax.nn.sigmoid(a0 + a_lr @ a2)
    g = jax.nn.sigmoid(g_lr) @ g2

    def heads(t):
        return t.reshape(bsz, seq, RWKV_HEADS, RWKV_N)

    kk = heads(k * k_k)
    kk = kk / jnp.maximum(jnp.linalg.norm(kk, axis=-1, keepdims=True), 1e-12)
    k = heads(k * (1.0 + (a - 1.0) * k_a))
    a_h = heads(a)
    r, v, decay = heads(r), heads(v), heads(decay)

    def tm(t):
        return t.transpose(1, 0, 2, 3)

    def step(state, inp):
        r_t, w_t, k_t, v_t, a_t, b_t = inp
        sa = jnp.einsum('bhvk,bhk->bhv', state, a_t)
        state = (state * w_t[:, :, None, :] + sa[..., None] * b_t[:, :, None, :]
                 + v_t[..., None] * k_t[:, :, None, :])
        return state, jnp.einsum('bhvk,bhk->bhv', state, r_t)

    state0 = jnp.zeros((bsz, RWKV_HEADS, RWKV_N, RWKV_N), f32)
    _, y = lax.scan(step, state0, (tm(r), tm(decay), tm(k), tm(v), tm(-kk), tm(kk * a_h)))
    y = tm(y)
    mean = jnp.mean(y, axis=-1, keepdims=True)
    var = jnp.mean(jnp.square(y - mean), axis=-1, keepdims=True)
    y = ((y - mean) * lax.rsqrt(var + RWKV_GN_EPS)).reshape(bsz, seq, RWKV_WIDTH) * ln_w + ln_b
    bonus = jnp.sum(r * k * r_k, axis=-1, keepdims=True) * v
    y = y + bonus.reshape(bsz, seq, RWKV_WIDTH)
    return y * g


def setup_inputs(seed: int = 0) -> dict:
    key = jax.random.key(seed)
    ks = jax.random.split(key, 32)
    f32 = jnp.float32
    L = DEPTH

    def nrm(k, shape, scale):
        return jax.random.normal(k, shape, f32) * scale

    def gain(k, shape):
        return 1.0 + 0.02 * jax.random.normal(k, shape, f32)

    return {
        "x": jax.random.normal(ks[0], (BATCH, SEQ, D_MODEL), f32),
        "ffn1_norm": gain(ks[1], (L, D_MODEL)),
        "ffn1_w_gate": nrm(ks[2], (L, D_MODEL, D_FF), D_MODEL ** -0.5),
        "ffn1_w_up": nrm(ks[3], (L, D_MODEL, D_FF), D_MODEL ** -0.5),
        "ffn1_w_down": nrm(ks[4], (L, D_FF, D_MODEL), D_FF ** -0.5),
        "mix_norm": gain(ks[5], (L, D_MODEL)),
        "w_in": nrm(ks[6], (L, D_MODEL, PROJ_WIDTH), D_MODEL ** -0.5),
        "gla_alpha_w2": nrm(ks[7], (L, GLA_GATE_RANK, GLA_HEADS * GLA_DK), GLA_GATE_RANK ** -0.5),
        "gla_alpha_b": nrm(ks[8], (L, GLA_HEADS * GLA_DK), 0.5),
        "gla_norm": gain(ks[9], (L, GLA_DV)),
        "rwkv_mu": jax.random.uniform(ks[10], (L, RWKV_PROJ), f32, 0.0, 1.0),
        "rwkv_w0": jax.random.uniform(ks[11], (L, RWKV_WIDTH), f32, -4.0, 0.0),
        "rwkv_w2": nrm(ks[12], (L, RWKV_DECAY_RANK, RWKV_WIDTH), 0.5 * RWKV_DECAY_RANK ** -0.5),
        "rwkv_a0": nrm(ks[13], (L, RWKV_WIDTH), 0.5),
        "rwkv_a2": nrm(ks[14], (L, RWKV_AAA_RANK, RWKV_WIDTH), RWKV_AAA_RANK ** -0.5),
        "rwkv_g2": nrm(ks[15], (L, RWKV_GATE_RANK, RWKV_WIDTH), RWKV_GATE_RANK ** -0.5),
        "rwkv_k_k": 0.85 + nrm(ks[16], (L, RWKV_WIDTH), 0.05),
        "rwkv_k_a": 1.0 + nrm(ks[17], (L, RWKV_WIDTH), 0.05),
        "rwkv_r_k": nrm(ks[18], (L, RWKV_HEADS, RWKV_N), 0.1),
        "rwkv_ln_w": gain(ks[19], (L, RWKV_WIDTH)),
        "rwkv_ln_b": nrm(ks[20], (L, RWKV_WIDTH), 0.02),
        "w_out": nrm(ks[21], (L, MIX_WIDTH, D_MODEL), MIX_WIDTH ** -0.5),
        "ffn2_norm": gain(ks[22], (L, D_MODEL)),
        "ffn2_w_gate": nrm(ks[23], (L, D_MODEL, D_FF), D_MODEL ** -0.5),
        "ffn2_w_up": nrm(ks[24], (L, D_MODEL, D_FF), D_MODEL ** -0.5),
        "ffn2_w_down": nrm(ks[25], (L, D_FF, D_MODEL), D_FF ** -0.5),
        "final_norm": gain(ks[26], (D_MODEL,)),
    }


def reference(x, ffn1_norm, ffn1_w_gate, ffn1_w_up, ffn1_w_down, mix_norm, w_in,
              gla_alpha_w2, gla_alpha_b, gla_norm, rwkv_mu, rwkv_w0, rwkv_w2, rwkv_a0,
              rwkv_a2, rwkv_g2, rwkv_k_k, rwkv_k_a, rwkv_r_k, rwkv_ln_w, rwkv_ln_b, w_out,
              ffn2_norm, ffn2_w_gate, ffn2_w_up, ffn2_w_down, final_norm):
    for l in range(DEPTH):
        x = x + MACARON_WEIGHT * _swiglu(_rms_norm(x, ffn1_norm[l]), ffn1_w_gate[l], ffn1_w_up[l], ffn1_w_down[l])
        h = _rms_norm(x, mix_norm[l])
        proj = h @ w_in[l]
        gla_p, rwkv_p = jnp.split(proj, [GLA_PROJ], axis=-1)
        q, k, v, g_out, a_lr = _split_cols(gla_p, GLA_COLS)
        o_gla = _gla_mixer(q, k, v, g_out, a_lr, gla_alpha_w2[l], gla_alpha_b[l], gla_norm[l])
        o_rwkv = _rwkv7_mixer(rwkv_p, rwkv_mu[l], rwkv_w0[l], rwkv_w2[l], rwkv_a0[l], rwkv_a2[l],
                              rwkv_g2[l], rwkv_k_k[l], rwkv_k_a[l], rwkv_r_k[l], rwkv_ln_w[l], rwkv_ln_b[l])
        mixed = jnp.concatenate([o_gla, o_rwkv], axis=-1).astype(x.dtype)
        x = x + mixed @ w_out[l]
        x = x + MACARON_WEIGHT * _swiglu(_rms_norm(x, ffn2_norm[l]), ffn2_w_gate[l], ffn2_w_up[l], ffn2_w_down[l])
    return _rms_norm(x, final_norm)
```

```python
import numpy as np
from contextlib import ExitStack
import concourse.bass as bass
import concourse.mybir as mybir
from concourse.bass_utils import run_bass_kernel_spmd

F32 = mybir.dt.float32
BF16 = mybir.dt.bfloat16
ALU = mybir.AluOpType
AF = mybir.ActivationFunctionType

D = 1024
DFF = 2816
NFC = DFF // 128
TT = 512
SEQ = 8192
NCORES = 8
EPS = 1e-6
GN_EPS = 64e-5
GLA_W = 1552
RW_W = 1792
PROJ = 3344

ENGS = ("pe", "act", "dve", "pool", "sp")
SEM_CH = 30000


class Res:
    __slots__ = ("name", "last_w", "readers", "excl")

    def __init__(self, name, excl=False):
        self.name = name
        self.last_w = None
        self.readers = []
        self.excl = excl


class Op:
    __slots__ = ("eng", "fn", "deps", "dkey", "needs_sig", "sig", "n")

    def __init__(self, eng, fn, dkey):
        self.eng = eng
        self.fn = fn
        self.deps = []
        self.dkey = dkey
        self.needs_sig = dkey is not None
        self.sig = None
        self.n = None


class Prog:
    def __init__(self):
        self.ops = {e: [] for e in ENGS}
        self.nops = 0

    def add(self, eng, fn, reads=(), writes=(), dkey=None, extra=()):
        op = Op(eng, fn, dkey)
        if any(r.excl for r in reads):
            writes = list(writes) + [r for r in reads if r.excl]
            reads = [r for r in reads if not r.excl]
        deps = {}
        for r in reads:
            w = r.last_w
            if w is not None:
                deps[id(w)] = (w, True)
        for wr in writes:
            w = wr.last_w
            if w is not None and id(w) not in deps:
                deps[id(w)] = (w, False)
            for rd in wr.readers:
                if id(rd) not in deps:
                    deps[id(rd)] = (rd, False)
        for e in extra:
            deps[id(e)] = (e, True)
        for d, raw in deps.values():
            if d is op:
                continue
            if d.eng == eng and d.dkey is None:
                if eng == "pe" or not raw:
                    continue
            op.deps.append(d)
            d.needs_sig = True
        for r in reads:
            if dkey is None:
                r.readers = [x for x in r.readers if not (x.eng == eng and x.dkey is None)]
            r.readers.append(op)
        for wr in writes:
            wr.last_w = op
            wr.readers = []
        self.ops[eng].append(op)
        self.nops += 1
        return op

    def emit(self, nc, es):
        nsig = {e: 0 for e in ENGS}
        dcount = {}
        for e in ENGS:
            for op in self.ops[e]:
                if op.dkey is not None:
                    dcount[op.dkey] = dcount.get(op.dkey, 0) + 1
                    op.sig = ("d", op.dkey, 16 * dcount[op.dkey])
                elif op.needs_sig:
                    op.sig = ("e", e, nsig[e])
                    nsig[e] += 1
        esems = {}
        for e in ENGS:
            nch = (nsig[e] + SEM_CH - 1) // SEM_CH
            esems[e] = [es.enter_context(nc.semaphore(f"s_{e}_{i}")) for i in range(nch)]
        dsems = {k: es.enter_context(nc.semaphore("d_" + "_".join(str(x) for x in k))) for k in dcount}
        self.nsig = nsig
        block = es.enter_context(nc.Block())
        prog = self

        def run(engname, eng):
            known_e = {e: -1 for e in ENGS}
            known_d = {}
            for op in prog.ops[engname]:
                for d in op.deps:
                    s = d.sig
                    if s[0] == "e":
                        if known_e[s[1]] >= s[2]:
                            continue
                        known_e[s[1]] = s[2]
                        eng.wait_ge(esems[s[1]][s[2] // SEM_CH], s[2] % SEM_CH + 1)
                    else:
                        if known_d.get(s[1], 0) >= s[2]:
                            continue
                        known_d[s[1]] = s[2]
                        eng.wait_ge(dsems[s[1]], s[2])
                ins = op.fn(eng)
                s = op.sig
                if s is not None:
                    if s[0] == "e":
                        ins.then_inc(esems[s[1]][s[2] // SEM_CH], 1)
                    else:
                        ins.then_inc(dsems[s[1]], 16)

        @block.tensor
        def _(e):
            run("pe", e)

        @block.scalar
        def _(e):
            run("act", e)

        @block.vector
        def _(e):
            run("dve", e)

        @block.gpsimd
        def _(e):
            run("pool", e)

        @block.sync
        def _(e):
            run("sp", e)


def _kblocks(w, cb):
    K, C = w.shape
    return np.ascontiguousarray(w.reshape(K // 128, 128, C // cb, cb).transpose(2, 1, 0, 3))


def _cols(v, n):
    return np.ascontiguousarray(v.reshape(n, 128).T)


def _host_consts():
    i = np.arange(128)
    same = (i[:, None] // 64) == (i[None, :] // 64)
    strictT = (same & (i[:, None] < i[None, :])).astype(np.float32)
    inclT = (same & (i[:, None] <= i[None, :])).astype(np.float32)
    strict = (same & (i[:, None] > i[None, :])).astype(np.float32)
    ident = np.eye(128, dtype=np.float32)
    blk = same.astype(np.float32)
    ones = np.ones((128, 128), np.float32)
    c = {}
    c["cm"] = np.ascontiguousarray(np.stack([strictT, inclT, strictT, inclT, strict, ident, blk, ones,
                                             inclT, inclT, inclT, inclT], axis=1))
    t = np.arange(TT)
    c["rst"] = np.ascontiguousarray(np.broadcast_to((t % 64 != 0).astype(np.float32)[None, :], (128, TT)))
    return c


PC_N1, PC_NM, PC_N2, PC_NF = 0, 8, 16, 24
PC_AB, PC_GN = 32, 34
PC_W0, PC_A0, PC_KK, PC_KA, PC_RK, PC_LW, PC_LB = 35, 39, 43, 47, 51, 55, 59
PC_NAB = 63
PC_EPS = 65
PC_TOT = 72


def build_program(T, cfg):
    NT = T // TT
    nc = bass.Bass("TRN2", target_bir_lowering=False)
    P = Prog()
    es = ExitStack()

    def din(name, shape, dt=F32):
        return nc.dram_tensor(name, list(shape), dt, kind="ExternalInput").ap()

    def dscr(name, shape, dt=BF16):
        return nc.dram_tensor(name, list(shape), dt, kind="Internal").ap()

    def sb(name, shape, dt):
        return es.enter_context(nc.sbuf_tensor("sb_" + name, list(shape), dt))

    XS = 2 if NT % 2 == 0 else 1
    TH = T // XS
    xT_ds = [din(f"xT{i}", [D, TH]) for i in range(XS)]
    out_ds = [nc.dram_tensor(f"outT{i}", [D, TH], F32, kind="ExternalOutput").ap() for i in range(XS)]
    wgu_d = [din(f"wgu{i}", [NFC, 128, 2048]) for i in (1, 2)]
    wd_d = [din(f"wd{i}", [8, 128, DFF]) for i in (1, 2)]
    wing_d = din("wing", [12, 128, 1024])
    winga_d = din("winga", [128, 128])
    winr_d = din("winr", [14, 128, 1024])
    mur_d = din("mur", [14, 128, 128])
    wout_d = din("wout", [8, 128, 1024])
    aw2_d = din("aw2", [16, 256])
    w2_d = din("w2", [64, 512])
    a2_d = din("a2", [64, 512])
    g2_d = din("g2", [128, 512])
    pc_d = din("pc", [128, PC_TOT])
    cm_d = din("cm", [128, 12 * 128])
    rst_d = din("rst", [128, TT])

    if cfg.get("dbg", False):
        dbg_d = nc.dram_tensor("dbg", [128, 8, T], BF16, kind="ExternalOutput").ap()
    sgu_s = [dscr(f"sgu{i}", [NFC, 128, 2048]) for i in (1, 2)]
    sd_s = [dscr(f"sd{i}", [8, 128, DFF]) for i in (1, 2)]
    sing_s = dscr("sing", [12, 128, 1024])
    singa_s = dscr("singa", [128, 128])
    sinr_s = dscr("sinr", [14, 128, 2048])
    sout_s = dscr("sout", [8, 128, 1024])

    xT = [sb(f"xT{i}", [128, 8, TT], F32) for i in range(2)]
    xT_r = [[Res(f"xT{i}_{c}") for c in range(8)] for i in range(2)]
    hT = sb("hT", [128, 8, TT + 2], BF16)
    hT_r = Res("hT")
    aT = sb("aT", [128, NFC, TT], BF16)
    aT_r = [Res(f"aT{j}") for j in range(NFC)]
    NRA, NRB = 4, 2
    ringA = [sb(f"ringA{i}", [128, 2048], BF16) for i in range(NRA)]
    ringA_r = [Res(f"ringA{i}") for i in range(NRA)]
    ringB = [sb(f"ringB{i}", [128, DFF], BF16) for i in range(NRB)]
    ringB_r = [Res(f"ringB{i}") for i in range(NRB)]
    pc = sb("pc", [128, PC_TOT], F32)
    pc_r = Res("pc")
    cmb = sb("cmb", [128, 12, 128], BF16)
    cm_r = Res("cm")
    rst = sb("rst", [128, TT], F32)
    NFT = 14
    ftmp = [sb(f"ftmp{i}", [128, TT], F32) for i in range(NFT)]
    ftmp_r = [Res(f"ftmp{i}") for i in range(NFT)]
    sqT = aT[:, 0:8, :]

    ps = [es.enter_context(nc.psum_tensor(f"ps{i}", [128, 512], F32)) for i in range(8)]
    ps_r = [Res(f"ps{i}", excl=True) for i in range(8)]

    ident = cmb[:, 5, :]
    blkones = cmb[:, 6, :]
    ones = cmb[:, 7, :]

    pro_stores = []

    last_by_key = {}
    scr_ops = {}

    def id_of(ap_):
        return (ap_.tensor.name, ap_.offset)

    def dram_cast(dst, src, key):
        prev = last_by_key.get(key)
        op = P.add("pool", lambda e, d=dst, s=src: e.dma_start(out=d, in_=s), dkey=key,
                   extra=(prev,) if prev is not None else ())
        last_by_key[key] = op
        pro_stores.append(op)
        scr_ops.setdefault(id_of(dst), []).append(op)

    kctr = [0]

    def nkey(base):
        kctr[0] += 1
        return (base, kctr[0] % 4)

    def cast_ffn(f):
        for j in range(NFC):
            dram_cast(sgu_s[f][j], wgu_d[f][j], nkey("pc"))
        for o in range(8):
            dram_cast(sd_s[f][o], wd_d[f][o], nkey("pc"))

    cast_ffn(0)
    dram_cast(singa_s, winga_d, nkey("pc"))
    for b in range(12):
        dram_cast(sing_s[b], wing_d[b], nkey("pc"))

    P.add("sp", lambda e: e.dma_start(out=pc[:, :], in_=pc_d), writes=[pc_r], dkey=("c", 0))
    P.add("sp", lambda e: e.dma_start(out=rst[:, :], in_=rst_d), writes=[cm_r], dkey=("c", 1))
    for h in range(3):
        P.add("sp", lambda e, h=h: e.dma_start(out=ftmp[h][:, :], in_=cm_d[:, h * 512:(h + 1) * 512]),
              writes=[ftmp_r[h]], dkey=("c", 2 + h))
        P.add("dve", lambda e, h=h: e.tensor_copy(cmb[:, 4 * h:4 * h + 4, :],
                                                  ftmp[h][:, :].rearrange("p (a b) -> p a b", a=4)),
              reads=[ftmp_r[h]], writes=[cm_r])
    P.add("dve", lambda e: e.tensor_scalar(pc[:, PC_NAB:PC_NAB + 2], pc[:, PC_AB:PC_AB + 2], -1.0, None, ALU.mult),
          reads=[pc_r], writes=[pc_r])

    ra = [0]
    rb = [0]
    first_stream = [True]

    def stream_A(src, ncols=2048):
        k = ra[0] % NRA
        ra[0] += 1
        extra = scr_ops.get(id_of(src), pro_stores)
        P.add("sp", lambda e, k=k, s=src, n=ncols: e.dma_start(out=ringA[k][:, 0:n], in_=s),
              writes=[ringA_r[k]], dkey=("ra", k), extra=extra)
        return ringA[k], ringA_r[k]

    def stream_B(src):
        k = rb[0] % NRB
        rb[0] += 1
        P.add("sp", lambda e, k=k, s=src: e.dma_start(out=ringB[k][:, :], in_=s),
              writes=[ringB_r[k]], dkey=("rb", k), extra=scr_ops.get(id_of(src), pro_stores))
        return ringB[k], ringB_r[k]

    def rmsnorm_to_h(xb, xr, gcol, hoff):
        P.add("act", lambda e: e.activation(sqT, xb[:, :, :], AF.Square), reads=xr, writes=aT_r[0:8])
        for c in range(8):
            P.add("pe", lambda e, c=c: e.matmul(ps[6][:, :], ones, sqT[:, c, :], start=(c == 0), stop=(c == 7)),
                  reads=aT_r[0:8] + [cm_r], writes=[ps_r[6]])
        P.add("act", lambda e: e.activation(ftmp[2][:, :], ps[6][:, :], AF.Ln, bias=pc[:, PC_EPS:PC_EPS + 1],
                                            scale=1.0 / D), reads=[ps_r[6], pc_r], writes=[ftmp_r[2]])
        P.add("act", lambda e: e.activation(ftmp[3][:, :], ftmp[2][:, :], AF.Exp, scale=-0.5),
              reads=[ftmp_r[2]], writes=[ftmp_r[3]])
        for c in range(8):
            P.add("dve", lambda e, c=c: e.scalar_tensor_tensor(
                hT[:, c, hoff:hoff + TT], xb[:, c, :], pc[:, gcol + c:gcol + c + 1], ftmp[3][:, :],
                ALU.mult, ALU.mult), reads=[xr[c], ftmp_r[3], pc_r], writes=[hT_r])

    def ffn(f, xb, xr, hoff):
        for j in range(NFC):
            slot, slot_r = stream_A(sgu_s[f][j])
            sv = slot[:, :].rearrange("p (g k c) -> p g k c", g=2, k=8)
            pg, pu = 0 + (j % 2), 2 + (j % 2)
            for kc in range(8):
                P.add("pe", lambda e, kc=kc, sv=sv, pg=pg: e.matmul(
                    ps[pg][:, :], sv[:, 0, kc, :], hT[:, kc, hoff:hoff + TT], start=(kc == 0), stop=(kc == 7)),
                    reads=[slot_r, hT_r], writes=[ps_r[pg]])
            for kc in range(8):
                P.add("pe", lambda e, kc=kc, sv=sv, pu=pu: e.matmul(
                    ps[pu][:, :], sv[:, 1, kc, :], hT[:, kc, hoff:hoff + TT], start=(kc == 0), stop=(kc == 7)),
                    reads=[slot_r, hT_r], writes=[ps_r[pu]])
            ft = j % 2
            P.add("act", lambda e, ft=ft, pg=pg: e.activation(ftmp[ft][:, :], ps[pg][:, :], AF.Silu),
                  reads=[ps_r[pg]], writes=[ftmp_r[ft]])
            P.add("dve", lambda e, ft=ft, pu=pu, j=j: e.tensor_tensor(aT[:, j, :], ftmp[ft][:, :], ps[pu][:, :], ALU.mult),
                  reads=[ftmp_r[ft], ps_r[pu]], writes=[aT_r[j]])
        for o in range(8):
            slot, slot_r = stream_B(sd_s[f][o])
            sv = slot[:, :].rearrange("p (j c) -> p j c", j=NFC)
            pd = 4 + (o % 2)
            for j in range(NFC):
                P.add("pe", lambda e, j=j, sv=sv, pd=pd: e.matmul(
                    ps[pd][:, :], sv[:, j, :], aT[:, j, :], start=(j == 0), stop=(j == NFC - 1)),
                    reads=[slot_r, aT_r[j]], writes=[ps_r[pd]])
            P.add("dve", lambda e, o=o, pd=pd: e.scalar_tensor_tensor(
                xb[:, o, :], ps[pd][:, :], 0.5, xb[:, o, :], ALU.mult, ALU.add),
                reads=[ps_r[pd], xr[o]], writes=[xr[o]])

    def final_norm(xb, xr):
        P.add("act", lambda e: e.activation(sqT, xb[:, :, :], AF.Square), reads=xr, writes=aT_r[0:8])
        for c in range(8):
            P.add("pe", lambda e, c=c: e.matmul(ps[6][:, :], ones, sqT[:, c, :], start=(c == 0), stop=(c == 7)),
                  reads=aT_r[0:8] + [cm_r], writes=[ps_r[6]])
        P.add("act", lambda e: e.activation(ftmp[2][:, :], ps[6][:, :], AF.Ln, bias=pc[:, PC_EPS:PC_EPS + 1],
                                            scale=1.0 / D), reads=[ps_r[6], pc_r], writes=[ftmp_r[2]])
        P.add("act", lambda e: e.activation(ftmp[3][:, :], ftmp[2][:, :], AF.Exp, scale=-0.5),
              reads=[ftmp_r[2]], writes=[ftmp_r[3]])
        for c in range(8):
            P.add("dve", lambda e, c=c: e.scalar_tensor_tensor(
                xb[:, c, :], xb[:, c, :], pc[:, PC_NF + c:PC_NF + c + 1], ftmp[3][:, :],
                ALU.mult, ALU.mult), reads=[xr[c], ftmp_r[3], pc_r], writes=[xr[c]])

    CDEC = 0.6065306597126334

    def RL(name, n):
        return [Res(f"{name}{i}") for i in range(n)]

    hprev = sb("hprev", [128, 8, 1], BF16); hprev_r = Res("hprev")
    aw2b = sb("aw2b", [16, 256], BF16)
    w2a2b = sb("w2a2b", [128, 512], BF16)
    g2b = sb("g2b", [128, 512], BF16)
    smallw_r = Res("smallw")
    qkg = sb("qkg", [128, 4, TT], BF16); qkg_r = RL("qkg", 4)
    vTg = sb("vTg", [128, 4, TT], BF16); vTg_r = RL("vTg", 4)
    gateg = sb("gateg", [128, 4, TT], BF16); gateg_r = RL("gateg", 4)
    gamCg = sb("gamCg", [128, 2, 8], F32); gamCg_r = Res("gamCg")
    alr = sb("alr", [16, TT], BF16); alr_r = Res("alr")
    AR = sb("AR", [128, 4, 2, TT], BF16); AR_r = RL("AR", 4)
    Bt = sb("Bt", [128, 4, TT], BF16); Bt_r = RL("Bt", 4)
    Kt = sb("Kt", [128, 4, TT], BF16); Kt_r = RL("Kt", 4)
    vTr = sb("vTr", [128, 4, TT], BF16); vTr_r = RL("vTr", 4)
    bonus = sb("bonus", [128, 4, TT], BF16); bonus_r = RL("bonus", 4)
    gater = sb("gater", [128, 4, TT], BF16); gater_r = RL("gater", 4)
    gamCr = sb("gamCr", [128, 4, 8], F32); gamCr_r = Res("gamCr")
    wa = sb("wa", [128, TT], BF16); wa_r = Res("wa")
    sgl = sb("sgl", [128, TT], BF16); sgl_r = Res("sgl")
    sq1 = sb("sq1", [128, TT], BF16); sq1_r = Res("sq1")
    Hs = sb("Hs", [128, 4, 64], F32); Hs_r = Res("Hs")
    Hbd = sb("Hbd", [128, 4, 128], BF16); Hbd_r = Res("Hbd")
    Sg = sb("Sg", [128, 2, 128], F32); Sg_r = Res("Sg")
    Sgbd = sb("Sgbd", [128, 4, 128], BF16); Sgbd_r = Res("Sgbd")
    Wb = sb("Wb", [128, 512], BF16); Wb_r = Res("Wb")
    Ub = sb("Ub", [128, 512], BF16); Ub_r = Res("Ub")
    tokBK = sb("tokBK", [128, 1024], BF16); tokBK_r = Res("tokBK")
    tokV = sb("tokV", [128, 1024], BF16); tokV_r = Res("tokV")
    tokKg = sb("tokKg", [128, 256], BF16); tokKg_r = Res("tokKg")
    Sball = sb("Sball", [128, 8, 4, 128], BF16); Sball_r = RL("Sball", 8)
    TTall = sb("TTall", [128, 8, 128], BF16); TTall_r = [Res("TTall")]
    Pb = [[sb(f"Pb{a}{b}", [128, 512], BF16) for b in range(2)] for a in range(4)]
    Pb_r = [[Res(f"Pb{a}{b}") for b in range(2)] for a in range(4)]
    STb = sb("STb", [128, 512], BF16); STb_r = Res("STb")
    tmpH = ftmp[0][:, 0:256]; tmpH_r = ftmp_r[0]
    tmpS = ftmp[1][:, 0:256]; tmpS_r = ftmp_r[1]
    mixedT = sb("mixedT", [128, 8, TT], BF16); mixedT_r = RL("mixedT", 8)
    Yraw = aT[:, 0:8, :].bitcast(F32)
    Oraw = aT[:, 8:16, :].bitcast(F32)
    Yraw_r = aT_r[0:8]
    Oraw_r = aT_r[8:16]

    def mm(out, lhsT, rhs, start, stop, reads, writes):
        return P.add("pe", lambda e: e.matmul(out, lhsT, rhs, start=start, stop=stop), reads, writes)

    def tr(out, in_, reads, writes):
        return P.add("pe", lambda e: e.transpose(out, in_, ident), list(reads) + [cm_r], writes)

    def act(out, in_, func, reads, writes, bias=None, scale=None):
        kw = {}
        if bias is not None:
            kw["bias"] = bias
        if scale is not None:
            kw["scale"] = scale
        return P.add("act", lambda e: e.activation(out, in_, func, **kw), reads, writes)

    def tt(out, in0, in1, op, reads, writes, eng="dve"):
        return P.add(eng, lambda e: e.tensor_tensor(out, in0, in1, op), reads, writes)

    def ts(out, in0, s1, s2, op0, op1, reads, writes, eng="dve"):
        if s2 is None:
            return P.add(eng, lambda e: e.tensor_scalar(out, in0, s1, None, op0), reads, writes)
        return P.add(eng, lambda e: e.tensor_scalar(out, in0, s1, s2, op0, op1), reads, writes)

    def stt(out, in0, sc, in1, op0, op1, reads, writes):
        return P.add("dve", lambda e: e.scalar_tensor_tensor(out, in0, sc, in1, op0, op1), reads, writes)

    def cp(out, in_, reads, writes, eng="act"):
        if eng == "act":
            return P.add("act", lambda e: e.activation(out, in_, AF.Copy), reads, writes)
        return P.add(eng, lambda e: e.tensor_copy(out, in_), reads, writes)

    def scan(out, d0, d1, reads, writes):
        return P.add("dve", lambda e: e.tensor_tensor_scan(out, d0, d1, 0.0, ALU.mult, ALU.add), reads, writes)

    def pcol(c):
        return pc[:, c:c + 1]

    def load_small(dst, src_d, rows, cols, ft, prow=0):
        P.add("sp", lambda e: e.dma_start(out=ftmp[ft][prow:prow + rows, 0:cols], in_=src_d),
              writes=[ftmp_r[ft]], dkey=("c", 5 + ft))
        cp(dst, ftmp[ft][prow:prow + rows, 0:cols], [ftmp_r[ft]], [smallw_r], eng="dve")

    load_small(aw2b[0:16, :], aw2_d, 16, 256, 3)
    load_small(w2a2b[0:64, :], w2_d, 64, 512, 4)
    load_small(w2a2b[64:128, :], a2_d, 64, 512, 5, prow=64)
    load_small(g2b[:, :], g2_d, 128, 512, 6)
    for tl, tr_ in ((Hs, Hs_r), (Hbd, Hbd_r), (Sg, Sg_r), (Sgbd, Sgbd_r), (Wb, Wb_r), (Ub, Ub_r), (hprev, hprev_r)):
        ap_ = tl[:, :, :] if len(tl.shape) == 3 else tl[:, :]
        P.add("pool", lambda e, a=ap_: e.memset(a, 0.0), writes=[tr_])
    stg_r = [Res("stgA"), Res("stgB")]
    for blk in range(14):
        s2 = blk % 2
        fW, fM, fO = ftmp[7 + 3 * s2], ftmp[8 + 3 * s2], ftmp[9 + 3 * s2]
        rW, rM, rO = ftmp_r[7 + 3 * s2], ftmp_r[8 + 3 * s2], ftmp_r[9 + 3 * s2]
        for half in range(2):
            P.add("sp", lambda e, blk=blk, half=half, fW=fW: e.dma_start(
                out=fW[:, :], in_=winr_d[blk][:, half * 512:(half + 1) * 512]), writes=[rW], dkey=("pw", s2))
            if half == 0:
                P.add("sp", lambda e, blk=blk, fM=fM: e.dma_start(out=fM[:, 0:128], in_=mur_d[blk]),
                      writes=[rM], dkey=("pm", s2))
                ts(fM[:, 128:256], fM[:, 0:128], -1.0, 1.0, ALU.mult, ALU.add, [rM], [rM])
            fWv = fW[:, :].rearrange("p (k c) -> p k c", k=4)
            fOv = fO[:, :].bitcast(BF16).rearrange("p (k c) -> p k c", k=8)
            tt(fOv[:, 0:4, :], fWv, fM[:, 128:256].unsqueeze(1).to_broadcast([128, 4, 128]), ALU.mult, [rW, rM], [rO])
            tt(fOv[:, 4:8, :], fWv, fM[:, 0:128].unsqueeze(1).to_broadcast([128, 4, 128]), ALU.mult, [rW, rM], [rO])
            dv = sinr_s[blk].rearrange("p (k c) -> p k c", k=16)
            op1 = P.add("pool", lambda e, dv=dv, fOv=fOv, half=half: e.dma_start(
                out=dv[:, half * 4:half * 4 + 4, :], in_=fOv[:, 0:4, :]), reads=[rO], dkey=("ps1", s2))
            op2 = P.add("pool", lambda e, dv=dv, fOv=fOv, half=half: e.dma_start(
                out=dv[:, 8 + half * 4:8 + half * 4 + 4, :], in_=fOv[:, 4:8, :]), reads=[rO], dkey=("ps2", s2))
            pro_stores.append(op1)
            pro_stores.append(op2)
            scr_ops.setdefault(id_of(sinr_s[blk]), []).extend([op1, op2])
    for o in range(8):
        dram_cast(sout_s[o], wout_d[o], nkey("pc"))
    cast_ffn(1)

    bank_rr = [0]

    def nextbank():
        b_ = bank_rr[0] % 8
        bank_rr[0] += 1
        return b_

    def proj_block(src, ncols, K16, M=128):
        slot, slot_r = stream_A(src, ncols)
        nk = 16 if K16 else 8
        sv = slot[:, 0:ncols].rearrange("p (k c) -> p k c", k=nk)
        bk = nextbank()
        for kc in range(nk):
            rhs = hT[:, kc, 2:TT + 2] if kc < 8 else hT[:, kc - 8, 1:TT + 1]
            mm(ps[bk][0:M, :], sv[:, kc, :], rhs, kc == 0, kc == nk - 1, [slot_r, hT_r], [ps_r[bk]])
        return bk

    def mixer(n, xb, xr):
        rmsnorm_to_h(xb, xr, PC_NM, 2)
        cp(hT[:, :, 1:2], hprev[:, :, :], [hprev_r], [hT_r], eng="pool")
        cp(hprev[:, :, :], hT[:, :, TT + 1:TT + 2], [hT_r], [hprev_r], eng="pool")

        stage = float(cfg.get("stage", 99))
        if stage < 1:
            return
        bk = proj_block(singa_s, 128, False, M=16)
        cp(alr[0:16, :], ps[bk][0:16, :], [ps_r[bk]], [alr_r])
        if stage < 0.5:
            return
        for c in range(2):
            bk = nextbank()
            mm(ps[bk][:, :], aw2b[0:16, c * 128:(c + 1) * 128], alr[0:16, :], True, True, [smallw_r, alr_r], [ps_r[bk]])
            act(ftmp[8][:, :], ps[bk][:, :], AF.Exp, [ps_r[bk], pc_r], [ftmp_r[8]], bias=pcol(PC_NAB + c), scale=-1.0)
            act(ftmp[9][:, :], ftmp[8][:, :], AF.Ln, [ftmp_r[8], pc_r], [ftmp_r[9]], bias=pcol(PC_EPS + 3))
            scan(ftmp[10][:, :], rst[:, :], ftmp[9][:, :], [ftmp_r[9], cm_r], [ftmp_r[10]])
            act(ftmp[4 + c][:, :], ftmp[10][:, :], AF.Exp, [ftmp_r[10]], [ftmp_r[4 + c]], scale=-1.0 / 16.0)
            act(ftmp[6 + c][:, :], ftmp[10][:, :], AF.Exp, [ftmp_r[10]], [ftmp_r[6 + c]], scale=1.0 / 16.0)
            cp(gamCg[:, c, :], ftmp[4 + c][:, :].rearrange("p (a b) -> p a b", b=64)[:, :, 63],
               [ftmp_r[4 + c]], [gamCg_r], eng="dve")
        if stage < 0.7:
            return
        for c in range(2):
            bk = proj_block(sing_s[c], 1024, False)
            stt(qkg[:, c, :], ps[bk][:, :], 0.125, ftmp[4 + c][:, :], ALU.mult, ALU.mult,
                [ps_r[bk], ftmp_r[4 + c]], [qkg_r[c]])
        for c in range(2):
            bk = proj_block(sing_s[2 + c], 1024, False)
            tt(qkg[:, 2 + c, :], ps[bk][:, :], ftmp[6 + c][:, :], ALU.mult, [ps_r[bk], ftmp_r[6 + c]], [qkg_r[2 + c]])
        for h in range(4):
            bk = proj_block(sing_s[4 + h], 1024, False)
            cp(vTg[:, h, :], ps[bk][:, :], [ps_r[bk]], [vTg_r[h]])
        for h in range(4):
            bk = proj_block(sing_s[8 + h], 1024, False)
            act(gateg[:, h, :], ps[bk][:, :], AF.Silu, [ps_r[bk]], [gateg_r[h]])

        if stage < 2:
            return
        bk = proj_block(sinr_s[12], 2048, True)
        act(wa[0:64, :], ps[bk][0:64, :], AF.Tanh, [ps_r[bk]], [wa_r])
        cp(wa[64:128, :], ps[bk][64:128, :], [ps_r[bk]], [wa_r])
        bk = proj_block(sinr_s[13], 2048, True)
        act(sgl[:, :], ps[bk][:, :], AF.Sigmoid, [ps_r[bk]], [sgl_r])
        for p in range(4):
            bk = nextbank()
            mm(ps[bk][:, :], g2b[:, p * 128:(p + 1) * 128], sgl[:, :], True, True, [smallw_r, sgl_r], [ps_r[bk]])
            cp(gater[:, p, :], ps[bk][:, :], [ps_r[bk]], [gater_r[p]])
        F = ftmp
        FR = ftmp_r
        if stage < 1.5:
            return
        for p in range(4):
            bk = nextbank()
            mm(ps[bk][:, :], w2a2b[0:64, p * 128:(p + 1) * 128], wa[0:64, :], True, True, [smallw_r, wa_r], [ps_r[bk]])
            act(F[8][:, :], ps[bk][:, :], AF.Sigmoid, [ps_r[bk], pc_r], [FR[8]], bias=pcol(PC_W0 + p))
            if stage < 1.6:
                continue
            bk = nextbank()
            mm(ps[bk][:, :], w2a2b[64:128, p * 128:(p + 1) * 128], wa[64:128, :], True, True, [smallw_r, wa_r], [ps_r[bk]])
            act(F[9][:, :], ps[bk][:, :], AF.Sigmoid, [ps_r[bk], pc_r], [FR[9]], bias=pcol(PC_A0 + p))
            if stage < 1.7:
                continue
            scan(F[10][:, :], rst[:, :], F[8][:, :], [FR[8], cm_r], [FR[10]])
            act(F[0][:, :], F[10][:, :], AF.Exp, [FR[10]], [FR[0]], scale=-CDEC)
            act(F[1][:, :], F[10][:, :], AF.Exp, [FR[10]], [FR[1]], scale=CDEC)
            tt(F[11][:, :], F[10][:, :], F[8][:, :], ALU.subtract, [FR[10], FR[8]], [FR[11]])
            act(F[2][:, :], F[11][:, :], AF.Exp, [FR[11]], [FR[2]], scale=-CDEC)
            cp(gamCr[:, p, :], F[0][:, :].rearrange("p (a b) -> p a b", b=64)[:, :, 63], [FR[0]], [gamCr_r], eng="dve")
            if stage < 1.8:
                continue
            bkK = proj_block(sinr_s[4 + p], 2048, True)
            ts(F[11][:, :], ps[bkK][:, :], pcol(PC_KK + p), None, ALU.mult, None, [ps_r[bkK], pc_r], [FR[11]])
            act(sq1[:, :], F[11][:, :], AF.Square, [FR[11]], [sq1_r])
            bk = nextbank()
            mm(ps[bk][:, :], blkones, sq1[:, :], True, True, [cm_r, sq1_r], [ps_r[bk]])
            act(F[3][:, :], ps[bk][:, :], AF.Ln, [ps_r[bk], pc_r], [FR[3]], bias=pcol(PC_EPS + 2))
            act(F[12][:, :], F[3][:, :], AF.Exp, [FR[3]], [FR[12]], scale=-0.5)
            tt(F[11][:, :], F[11][:, :], F[12][:, :], ALU.mult, [FR[11], FR[12]], [FR[11]])
            if stage < 1.9:
                continue
            tt(F[3][:, :], F[11][:, :], F[9][:, :], ALU.mult, [FR[11], FR[9]], [FR[3]])
            tt(Bt[:, p, :], F[3][:, :], F[1][:, :], ALU.mult, [FR[3], FR[1]], [Bt_r[p]])
            stt(AR[:, p, 0, :], F[11][:, :], -1.0, F[2][:, :], ALU.mult, ALU.mult, [FR[11], FR[2]], [AR_r[p]])
            ts(F[3][:, :], F[9][:, :], -1.0, pcol(PC_KA + p), ALU.add, ALU.mult, [FR[9], pc_r], [FR[3]])
            stt(F[13][:, :], F[3][:, :], 1.0, ps[bkK][:, :], ALU.add, ALU.mult, [FR[3], ps_r[bkK]], [FR[13]])
            tt(Kt[:, p, :], F[13][:, :], F[1][:, :], ALU.mult, [FR[13], FR[1]], [Kt_r[p]])
            if stage < 1.95:
                continue
            bkR = proj_block(sinr_s[p], 2048, True)
            tt(AR[:, p, 1, :], ps[bkR][:, :], F[0][:, :], ALU.mult, [ps_r[bkR], FR[0]], [AR_r[p]])
            if stage < 1.98:
                continue
            stt(sq1[:, :], ps[bkR][:, :], pcol(PC_RK + p), F[13][:, :], ALU.mult, ALU.mult,
                [ps_r[bkR], pc_r, FR[13]], [sq1_r])
            if stage < 1.985:
                continue
            bk = nextbank()
            mm(ps[bk][:, :], blkones, sq1[:, :], True, True, [cm_r, sq1_r], [ps_r[bk]])
            cp(F[12][:, :], ps[bk][:, :], [ps_r[bk]], [FR[12]])
            if stage < 1.99:
                continue
            bkV = proj_block(sinr_s[8 + p], 2048, True)
            cp(vTr[:, p, :], ps[bkV][:, :], [ps_r[bkV]], [vTr_r[p]])
            if stage < 1.995:
                continue
            tt(bonus[:, p, :], ps[bkV][:, :], F[12][:, :], ALU.mult, [ps_r[bkV], FR[12]], [bonus_r[p]])

        if stage < 3:
            return
        ps0b = ps[0][:, :].bitcast(BF16)
        ps1b = ps[1][:, :].bitcast(BF16)
        Yv = Yraw.rearrange("p a (b t) -> p (a b) t", b=2) if False else None
        for s in range(TT // 128):
            tk = slice(s * 128, (s + 1) * 128)
            for p in range(4):
                tr(ps0b[:, p * 128:(p + 1) * 128], Bt[:, p, tk], [Bt_r[p]], [ps_r[0]])
            for p in range(4):
                tr(ps0b[:, 512 + p * 128:512 + (p + 1) * 128], Kt[:, p, tk], [Kt_r[p]], [ps_r[0]])
            cp(tokBK[:, :], ps0b, [ps_r[0]], [tokBK_r], eng="dve")
            for p in range(4):
                tr(ps1b[:, p * 128:(p + 1) * 128], vTr[:, p, tk], [vTr_r[p]], [ps_r[1]])
            for h in range(4):
                tr(ps1b[:, 512 + h * 128:512 + (h + 1) * 128], vTg[:, h, tk], [vTg_r[h]], [ps_r[1]])
            cp(tokV[:, :], ps1b, [ps_r[1]], [tokV_r])
            for c in range(2):
                tr(ps0b[:, c * 128:(c + 1) * 128], qkg[:, 2 + c, tk], [qkg_r[2 + c]], [ps_r[0]])
            cp(tokKg[:, :], ps0b[:, 0:256], [ps_r[0]], [tokKg_r], eng="dve")

            TTv = TTall[:, :, :].rearrange("q (p s) c -> q s p c", s=2)
            order = [(p_, 0) for p_ in range(4)] + [(p_, 1) for p_ in range(4)]
            for idx, (p, s2) in enumerate(order):
                h = 2 * p + s2
                hp = slice(s2 * 64, (s2 + 1) * 64)
                SB = (0, 1, 6, 7)[idx % 4]
                bi = s2 * 2 + p // 2
                PB = 2 + bi
                c0 = (p % 2) * 256
                arv = AR[hp, p, :, tk]
                mm(ps[SB][:, 0:256].rearrange("p (a b) -> p a b", a=2), Bt[hp, p, tk], arv, True, True,
                   [Bt_r[p], AR_r[p]], [ps_r[SB]])
                mm(ps[SB][:, 256:512].rearrange("p (a b) -> p a b", a=2), Kt[hp, p, tk], arv, True, True,
                   [Kt_r[p], AR_r[p]], [ps_r[SB]])
                mm(ps[PB][:, c0:c0 + 128], AR[hp, p, 0, tk], Bt[hp, p, tk], True, True, [Bt_r[p], AR_r[p]], [ps_r[PB]])
                tt(Sball[:, h, :, :], ps[SB][:, :].rearrange("p (a b) -> p a b", a=4), cmb[:, 0:4, :], ALU.mult,
                   [ps_r[SB], cm_r], [Sball_r[h]])
                tt(Pb[bi][0][:, c0:c0 + 128], ps[PB][:, c0:c0 + 128], cmb[:, 4, :], ALU.mult, [ps_r[PB], cm_r], [Pb_r[bi][0]])
                cp(Pb[bi][0][:, c0 + 128:c0 + 256], Sball[:, h, 0, :], [Sball_r[h]], [Pb_r[bi][0]], eng="pool")
                tt(TTall[:, h, :], Sball[:, h, 0, :], ident, ALU.add, [Sball_r[h], cm_r], TTall_r, eng="pool")
            for lv in range(5):
                for bi in range(4):
                    cur, cur_r = Pb[bi][lv % 2], Pb_r[bi][lv % 2]
                    for c0 in (0, 256):
                        mm(ps[2 + bi][:, c0:c0 + 128], cur[:, c0 + 128:c0 + 256], cur[:, c0:c0 + 128], True, True,
                           [cur_r], [ps_r[2 + bi]])
                        if lv < 4:
                            mm(ps[2 + bi][:, c0 + 128:c0 + 256], cur[:, c0:c0 + 128], cur[:, c0 + 128:c0 + 256], True, True,
                               [cur_r], [ps_r[2 + bi]])
                for bi in range(4):
                    nxt, nxt_r = Pb[bi][(lv + 1) % 2], Pb_r[bi][(lv + 1) % 2]
                    ev_eng = "dve" if bi == 3 else "act"
                    if lv < 4:
                        cp(nxt[:, :], ps[2 + bi][:, :], [ps_r[2 + bi]], [nxt_r], eng=ev_eng)
                    else:
                        cp(nxt[:, :].rearrange("q (a b) -> q a b", a=2)[:, :, 0:128],
                           ps[2 + bi][:, :].rearrange("q (a b) -> q a b", a=2)[:, :, 0:128], [ps_r[2 + bi]], [nxt_r], eng=ev_eng)
                for s2 in range(2):
                    for p in range(4):
                        h = 2 * p + s2
                        bi = s2 * 2 + p // 2
                        c0 = (p % 2) * 256
                        nxt, nxt_r = Pb[bi][(lv + 1) % 2], Pb_r[bi][(lv + 1) % 2]
                        mm(ps[6 + s2][:, p * 128:(p + 1) * 128], nxt[:, c0:c0 + 128], TTall[:, h, :], True, True,
                           [nxt_r] + TTall_r, [ps_r[6 + s2]])
                for s2 in range(2):
                    tt(TTv[:, s2, :, :], TTv[:, s2, :, :], ps[6 + s2][:, :].rearrange("q (p c) -> q p c", p=4), ALU.add,
                       TTall_r + [ps_r[6 + s2]], TTall_r)

            for h in range(4):
                p, s2 = h // 2, h % 2
                hp = slice(s2 * 64, (s2 + 1) * 64)
                mm(ps[4 + s2][:, p * 128:(p + 1) * 128], qkg[hp, 2 + p, tk], qkg[hp, p, tk], True, True,
                   [qkg_r[p], qkg_r[2 + p]], [ps_r[4 + s2]])
            STv = STb[:, :].rearrange("p (a b c) -> p a b c", a=2, b=2)
            for s2 in range(2):
                tt(STv[:, :, s2, :], ps[4 + s2][:, 0:256].rearrange("p (a c) -> p a c", a=2), cmb[:, 8:10, :], ALU.mult,
                   [ps_r[4 + s2], cm_r], [STb_r])

            for c in range(2):
                cs = slice(c * 64, (c + 1) * 64)
                tkc = slice(s * 128 + c * 64, s * 128 + (c + 1) * 64)
                ci = s * 2 + c
                for h in range(8):
                    p, s2 = h // 2, h % 2
                    o = ps[6][cs, h * 64:(h + 1) * 64]
                    mm(o, AR[:, p, 0, tkc], Hbd[:, p, s2 * 64:(s2 + 1) * 64], True, False, [AR_r[p], Hbd_r], [ps_r[6]])
                    mm(o, Sball[:, h, 2, cs], tokV[:, p * 128 + s2 * 64:p * 128 + (s2 + 1) * 64], False, True,
                       [Sball_r[h], tokV_r], [ps_r[6]])
                for h in range(4):
                    p = h // 2
                    o = ps[5][:, h * 128 + c * 64:h * 128 + (c + 1) * 64]
                    mm(o, tokV[:, 512 + h * 128:512 + (h + 1) * 128], STb[:, h * 128 + c * 64:h * 128 + (c + 1) * 64],
                       True, False, [tokV_r, STb_r], [ps_r[5]])
                    mm(o, Sgbd[:, h, :], qkg[:, p, tkc], False, True, [Sgbd_r, qkg_r[p]], [ps_r[5]])
                cp(Wb[cs, :], ps[6][cs, :], [ps_r[6]], [Wb_r])
                for h in range(4):
                    p, s2 = h // 2, h % 2
                    mm(ps[4][s2 * 64:(s2 + 1) * 64, p * 128:(p + 1) * 128], tokKg[cs, h * 64:(h + 1) * 64],
                       tokV[cs, 512 + h * 128:512 + (h + 1) * 128], True, True, [tokKg_r, tokV_r], [ps_r[4]])
                for h in range(8):
                    mm(ps[7][cs, h * 64:(h + 1) * 64], TTall[:, h, cs], Wb[:, h * 64:(h + 1) * 64], True, True,
                       [TTall_r[0], Wb_r], [ps_r[7]])
                tt(tmpS[:, :], ps[4][:, 0:256], Sg[:, :, :].rearrange("p a b -> p (a b)"), ALU.add, [ps_r[4], Sg_r], [tmpS_r])
                cp(Ub[cs, :], ps[7][cs, :], [ps_r[7]], [Ub_r], eng="dve")
                tt(Sg[:, :, :], tmpS[:, :].rearrange("p (a b) -> p a b", a=2),
                   gamCg[:, :, ci:ci + 1].to_broadcast([128, 2, 128]), ALU.mult, [tmpS_r, gamCg_r], [Sg_r])
                Sgv = Sgbd[:, :, :].rearrange("p (a b) v -> p a b v", b=2)
                cp(Sgv[0:64, :, 0, :], Sg[0:64, :, :], [Sg_r], [Sgbd_r], eng="pool")
                cp(Sgv[64:128, :, 1, :], Sg[64:128, :, :], [Sg_r], [Sgbd_r], eng="pool")
                for h in range(8):
                    p, s2 = h // 2, h % 2
                    o = ps[2][s2 * 64:(s2 + 1) * 64, c * 256 + p * 64:c * 256 + (p + 1) * 64]
                    mm(o, Hbd[:, p, s2 * 64:(s2 + 1) * 64], AR[:, p, 1, tkc], True, False, [Hbd_r, AR_r[p]], [ps_r[2]])
                    mm(o, Ub[:, h * 64:(h + 1) * 64], Sball[:, h, 1, cs], False, False, [Ub_r, Sball_r[h]], [ps_r[2]])
                    mm(o, tokV[:, p * 128 + s2 * 64:p * 128 + (s2 + 1) * 64], Sball[:, h, 3, cs], False, True,
                       [tokV_r, Sball_r[h]], [ps_r[2]])
                for h in range(8):
                    p, s2 = h // 2, h % 2
                    o = ps[3][s2 * 64:(s2 + 1) * 64, p * 64:(p + 1) * 64]
                    cb = p * 128 + s2 * 64
                    mm(o, tokBK[cs, cb:cb + 64], Ub[cs, h * 64:(h + 1) * 64], True, False, [tokBK_r, Ub_r], [ps_r[3]])
                    mm(o, tokBK[cs, 512 + cb:512 + cb + 64], tokV[cs, cb:cb + 64], False, True, [tokBK_r, tokV_r], [ps_r[3]])
                Hs2 = Hs[:, :, :].rearrange("p a b -> p (a b)")
                tt(tmpH[:, :], ps[3][:, 0:256], Hs2, ALU.add, [ps_r[3], Hs_r], [tmpH_r])
                tt(Hs[:, :, :], tmpH[:, :].rearrange("p (a b) -> p a b", a=4),
                   gamCr[:, :, ci:ci + 1].to_broadcast([128, 4, 64]), ALU.mult, [tmpH_r, gamCr_r], [Hs_r])
                cp(Hbd[0:64, :, 0:64], Hs[0:64, :, :], [Hs_r], [Hbd_r], eng="pool")
                cp(Hbd[64:128, :, 64:128], Hs[64:128, :, :], [Hs_r], [Hbd_r])
                cp(aT[:, 0:8, :].bitcast(F32).rearrange("p a t -> p (a t)").rearrange("p (a t) -> p a t", a=4)[:, :, tkc],
                   ps[2][:, c * 256:(c + 1) * 256].rearrange("p (a b) -> p a b", a=4), [ps_r[2]], Yraw_r)
            Ov = aT[:, 8:16, :].bitcast(F32).rearrange("p a t -> p (a t)").rearrange("p (a t) -> p a t", a=4)
            cp(Ov[:, :, tk], ps[5][:, :].rearrange("p (a b) -> p a b", a=4), [ps_r[5]], Oraw_r)

        Yv4 = aT[:, 0:8, :].bitcast(F32).rearrange("p a t -> p (a t)").rearrange("p (a t) -> p a t", a=4)
        Ov4 = aT[:, 8:16, :].bitcast(F32).rearrange("p a t -> p (a t)").rearrange("p (a t) -> p a t", a=4)
        for p in range(4):
            y = Yv4[:, p, :]
            cp(sq1[:, :], y, Yraw_r, [sq1_r])
            bkm = nextbank()
            mm(ps[bkm][:, :], blkones, sq1[:, :], True, True, [cm_r, sq1_r], [ps_r[bkm]])
            ts(F[0][:, :], ps[bkm][:, :], 1.0 / 64.0, None, ALU.mult, None, [ps_r[bkm]], [FR[0]])
            act(sq1[:, :], y, AF.Square, Yraw_r, [sq1_r])
            bke = nextbank()
            mm(ps[bke][:, :], blkones, sq1[:, :], True, True, [cm_r, sq1_r], [ps_r[bke]])
            tt(F[1][:, :], F[0][:, :], F[0][:, :], ALU.mult, [FR[0]], [FR[1]])
            stt(F[2][:, :], ps[bke][:, :], 1.0 / 64.0, F[1][:, :], ALU.mult, ALU.subtract, [ps_r[bke], FR[1]], [FR[2]])
            ts(F[2][:, :], F[2][:, :], 0.0, None, ALU.max, None, [FR[2]], [FR[2]])
            act(F[3][:, :], F[2][:, :], AF.Ln, [FR[2], pc_r], [FR[3]], bias=pcol(PC_EPS + 1))
            act(F[1][:, :], F[3][:, :], AF.Exp, [FR[3]], [FR[1]], scale=-0.5)
            tt(F[2][:, :], y, F[0][:, :], ALU.subtract, Yraw_r + [FR[0]], [FR[2]])
            tt(F[2][:, :], F[2][:, :], F[1][:, :], ALU.mult, [FR[2], FR[1]], [FR[2]])
            ts(F[2][:, :], F[2][:, :], pcol(PC_LW + p), pcol(PC_LB + p), ALU.mult, ALU.add, [FR[2], pc_r], [FR[2]])
            tt(F[2][:, :], F[2][:, :], bonus[:, p, :], ALU.add, [FR[2], bonus_r[p]], [FR[2]])
            tt(mixedT[:, 4 + p, :], F[2][:, :], gater[:, p, :], ALU.mult, [FR[2], gater_r[p]], [mixedT_r[4 + p]])
        for h in range(4):
            o = Ov4[:, h, :]
            act(sq1[:, :], o, AF.Square, Oraw_r, [sq1_r])
            bks = nextbank()
            mm(ps[bks][:, :], ones, sq1[:, :], True, True, [cm_r, sq1_r], [ps_r[bks]])
            act(F[3][:, :], ps[bks][:, :], AF.Ln, [ps_r[bks], pc_r], [FR[3]], bias=pcol(PC_EPS), scale=1.0 / 128.0)
            act(F[1][:, :], F[3][:, :], AF.Exp, [FR[3]], [FR[1]], scale=-0.5)
            tt(F[2][:, :], o, F[1][:, :], ALU.mult, Oraw_r + [FR[1]], [FR[2]])
            stt(mixedT[:, h, :], F[2][:, :], pcol(PC_GN), gateg[:, h, :], ALU.mult, ALU.mult,
                [FR[2], pc_r, gateg_r[h]], [mixedT_r[h]])

        for oc in range(8):
            slot, slot_r = stream_A(sout_s[oc], 1024)
            sv = slot[:, 0:1024].rearrange("p (k c) -> p k c", k=8)
            bk = nextbank()
            for fc in range(8):
                mm(ps[bk][:, :], sv[:, fc, :], mixedT[:, fc, :], fc == 0, fc == 7, [slot_r, mixedT_r[fc]], [ps_r[bk]])
            tt(xb[:, oc, :], ps[bk][:, :], xb[:, oc, :], ALU.add, [ps_r[bk], xr[oc]], [xr[oc]])

    xvs = [a.rearrange("(c p) t -> p c t", p=128) for a in xT_ds]
    ovs = [a.rearrange("(c p) t -> p c t", p=128) for a in out_ds]
    out_ops = []
    for n in range(NT):
        b = n % 2
        xb, xr = xT[b], xT_r[b]
        t0 = n * TT
        xv = xvs[t0 // TH]
        ov = ovs[t0 // TH]
        tl = t0 % TH
        P.add("pool", lambda e, xb=xb, tl=tl, xv=xv: e.dma_start(out=xb[:, :, :], in_=xv[:, :, tl:tl + TT]),
              writes=xr, dkey=("x", b))
        if cfg.get("ffn1", True):
            rmsnorm_to_h(xb, xr, PC_N1, 2)
            ffn(0, xb, xr, 2)
        if cfg.get("mix", True):
            mixer(n, xb, xr)
            if cfg.get("dbg", False):
                P.add("pool", lambda e, t0=t0: e.dma_start(out=dbg_d[:, :, t0:t0 + TT], in_=mixedT[:, :, :]),
                      reads=mixedT_r, dkey=("dbg", 0))
        if cfg.get("ffn2", True):
            rmsnorm_to_h(xb, xr, PC_N2, 2)
            ffn(1, xb, xr, 2)
        final_norm(xb, xr)
        op = P.add("pool", lambda e, xb=xb, tl=tl, ov=ov: e.dma_start(out=ov[:, :, tl:tl + TT], in_=xb[:, :, :]),
                   reads=xr, dkey=("o", b))
        out_ops.append(op)
    P.add("pool", lambda e: e.nop(), extra=out_ops[-2:])

    P.emit(nc, es)
    es.close()
    return nc


def _prep_shared(inp):
    f32 = np.float32
    m = {}
    for i, tag in ((1, "ffn1"), (2, "ffn2")):
        g = _kblocks(np.asarray(inp[f"{tag}_w_gate"][0], f32), 128)
        u = _kblocks(np.asarray(inp[f"{tag}_w_up"][0], f32), 128)
        m[f"wgu{i}"] = np.ascontiguousarray(np.stack([g, u], axis=2)).reshape(NFC, 128, 2048)
        m[f"wd{i}"] = _kblocks(np.asarray(inp[f"{tag}_w_down"][0], f32), 128).reshape(8, 128, DFF)
    win = np.asarray(inp["w_in"][0], f32)
    m["wing"] = _kblocks(win[:, 0:1536], 128).reshape(12, 128, 1024)
    m["winga"] = _kblocks(win[:, 1536:1552], 16).reshape(128, 128)
    m["winr"] = _kblocks(win[:, 1552:3344], 128).reshape(14, 128, 1024)
    mu = np.asarray(inp["rwkv_mu"][0], f32).reshape(14, 1, 128)
    m["mur"] = np.ascontiguousarray(np.broadcast_to(mu, (14, 128, 128)))
    m["wout"] = _kblocks(np.asarray(inp["w_out"][0], f32), 128).reshape(8, 128, 1024)
    m["aw2"] = np.ascontiguousarray(np.asarray(inp["gla_alpha_w2"][0], f32))
    m["w2"] = np.ascontiguousarray(np.asarray(inp["rwkv_w2"][0], f32))
    m["a2"] = np.ascontiguousarray(np.asarray(inp["rwkv_a2"][0], f32))
    m["g2"] = np.ascontiguousarray(np.asarray(inp["rwkv_g2"][0], f32))
    pc = np.zeros((128, PC_TOT), f32)
    pc[:, PC_N1:PC_N1 + 8] = _cols(np.asarray(inp["ffn1_norm"][0], f32), 8)
    pc[:, PC_NM:PC_NM + 8] = _cols(np.asarray(inp["mix_norm"][0], f32), 8)
    pc[:, PC_N2:PC_N2 + 8] = _cols(np.asarray(inp["ffn2_norm"][0], f32), 8)
    pc[:, PC_NF:PC_NF + 8] = _cols(np.asarray(inp["final_norm"], f32), 8)
    pc[:, PC_AB:PC_AB + 2] = _cols(np.asarray(inp["gla_alpha_b"][0], f32), 2)
    pc[:, PC_GN] = np.asarray(inp["gla_norm"][0], f32)
    for col, key in ((PC_W0, "rwkv_w0"), (PC_A0, "rwkv_a0"), (PC_KK, "rwkv_k_k"), (PC_KA, "rwkv_k_a"),
                     (PC_LW, "rwkv_ln_w"), (PC_LB, "rwkv_ln_b")):
        pc[:, col:col + 4] = _cols(np.asarray(inp[key][0], f32), 4)
    pc[:, PC_RK:PC_RK + 4] = _cols(np.asarray(inp["rwkv_r_k"][0], f32).reshape(512), 4)
    pc[:, PC_EPS] = EPS
    pc[:, PC_EPS + 1] = GN_EPS
    pc[:, PC_EPS + 2] = 1e-24
    pc[:, PC_EPS + 3] = 1.0
    m["pc"] = pc
    c = _host_consts()
    m["cm"] = c["cm"].reshape(128, 12 * 128)
    m["rst"] = c["rst"]
    return m


_CFG = {"ffn1": True, "mix": True, "ffn2": True}


def kernel(**inputs):
    x = np.asarray(inputs["x"], np.float32)
    B, T, _ = x.shape
    shared = _prep_shared(inputs)
    nc = build_program(T, _CFG)
    in_maps = []
    for b in range(B):
        m = dict(shared)
        xs = 2 if (T // TT) % 2 == 0 else 1
        th = T // xs
        for i in range(xs):
            m[f"xT{i}"] = np.ascontiguousarray(x[b, i * th:(i + 1) * th].T)
        in_maps.append(m)
    res = run_bass_kernel_spmd(nc, in_maps, core_ids=list(range(B)))
    xs = 2 if (T // TT) % 2 == 0 else 1
    out = np.stack([np.concatenate([r[f"outT{i}"].T for i in range(xs)], axis=0) for r in res.results], axis=0)
    return out.astype(np.float32)
```

```python
import numpy as np
from contextlib import ExitStack
import concourse.bass as bass
import concourse.mybir as mybir
from concourse.bass_utils import run_bass_kernel_spmd

F32 = mybir.dt.float32
BF16 = mybir.dt.bfloat16
ALU = mybir.AluOpType
AF = mybir.ActivationFunctionType

D = 1024
DFF = 2816
NFC = DFF // 128
TT = 512
SEQ = 8192
NCORES = 8
EPS = 1e-6
GN_EPS = 64e-5
GLA_W = 1552
RW_W = 1792
PROJ = 3344

ENGS = ("pe", "act", "dve", "pool", "sp")
SEM_CH = 30000


class Res:
    __slots__ = ("name", "last_w", "readers", "excl")

    def __init__(self, name, excl=False):
        self.name = name
        self.last_w = None
        self.readers = []
        self.excl = excl


class Op:
    __slots__ = ("eng", "fn", "deps", "dkey", "needs_sig", "sig", "n")

    def __init__(self, eng, fn, dkey):
        self.eng = eng
        self.fn = fn
        self.deps = []
        self.dkey = dkey
        self.needs_sig = dkey is not None
        self.sig = None
        self.n = None


class Prog:
    def __init__(self):
        self.ops = {e: [] for e in ENGS}
        self.nops = 0

    def add(self, eng, fn, reads=(), writes=(), dkey=None, extra=()):
        op = Op(eng, fn, dkey)
        if any(r.excl for r in reads):
            writes = list(writes) + [r for r in reads if r.excl]
            reads = [r for r in reads if not r.excl]
        deps = {}
        for r in reads:
            w = r.last_w
            if w is not None:
                deps[id(w)] = (w, True)
        for wr in writes:
            w = wr.last_w
            if w is not None and id(w) not in deps:
                deps[id(w)] = (w, False)
            for rd in wr.readers:
                if id(rd) not in deps:
                    deps[id(rd)] = (rd, False)
        for e in extra:
            deps[id(e)] = (e, True)
        for d, raw in deps.values():
            if d is op:
                continue
            if d.eng == eng and d.dkey is None:
                if eng == "pe" or not raw:
                    continue
            op.deps.append(d)
            d.needs_sig = True
        for r in reads:
            if dkey is None:
                r.readers = [x for x in r.readers if not (x.eng == eng and x.dkey is None)]
            r.readers.append(op)
        for wr in writes:
            wr.last_w = op
            wr.readers = []
        self.ops[eng].append(op)
        self.nops += 1
        return op

    def emit(self, nc, es):
        nsig = {e: 0 for e in ENGS}
        dcount = {}
        for e in ENGS:
            for op in self.ops[e]:
                if op.dkey is not None:
                    dcount[op.dkey] = dcount.get(op.dkey, 0) + 1
                    op.sig = ("d", op.dkey, 16 * dcount[op.dkey])
                elif op.needs_sig:
                    op.sig = ("e", e, nsig[e])
                    nsig[e] += 1
        esems = {}
        for e in ENGS:
            nch = (nsig[e] + SEM_CH - 1) // SEM_CH
            esems[e] = [es.enter_context(nc.semaphore(f"s_{e}_{i}")) for i in range(nch)]
        dsems = {k: es.enter_context(nc.semaphore("d_" + "_".join(str(x) for x in k))) for k in dcount}
        self.nsig = nsig
        block = es.enter_context(nc.Block())
        prog = self

        def run(engname, eng):
            known_e = {e: -1 for e in ENGS}
            known_d = {}
            fuse = engname in ("act", "dve")
            for op in prog.ops[engname]:
                waits = []
                for d in op.deps:
                    s = d.sig
                    if s[0] == "e":
                        if known_e[s[1]] >= s[2]:
                            continue
                        known_e[s[1]] = s[2]
                        waits.append((esems[s[1]][s[2] // SEM_CH], s[2] % SEM_CH + 1))
                    else:
                        if known_d.get(s[1], 0) >= s[2]:
                            continue
                        known_d[s[1]] = s[2]
                        waits.append((dsems[s[1]], s[2]))
                last = waits.pop() if (fuse and waits and op.dkey is None) else None
                for sm, v in waits:
                    eng.wait_ge(sm, v)
                ins = op.fn(eng)
                if last is not None:
                    ins._wait_ge(last[0], last[1])
                s = op.sig
                if s is not None:
                    if s[0] == "e":
                        ins.then_inc(esems[s[1]][s[2] // SEM_CH], 1)
                    else:
                        ins.then_inc(dsems[s[1]], 16)

        @block.tensor
        def _(e):
            run("pe", e)

        @block.scalar
        def _(e):
            run("act", e)

        @block.vector
        def _(e):
            run("dve", e)

        @block.gpsimd
        def _(e):
            run("pool", e)

        @block.sync
        def _(e):
            run("sp", e)


def _kblocks(w, cb):
    K, C = w.shape
    return np.ascontiguousarray(w.reshape(K // 128, 128, C // cb, cb).transpose(2, 1, 0, 3))


def _cols(v, n):
    return np.ascontiguousarray(v.reshape(n, 128).T)


def _host_consts():
    i = np.arange(128)
    same = (i[:, None] // 64) == (i[None, :] // 64)
    strictT = (same & (i[:, None] < i[None, :])).astype(np.float32)
    inclT = (same & (i[:, None] <= i[None, :])).astype(np.float32)
    strict = (same & (i[:, None] > i[None, :])).astype(np.float32)
    ident = np.eye(128, dtype=np.float32)
    blk = same.astype(np.float32)
    ones = np.ones((128, 128), np.float32)
    c = {}
    c["cm"] = np.ascontiguousarray(np.stack([strictT, inclT, strictT, inclT, strict, ident, blk, ones,
                                             inclT, inclT, inclT, inclT], axis=1))
    t = np.arange(TT)
    c["rst"] = np.ascontiguousarray(np.broadcast_to((t % 64 != 0).astype(np.float32)[None, :], (128, TT)))
    return c


PC_N1, PC_NM, PC_N2, PC_NF = 0, 8, 16, 24
PC_AB, PC_GN = 32, 34
PC_W0, PC_A0, PC_KK, PC_KA, PC_RK, PC_LW, PC_LB = 35, 39, 43, 47, 51, 55, 59
PC_NAB = 63
PC_EPS = 65
PC_TOT = 72


def build_program(T, cfg):
    NT = T // TT
    nc = bass.Bass("TRN2", target_bir_lowering=False)
    P = Prog()
    es = ExitStack()

    def din(name, shape, dt=F32):
        return nc.dram_tensor(name, list(shape), dt, kind="ExternalInput").ap()

    def dscr(name, shape, dt=BF16):
        return nc.dram_tensor(name, list(shape), dt, kind="Internal").ap()

    def sb(name, shape, dt):
        return es.enter_context(nc.sbuf_tensor("sb_" + name, list(shape), dt))

    XS = 2 if NT % 2 == 0 else 1
    TH = T // XS
    xT_ds = [din(f"xT{i}", [D, TH]) for i in range(XS)]
    out_ds = [nc.dram_tensor(f"outT{i}", [D, TH], F32, kind="ExternalOutput").ap() for i in range(XS)]
    wgu_d = [din(f"wgu{i}", [NFC, 128, 2048]) for i in (1, 2)]
    wd_d = [din(f"wd{i}", [8, 128, DFF]) for i in (1, 2)]
    wing_d = din("wing", [12, 128, 1024])
    winga_d = din("winga", [128, 128])
    winr_d = din("winr", [14, 128, 1024])
    mur_d = din("mur", [14, 128, 128])
    wout_d = din("wout", [8, 128, 1024])
    aw2_d = din("aw2", [16, 256])
    w2_d = din("w2", [64, 512])
    a2_d = din("a2", [64, 512])
    g2_d = din("g2", [128, 512])
    pc_d = din("pc", [128, PC_TOT])
    cm_d = din("cm", [128, 12 * 128])
    rst_d = din("rst", [128, TT])

    if cfg.get("dbg", False):
        dbg_d = nc.dram_tensor("dbg", [128, 8, T], BF16, kind="ExternalOutput").ap()
    sgu_s = [dscr(f"sgu{i}", [NFC, 128, 2048]) for i in (1, 2)]
    sd_s = [dscr(f"sd{i}", [8, 128, DFF]) for i in (1, 2)]
    sing_s = dscr("sing", [12, 128, 1024])
    singa_s = dscr("singa", [128, 128])
    sinr_s = dscr("sinr", [14, 128, 2048])
    sout_s = dscr("sout", [8, 128, 1024])

    xT = [sb(f"xT{i}", [128, 8, TT], F32) for i in range(2)]
    xT_r = [[Res(f"xT{i}_{c}") for c in range(8)] for i in range(2)]
    hT = sb("hT", [128, 8, TT + 2], BF16)
    hT_r = Res("hT")
    aT = sb("aT", [128, NFC, TT], BF16)
    aT_r = [Res(f"aT{j}") for j in range(NFC)]
    NRA, NRB = 4, 2
    ringA = [sb(f"ringA{i}", [128, 2048], BF16) for i in range(NRA)]
    ringA_r = [Res(f"ringA{i}") for i in range(NRA)]
    ringB = [sb(f"ringB{i}", [128, DFF], BF16) for i in range(NRB)]
    ringB_r = [Res(f"ringB{i}") for i in range(NRB)]
    pc = sb("pc", [128, PC_TOT], F32)
    pc_r = Res("pc")
    cmb = sb("cmb", [128, 12, 128], BF16)
    cm_r = Res("cm")
    rst = sb("rst", [128, TT], F32)
    NFT = 14
    ftmp = [sb(f"ftmp{i}", [128, TT], F32) for i in range(NFT)]
    ftmp_r = [Res(f"ftmp{i}") for i in range(NFT)]
    sqT = aT[:, 0:8, :]

    ps = [es.enter_context(nc.psum_tensor(f"ps{i}", [128, 512], F32)) for i in range(8)]
    ps_r = [Res(f"ps{i}", excl=True) for i in range(8)]

    ident = cmb[:, 5, :]
    blkones = cmb[:, 6, :]
    ones = cmb[:, 7, :]

    pro_stores = []

    last_by_key = {}
    scr_ops = {}

    def id_of(ap_):
        return (ap_.tensor.name, ap_.offset)

    def dram_cast(dst, src, key):
        prev = last_by_key.get(key)
        op = P.add("pool", lambda e, d=dst, s=src: e.dma_start(out=d, in_=s), dkey=key,
                   extra=(prev,) if prev is not None else ())
        last_by_key[key] = op
        pro_stores.append(op)
        scr_ops.setdefault(id_of(dst), []).append(op)

    kctr = [0]

    def nkey(base):
        kctr[0] += 1
        return (base, kctr[0] % 4)

    def cast_ffn(f):
        for j in range(NFC):
            dram_cast(sgu_s[f][j], wgu_d[f][j], nkey("pc"))
        for o in range(8):
            dram_cast(sd_s[f][o], wd_d[f][o], nkey("pc"))

    cast_ffn(0)
    dram_cast(singa_s, winga_d, nkey("pc"))
    for b in range(12):
        dram_cast(sing_s[b], wing_d[b], nkey("pc"))

    P.add("sp", lambda e: e.dma_start(out=pc[:, :], in_=pc_d), writes=[pc_r], dkey=("c", 0))
    P.add("sp", lambda e: e.dma_start(out=rst[:, :], in_=rst_d), writes=[cm_r], dkey=("c", 1))
    for h in range(3):
        P.add("sp", lambda e, h=h: e.dma_start(out=ftmp[h][:, :], in_=cm_d[:, h * 512:(h + 1) * 512]),
              writes=[ftmp_r[h]], dkey=("c", 2 + h))
        P.add("dve", lambda e, h=h: e.tensor_copy(cmb[:, 4 * h:4 * h + 4, :],
                                                  ftmp[h][:, :].rearrange("p (a b) -> p a b", a=4)),
              reads=[ftmp_r[h]], writes=[cm_r])
    P.add("dve", lambda e: e.tensor_scalar(pc[:, PC_NAB:PC_NAB + 2], pc[:, PC_AB:PC_AB + 2], -1.0, None, ALU.mult),
          reads=[pc_r], writes=[pc_r])

    ra = [0]
    rb = [0]
    first_stream = [True]

    def stream_A(src, ncols=2048):
        k = ra[0] % NRA
        ra[0] += 1
        extra = scr_ops.get(id_of(src), pro_stores)
        P.add("sp", lambda e, k=k, s=src, n=ncols: e.dma_start(out=ringA[k][:, 0:n], in_=s),
              writes=[ringA_r[k]], dkey=("ra", k), extra=extra)
        return ringA[k], ringA_r[k]

    def stream_B(src):
        k = rb[0] % NRB
        rb[0] += 1
        P.add("sp", lambda e, k=k, s=src: e.dma_start(out=ringB[k][:, :], in_=s),
              writes=[ringB_r[k]], dkey=("rb", k), extra=scr_ops.get(id_of(src), pro_stores))
        return ringB[k], ringB_r[k]

    def rmsnorm_to_h(xb, xr, gcol, hoff):
        P.add("act", lambda e: e.activation(sqT, xb[:, :, :], AF.Square), reads=xr, writes=aT_r[0:8])
        for c in range(8):
            P.add("pe", lambda e, c=c: e.matmul(ps[6][:, :], ones, sqT[:, c, :], start=(c == 0), stop=(c == 7)),
                  reads=aT_r[0:8] + [cm_r], writes=[ps_r[6]])
        P.add("act", lambda e: e.activation(ftmp[2][:, :], ps[6][:, :], AF.Ln, bias=pc[:, PC_EPS:PC_EPS + 1],
                                            scale=1.0 / D), reads=[ps_r[6], pc_r], writes=[ftmp_r[2]])
        P.add("act", lambda e: e.activation(ftmp[3][:, :], ftmp[2][:, :], AF.Exp, scale=-0.5),
              reads=[ftmp_r[2]], writes=[ftmp_r[3]])
        for c in range(8):
            P.add("dve", lambda e, c=c: e.scalar_tensor_tensor(
                hT[:, c, hoff:hoff + TT], xb[:, c, :], pc[:, gcol + c:gcol + c + 1], ftmp[3][:, :],
                ALU.mult, ALU.mult), reads=[xr[c], ftmp_r[3], pc_r], writes=[hT_r])

    def ffn(f, xb, xr, hoff):
        for j in range(NFC):
            slot, slot_r = stream_A(sgu_s[f][j])
            sv = slot[:, :].rearrange("p (g k c) -> p g k c", g=2, k=8)
            pg, pu = 0 + (j % 2), 2 + (j % 2)
            for kc in range(8):
                P.add("pe", lambda e, kc=kc, sv=sv, pg=pg: e.matmul(
                    ps[pg][:, :], sv[:, 0, kc, :], hT[:, kc, hoff:hoff + TT], start=(kc == 0), stop=(kc == 7)),
                    reads=[slot_r, hT_r], writes=[ps_r[pg]])
            for kc in range(8):
                P.add("pe", lambda e, kc=kc, sv=sv, pu=pu: e.matmul(
                    ps[pu][:, :], sv[:, 1, kc, :], hT[:, kc, hoff:hoff + TT], start=(kc == 0), stop=(kc == 7)),
                    reads=[slot_r, hT_r], writes=[ps_r[pu]])
            ft = j % 2
            P.add("act", lambda e, ft=ft, pg=pg: e.activation(ftmp[ft][:, :], ps[pg][:, :], AF.Silu),
                  reads=[ps_r[pg]], writes=[ftmp_r[ft]])
            P.add("dve", lambda e, ft=ft, pu=pu, j=j: e.tensor_tensor(aT[:, j, :], ftmp[ft][:, :], ps[pu][:, :], ALU.mult),
                  reads=[ftmp_r[ft], ps_r[pu]], writes=[aT_r[j]])
        for o in range(8):
            slot, slot_r = stream_B(sd_s[f][o])
            sv = slot[:, :].rearrange("p (j c) -> p j c", j=NFC)
            pd = 4 + (o % 2)
            for j in range(NFC):
                P.add("pe", lambda e, j=j, sv=sv, pd=pd: e.matmul(
                    ps[pd][:, :], sv[:, j, :], aT[:, j, :], start=(j == 0), stop=(j == NFC - 1)),
                    reads=[slot_r, aT_r[j]], writes=[ps_r[pd]])
            P.add("dve", lambda e, o=o, pd=pd: e.scalar_tensor_tensor(
                xb[:, o, :], ps[pd][:, :], 0.5, xb[:, o, :], ALU.mult, ALU.add),
                reads=[ps_r[pd], xr[o]], writes=[xr[o]])

    def final_norm(xb, xr):
        P.add("act", lambda e: e.activation(sqT, xb[:, :, :], AF.Square), reads=xr, writes=aT_r[0:8])
        for c in range(8):
            P.add("pe", lambda e, c=c: e.matmul(ps[6][:, :], ones, sqT[:, c, :], start=(c == 0), stop=(c == 7)),
                  reads=aT_r[0:8] + [cm_r], writes=[ps_r[6]])
        P.add("act", lambda e: e.activation(ftmp[2][:, :], ps[6][:, :], AF.Ln, bias=pc[:, PC_EPS:PC_EPS + 1],
                                            scale=1.0 / D), reads=[ps_r[6], pc_r], writes=[ftmp_r[2]])
        P.add("act", lambda e: e.activation(ftmp[3][:, :], ftmp[2][:, :], AF.Exp, scale=-0.5),
              reads=[ftmp_r[2]], writes=[ftmp_r[3]])
        for c in range(8):
            P.add("dve", lambda e, c=c: e.scalar_tensor_tensor(
                xb[:, c, :], xb[:, c, :], pc[:, PC_NF + c:PC_NF + c + 1], ftmp[3][:, :],
                ALU.mult, ALU.mult), reads=[xr[c], ftmp_r[3], pc_r], writes=[xr[c]])

    CDEC = 0.6065306597126334

    def RL(name, n):
        return [Res(f"{name}{i}") for i in range(n)]

    hprev = sb("hprev", [128, 8, 1], BF16); hprev_r = Res("hprev")
    aw2b = sb("aw2b", [16, 256], BF16)
    w2a2b = sb("w2a2b", [128, 512], BF16)
    g2b = sb("g2b", [128, 512], BF16)
    smallw_r = Res("smallw")
    qkg = sb("qkg", [128, 4, TT], BF16); qkg_r = RL("qkg", 4)
    vTg = sb("vTg", [128, 4, TT], BF16); vTg_r = RL("vTg", 4)
    gateg = sb("gateg", [128, 4, TT], BF16); gateg_r = RL("gateg", 4)
    gamCg = sb("gamCg", [128, 2, 8], F32); gamCg_r = Res("gamCg")
    alr = sb("alr", [16, TT], BF16); alr_r = Res("alr")
    AR = sb("AR", [128, 4, 2, TT], BF16); AR_r = RL("AR", 4)
    Bt = sb("Bt", [128, 4, TT], BF16); Bt_r = RL("Bt", 4)
    Kt = sb("Kt", [128, 4, TT], BF16); Kt_r = RL("Kt", 4)
    vTr = sb("vTr", [128, 4, TT], BF16); vTr_r = RL("vTr", 4)
    bonus = sb("bonus", [128, 4, TT], BF16); bonus_r = RL("bonus", 4)
    gater = sb("gater", [128, 4, TT], BF16); gater_r = RL("gater", 4)
    gamCr = sb("gamCr", [128, 4, 8], F32); gamCr_r = Res("gamCr")
    wa = sb("wa", [128, TT], BF16); wa_r = Res("wa")
    sgl = sb("sgl", [128, TT], BF16); sgl_r = Res("sgl")
    sq1 = sb("sq1", [128, TT], BF16); sq1_r = Res("sq1")
    Hs = sb("Hs", [128, 4, 64], F32); Hs_r = Res("Hs")
    Hbd = sb("Hbd", [128, 4, 128], BF16); Hbd_r = Res("Hbd")
    Sg = sb("Sg", [128, 2, 128], F32); Sg_r = Res("Sg")
    Sgbd = sb("Sgbd", [128, 4, 128], BF16); Sgbd_r = Res("Sgbd")
    Wb = sb("Wb", [128, 512], BF16); Wb_r = Res("Wb")
    Ub = sb("Ub", [128, 512], BF16); Ub_r = Res("Ub")
    tokBK = sb("tokBK", [128, 1024], BF16); tokBK_r = Res("tokBK")
    tokV = sb("tokV", [128, 1024], BF16); tokV_r = Res("tokV")
    tokKg = sb("tokKg", [128, 256], BF16); tokKg_r = Res("tokKg")
    Sball = sb("Sball", [128, 8, 4, 128], BF16); Sball_r = RL("Sball", 8)
    TTall = sb("TTall", [128, 8, 128], BF16); TTall_r = [Res("TTall")]
    Pb = [[sb(f"Pb{a}{b}", [128, 512], BF16) for b in range(2)] for a in range(4)]
    Pb_r = [[Res(f"Pb{a}{b}") for b in range(2)] for a in range(4)]
    STb = sb("STb", [128, 512], BF16); STb_r = Res("STb")
    tmpH = ftmp[0][:, 0:256]; tmpH_r = ftmp_r[0]
    tmpS = ftmp[1][:, 0:256]; tmpS_r = ftmp_r[1]
    mixedT = sb("mixedT", [128, 8, TT], BF16); mixedT_r = RL("mixedT", 8)
    Yraw = aT[:, 0:8, :].bitcast(F32)
    Oraw = aT[:, 8:16, :].bitcast(F32)
    Yraw_r = aT_r[0:8]
    Oraw_r = aT_r[8:16]

    def mm(out, lhsT, rhs, start, stop, reads, writes):
        return P.add("pe", lambda e: e.matmul(out, lhsT, rhs, start=start, stop=stop), reads, writes)

    def tr(out, in_, reads, writes):
        return P.add("pe", lambda e: e.transpose(out, in_, ident), list(reads) + [cm_r], writes)

    def act(out, in_, func, reads, writes, bias=None, scale=None):
        kw = {}
        if bias is not None:
            kw["bias"] = bias
        if scale is not None:
            kw["scale"] = scale
        return P.add("act", lambda e: e.activation(out, in_, func, **kw), reads, writes)

    def tt(out, in0, in1, op, reads, writes, eng="dve"):
        return P.add(eng, lambda e: e.tensor_tensor(out, in0, in1, op), reads, writes)

    def ts(out, in0, s1, s2, op0, op1, reads, writes, eng="dve"):
        if s2 is None:
            return P.add(eng, lambda e: e.tensor_scalar(out, in0, s1, None, op0), reads, writes)
        return P.add(eng, lambda e: e.tensor_scalar(out, in0, s1, s2, op0, op1), reads, writes)

    def stt(out, in0, sc, in1, op0, op1, reads, writes):
        return P.add("dve", lambda e: e.scalar_tensor_tensor(out, in0, sc, in1, op0, op1), reads, writes)

    def cp(out, in_, reads, writes, eng="act"):
        if eng == "act":
            return P.add("act", lambda e: e.activation(out, in_, AF.Copy), reads, writes)
        return P.add(eng, lambda e: e.tensor_copy(out, in_), reads, writes)

    def scan(out, d0, d1, reads, writes):
        return P.add("dve", lambda e: e.tensor_tensor_scan(out, d0, d1, 0.0, ALU.mult, ALU.add), reads, writes)

    def pcol(c):
        return pc[:, c:c + 1]

    def load_small(dst, src_d, rows, cols, ft, prow=0):
        P.add("sp", lambda e: e.dma_start(out=ftmp[ft][prow:prow + rows, 0:cols], in_=src_d),
              writes=[ftmp_r[ft]], dkey=("c", 5 + ft))
        cp(dst, ftmp[ft][prow:prow + rows, 0:cols], [ftmp_r[ft]], [smallw_r], eng="dve")

    load_small(aw2b[0:16, :], aw2_d, 16, 256, 3)
    load_small(w2a2b[0:64, :], w2_d, 64, 512, 4)
    load_small(w2a2b[64:128, :], a2_d, 64, 512, 5, prow=64)
    load_small(g2b[:, :], g2_d, 128, 512, 6)
    for tl, tr_ in ((Hs, Hs_r), (Hbd, Hbd_r), (Sg, Sg_r), (Sgbd, Sgbd_r), (Wb, Wb_r), (Ub, Ub_r), (hprev, hprev_r)):
        ap_ = tl[:, :, :] if len(tl.shape) == 3 else tl[:, :]
        P.add("pool", lambda e, a=ap_: e.memset(a, 0.0), writes=[tr_])
    stg_r = [Res("stgA"), Res("stgB")]
    for blk in range(14):
        s2 = blk % 2
        fW, fM, fO = ftmp[7 + 3 * s2], ftmp[8 + 3 * s2], ftmp[9 + 3 * s2]
        rW, rM, rO = ftmp_r[7 + 3 * s2], ftmp_r[8 + 3 * s2], ftmp_r[9 + 3 * s2]
        for half in range(2):
            P.add("sp", lambda e, blk=blk, half=half, fW=fW: e.dma_start(
                out=fW[:, :], in_=winr_d[blk][:, half * 512:(half + 1) * 512]), writes=[rW], dkey=("pw", s2))
            if half == 0:
                P.add("sp", lambda e, blk=blk, fM=fM: e.dma_start(out=fM[:, 0:128], in_=mur_d[blk]),
                      writes=[rM], dkey=("pm", s2))
                ts(fM[:, 128:256], fM[:, 0:128], -1.0, 1.0, ALU.mult, ALU.add, [rM], [rM])
            fWv = fW[:, :].rearrange("p (k c) -> p k c", k=4)
            fOv = fO[:, :].bitcast(BF16).rearrange("p (k c) -> p k c", k=8)
            tt(fOv[:, 0:4, :], fWv, fM[:, 128:256].unsqueeze(1).to_broadcast([128, 4, 128]), ALU.mult, [rW, rM], [rO])
            tt(fOv[:, 4:8, :], fWv, fM[:, 0:128].unsqueeze(1).to_broadcast([128, 4, 128]), ALU.mult, [rW, rM], [rO])
            dv = sinr_s[blk].rearrange("p (k c) -> p k c", k=16)
            op1 = P.add("pool", lambda e, dv=dv, fOv=fOv, half=half: e.dma_start(
                out=dv[:, half * 4:half * 4 + 4, :], in_=fOv[:, 0:4, :]), reads=[rO], dkey=("ps1", s2))
            op2 = P.add("pool", lambda e, dv=dv, fOv=fOv, half=half: e.dma_start(
                out=dv[:, 8 + half * 4:8 + half * 4 + 4, :], in_=fOv[:, 4:8, :]), reads=[rO], dkey=("ps2", s2))
            pro_stores.append(op1)
            pro_stores.append(op2)
            scr_ops.setdefault(id_of(sinr_s[blk]), []).extend([op1, op2])
    for o in range(8):
        dram_cast(sout_s[o], wout_d[o], nkey("pc"))
    cast_ffn(1)

    bank_rr = [0]

    def nextbank():
        b_ = bank_rr[0] % 8
        bank_rr[0] += 1
        return b_

    def proj_block(src, ncols, K16, M=128):
        slot, slot_r = stream_A(src, ncols)
        nk = 16 if K16 else 8
        sv = slot[:, 0:ncols].rearrange("p (k c) -> p k c", k=nk)
        bk = nextbank()
        for kc in range(nk):
            rhs = hT[:, kc, 2:TT + 2] if kc < 8 else hT[:, kc - 8, 1:TT + 1]
            mm(ps[bk][0:M, :], sv[:, kc, :], rhs, kc == 0, kc == nk - 1, [slot_r, hT_r], [ps_r[bk]])
        return bk

    def mixer(n, xb, xr):
        rmsnorm_to_h(xb, xr, PC_NM, 2)
        cp(hT[:, :, 1:2], hprev[:, :, :], [hprev_r], [hT_r], eng="pool")
        cp(hprev[:, :, :], hT[:, :, TT + 1:TT + 2], [hT_r], [hprev_r], eng="pool")

        stage = float(cfg.get("stage", 99))
        if stage < 1:
            return
        bk = proj_block(singa_s, 128, False, M=16)
        cp(alr[0:16, :], ps[bk][0:16, :], [ps_r[bk]], [alr_r])
        if stage < 0.5:
            return
        for c in range(2):
            bk = nextbank()
            mm(ps[bk][:, :], aw2b[0:16, c * 128:(c + 1) * 128], alr[0:16, :], True, True, [smallw_r, alr_r], [ps_r[bk]])
            act(ftmp[8][:, :], ps[bk][:, :], AF.Exp, [ps_r[bk], pc_r], [ftmp_r[8]], bias=pcol(PC_NAB + c), scale=-1.0)
            act(ftmp[9][:, :], ftmp[8][:, :], AF.Ln, [ftmp_r[8], pc_r], [ftmp_r[9]], bias=pcol(PC_EPS + 3))
            scan(ftmp[10][:, :], rst[:, :], ftmp[9][:, :], [ftmp_r[9], cm_r], [ftmp_r[10]])
            act(ftmp[4 + c][:, :], ftmp[10][:, :], AF.Exp, [ftmp_r[10]], [ftmp_r[4 + c]], scale=-1.0 / 16.0)
            act(ftmp[6 + c][:, :], ftmp[10][:, :], AF.Exp, [ftmp_r[10]], [ftmp_r[6 + c]], scale=1.0 / 16.0)
            cp(gamCg[:, c, :], ftmp[4 + c][:, :].rearrange("p (a b) -> p a b", b=64)[:, :, 63],
               [ftmp_r[4 + c]], [gamCg_r], eng="dve")
        if stage < 0.7:
            return
        for c in range(2):
            bk = proj_block(sing_s[c], 1024, False)
            stt(qkg[:, c, :], ps[bk][:, :], 0.125, ftmp[4 + c][:, :], ALU.mult, ALU.mult,
                [ps_r[bk], ftmp_r[4 + c]], [qkg_r[c]])
        for c in range(2):
            bk = proj_block(sing_s[2 + c], 1024, False)
            tt(qkg[:, 2 + c, :], ps[bk][:, :], ftmp[6 + c][:, :], ALU.mult, [ps_r[bk], ftmp_r[6 + c]], [qkg_r[2 + c]])
        for h in range(4):
            bk = proj_block(sing_s[4 + h], 1024, False)
            cp(vTg[:, h, :], ps[bk][:, :], [ps_r[bk]], [vTg_r[h]])
        for h in range(4):
            bk = proj_block(sing_s[8 + h], 1024, False)
            act(gateg[:, h, :], ps[bk][:, :], AF.Silu, [ps_r[bk]], [gateg_r[h]])

        if stage < 2:
            return
        bk = proj_block(sinr_s[12], 2048, True)
        act(wa[0:64, :], ps[bk][0:64, :], AF.Tanh, [ps_r[bk]], [wa_r])
        cp(wa[64:128, :], ps[bk][64:128, :], [ps_r[bk]], [wa_r])
        bk = proj_block(sinr_s[13], 2048, True)
        act(sgl[:, :], ps[bk][:, :], AF.Sigmoid, [ps_r[bk]], [sgl_r])
        for p in range(4):
            bk = nextbank()
            mm(ps[bk][:, :], g2b[:, p * 128:(p + 1) * 128], sgl[:, :], True, True, [smallw_r, sgl_r], [ps_r[bk]])
            cp(gater[:, p, :], ps[bk][:, :], [ps_r[bk]], [gater_r[p]])
        F = ftmp
        FR = ftmp_r
        if stage < 1.5:
            return
        for p in range(4):
            bk = nextbank()
            mm(ps[bk][:, :], w2a2b[0:64, p * 128:(p + 1) * 128], wa[0:64, :], True, True, [smallw_r, wa_r], [ps_r[bk]])
            act(F[8][:, :], ps[bk][:, :], AF.Sigmoid, [ps_r[bk], pc_r], [FR[8]], bias=pcol(PC_W0 + p))
            if stage < 1.6:
                continue
            bk = nextbank()
            mm(ps[bk][:, :], w2a2b[64:128, p * 128:(p + 1) * 128], wa[64:128, :], True, True, [smallw_r, wa_r], [ps_r[bk]])
            act(F[9][:, :], ps[bk][:, :], AF.Sigmoid, [ps_r[bk], pc_r], [FR[9]], bias=pcol(PC_A0 + p))
            if stage < 1.7:
                continue
            scan(F[10][:, :], rst[:, :], F[8][:, :], [FR[8], cm_r], [FR[10]])
            act(F[0][:, :], F[10][:, :], AF.Exp, [FR[10]], [FR[0]], scale=-CDEC)
            act(F[1][:, :], F[10][:, :], AF.Exp, [FR[10]], [FR[1]], scale=CDEC)
            tt(F[11][:, :], F[10][:, :], F[8][:, :], ALU.subtract, [FR[10], FR[8]], [FR[11]])
            act(F[2][:, :], F[11][:, :], AF.Exp, [FR[11]], [FR[2]], scale=-CDEC)
            cp(gamCr[:, p, :], F[0][:, :].rearrange("p (a b) -> p a b", b=64)[:, :, 63], [FR[0]], [gamCr_r], eng="dve")
            if stage < 1.8:
                continue
            bkK = proj_block(sinr_s[4 + p], 2048, True)
            ts(F[11][:, :], ps[bkK][:, :], pcol(PC_KK + p), None, ALU.mult, None, [ps_r[bkK], pc_r], [FR[11]])
            act(sq1[:, :], F[11][:, :], AF.Square, [FR[11]], [sq1_r])
            bk = nextbank()
            mm(ps[bk][:, :], blkones, sq1[:, :], True, True, [cm_r, sq1_r], [ps_r[bk]])
            act(F[3][:, :], ps[bk][:, :], AF.Ln, [ps_r[bk], pc_r], [FR[3]], bias=pcol(PC_EPS + 2))
            act(F[12][:, :], F[3][:, :], AF.Exp, [FR[3]], [FR[12]], scale=-0.5)
            tt(F[11][:, :], F[11][:, :], F[12][:, :], ALU.mult, [FR[11], FR[12]], [FR[11]])
            if stage < 1.9:
                continue
            tt(F[3][:, :], F[11][:, :], F[9][:, :], ALU.mult, [FR[11], FR[9]], [FR[3]])
            tt(Bt[:, p, :], F[3][:, :], F[1][:, :], ALU.mult, [FR[3], FR[1]], [Bt_r[p]])
            stt(AR[:, p, 0, :], F[11][:, :], -1.0, F[2][:, :], ALU.mult, ALU.mult, [FR[11], FR[2]], [AR_r[p]])
            ts(F[3][:, :], F[9][:, :], -1.0, pcol(PC_KA + p), ALU.add, ALU.mult, [FR[9], pc_r], [FR[3]])
            stt(F[13][:, :], F[3][:, :], 1.0, ps[bkK][:, :], ALU.add, ALU.mult, [FR[3], ps_r[bkK]], [FR[13]])
            tt(Kt[:, p, :], F[13][:, :], F[1][:, :], ALU.mult, [FR[13], FR[1]], [Kt_r[p]])
            if stage < 1.95:
                continue
            bkR = proj_block(sinr_s[p], 2048, True)
            tt(AR[:, p, 1, :], ps[bkR][:, :], F[0][:, :], ALU.mult, [ps_r[bkR], FR[0]], [AR_r[p]])
            if stage < 1.98:
                continue
            stt(sq1[:, :], ps[bkR][:, :], pcol(PC_RK + p), F[13][:, :], ALU.mult, ALU.mult,
                [ps_r[bkR], pc_r, FR[13]], [sq1_r])
            if stage < 1.985:
                continue
            bk = nextbank()
            mm(ps[bk][:, :], blkones, sq1[:, :], True, True, [cm_r, sq1_r], [ps_r[bk]])
            cp(F[12][:, :], ps[bk][:, :], [ps_r[bk]], [FR[12]])
            if stage < 1.99:
                continue
            bkV = proj_block(sinr_s[8 + p], 2048, True)
            cp(vTr[:, p, :], ps[bkV][:, :], [ps_r[bkV]], [vTr_r[p]])
            if stage < 1.995:
                continue
            tt(bonus[:, p, :], ps[bkV][:, :], F[12][:, :], ALU.mult, [ps_r[bkV], FR[12]], [bonus_r[p]])

        if stage < 3:
            return
        ps0b = ps[0][:, :].bitcast(BF16)
        ps1b = ps[1][:, :].bitcast(BF16)
        Yv = Yraw.rearrange("p a (b t) -> p (a b) t", b=2) if False else None
        for s in range(TT // 128):
            tk = slice(s * 128, (s + 1) * 128)
            for p in range(4):
                tr(ps0b[:, p * 128:(p + 1) * 128], Bt[:, p, tk], [Bt_r[p]], [ps_r[0]])
            for p in range(4):
                tr(ps0b[:, 512 + p * 128:512 + (p + 1) * 128], Kt[:, p, tk], [Kt_r[p]], [ps_r[0]])
            cp(tokBK[:, :], ps0b, [ps_r[0]], [tokBK_r], eng="dve")
            for p in range(4):
                tr(ps1b[:, p * 128:(p + 1) * 128], vTr[:, p, tk], [vTr_r[p]], [ps_r[1]])
            for h in range(4):
                tr(ps1b[:, 512 + h * 128:512 + (h + 1) * 128], vTg[:, h, tk], [vTg_r[h]], [ps_r[1]])
            cp(tokV[:, :], ps1b, [ps_r[1]], [tokV_r])
            for c in range(2):
                tr(ps0b[:, c * 128:(c + 1) * 128], qkg[:, 2 + c, tk], [qkg_r[2 + c]], [ps_r[0]])
            cp(tokKg[:, :], ps0b[:, 0:256], [ps_r[0]], [tokKg_r], eng="dve")

            TTv = TTall[:, :, :].rearrange("q (p s) c -> q s p c", s=2)
            order = [(p_, 0) for p_ in range(4)] + [(p_, 1) for p_ in range(4)]
            for idx, (p, s2) in enumerate(order):
                h = 2 * p + s2
                hp = slice(s2 * 64, (s2 + 1) * 64)
                SB = (0, 1, 6, 7)[idx % 4]
                bi = s2 * 2 + p // 2
                PB = 2 + bi
                c0 = (p % 2) * 256
                arv = AR[hp, p, :, tk]
                mm(ps[SB][:, 0:256].rearrange("p (a b) -> p a b", a=2), Bt[hp, p, tk], arv, True, True,
                   [Bt_r[p], AR_r[p]], [ps_r[SB]])
                mm(ps[SB][:, 256:512].rearrange("p (a b) -> p a b", a=2), Kt[hp, p, tk], arv, True, True,
                   [Kt_r[p], AR_r[p]], [ps_r[SB]])
                mm(ps[PB][:, c0:c0 + 128], AR[hp, p, 0, tk], Bt[hp, p, tk], True, True, [Bt_r[p], AR_r[p]], [ps_r[PB]])
                tt(Sball[:, h, :, :], ps[SB][:, :].rearrange("p (a b) -> p a b", a=4), cmb[:, 0:4, :], ALU.mult,
                   [ps_r[SB], cm_r], [Sball_r[h]])
                tt(Pb[bi][0][:, c0:c0 + 128], ps[PB][:, c0:c0 + 128], cmb[:, 4, :], ALU.mult, [ps_r[PB], cm_r], [Pb_r[bi][0]])
                cp(Pb[bi][0][:, c0 + 128:c0 + 256], Sball[:, h, 0, :], [Sball_r[h]], [Pb_r[bi][0]], eng="pool")
                tt(TTall[:, h, :], Sball[:, h, 0, :], ident, ALU.add, [Sball_r[h], cm_r], TTall_r, eng="pool")
            for lv in range(5):
                for bi in range(4):
                    cur, cur_r = Pb[bi][lv % 2], Pb_r[bi][lv % 2]
                    for c0 in (0, 256):
                        mm(ps[2 + bi][:, c0:c0 + 128], cur[:, c0 + 128:c0 + 256], cur[:, c0:c0 + 128], True, True,
                           [cur_r], [ps_r[2 + bi]])
                        if lv < 4:
                            mm(ps[2 + bi][:, c0 + 128:c0 + 256], cur[:, c0:c0 + 128], cur[:, c0 + 128:c0 + 256], True, True,
                               [cur_r], [ps_r[2 + bi]])
                for bi in range(4):
                    nxt, nxt_r = Pb[bi][(lv + 1) % 2], Pb_r[bi][(lv + 1) % 2]
                    ev_eng = "dve" if bi == 3 else "act"
                    if lv < 4:
                        cp(nxt[:, :], ps[2 + bi][:, :], [ps_r[2 + bi]], [nxt_r], eng=ev_eng)
                    else:
                        cp(nxt[:, :].rearrange("q (a b) -> q a b", a=2)[:, :, 0:128],
                           ps[2 + bi][:, :].rearrange("q (a b) -> q a b", a=2)[:, :, 0:128], [ps_r[2 + bi]], [nxt_r], eng=ev_eng)
                for s2 in range(2):
                    for p in range(4):
                        h = 2 * p + s2
                        bi = s2 * 2 + p // 2
                        c0 = (p % 2) * 256
                        nxt, nxt_r = Pb[bi][(lv + 1) % 2], Pb_r[bi][(lv + 1) % 2]
                        mm(ps[6 + s2][:, p * 128:(p + 1) * 128], nxt[:, c0:c0 + 128], TTall[:, h, :], True, True,
                           [nxt_r] + TTall_r, [ps_r[6 + s2]])
                for s2 in range(2):
                    tt(TTv[:, s2, :, :], TTv[:, s2, :, :], ps[6 + s2][:, :].rearrange("q (p c) -> q p c", p=4), ALU.add,
                       TTall_r + [ps_r[6 + s2]], TTall_r)

            for h in range(4):
                p, s2 = h // 2, h % 2
                hp = slice(s2 * 64, (s2 + 1) * 64)
                mm(ps[4 + s2][:, p * 128:(p + 1) * 128], qkg[hp, 2 + p, tk], qkg[hp, p, tk], True, True,
                   [qkg_r[p], qkg_r[2 + p]], [ps_r[4 + s2]])
            STv = STb[:, :].rearrange("p (a b c) -> p a b c", a=2, b=2)
            for s2 in range(2):
                tt(STv[:, :, s2, :], ps[4 + s2][:, 0:256].rearrange("p (a c) -> p a c", a=2), cmb[:, 8:10, :], ALU.mult,
                   [ps_r[4 + s2], cm_r], [STb_r])

            for c in range(2):
                cs = slice(c * 64, (c + 1) * 64)
                tkc = slice(s * 128 + c * 64, s * 128 + (c + 1) * 64)
                ci = s * 2 + c
                for h in range(8):
                    p, s2 = h // 2, h % 2
                    o = ps[6][cs, h * 64:(h + 1) * 64]
                    mm(o, AR[:, p, 0, tkc], Hbd[:, p, s2 * 64:(s2 + 1) * 64], True, False, [AR_r[p], Hbd_r], [ps_r[6]])
                    mm(o, Sball[:, h, 2, cs], tokV[:, p * 128 + s2 * 64:p * 128 + (s2 + 1) * 64], False, True,
                       [Sball_r[h], tokV_r], [ps_r[6]])
                for h in range(4):
                    p = h // 2
                    o = ps[5][:, h * 128 + c * 64:h * 128 + (c + 1) * 64]
                    mm(o, tokV[:, 512 + h * 128:512 + (h + 1) * 128], STb[:, h * 128 + c * 64:h * 128 + (c + 1) * 64],
                       True, False, [tokV_r, STb_r], [ps_r[5]])
                    mm(o, Sgbd[:, h, :], qkg[:, p, tkc], False, True, [Sgbd_r, qkg_r[p]], [ps_r[5]])
                cp(Wb[cs, :], ps[6][cs, :], [ps_r[6]], [Wb_r])
                for h in range(4):
                    p, s2 = h // 2, h % 2
                    mm(ps[4][s2 * 64:(s2 + 1) * 64, p * 128:(p + 1) * 128], tokKg[cs, h * 64:(h + 1) * 64],
                       tokV[cs, 512 + h * 128:512 + (h + 1) * 128], True, True, [tokKg_r, tokV_r], [ps_r[4]])
                for h in range(8):
                    mm(ps[7][cs, h * 64:(h + 1) * 64], TTall[:, h, cs], Wb[:, h * 64:(h + 1) * 64], True, True,
                       [TTall_r[0], Wb_r], [ps_r[7]])
                tt(tmpS[:, :], ps[4][:, 0:256], Sg[:, :, :].rearrange("p a b -> p (a b)"), ALU.add, [ps_r[4], Sg_r], [tmpS_r])
                cp(Ub[cs, :], ps[7][cs, :], [ps_r[7]], [Ub_r], eng="dve")
                tt(Sg[:, :, :], tmpS[:, :].rearrange("p (a b) -> p a b", a=2),
                   gamCg[:, :, ci:ci + 1].to_broadcast([128, 2, 128]), ALU.mult, [tmpS_r, gamCg_r], [Sg_r])
                Sgv = Sgbd[:, :, :].rearrange("p (a b) v -> p a b v", b=2)
                cp(Sgv[0:64, :, 0, :], Sg[0:64, :, :], [Sg_r], [Sgbd_r], eng="pool")
                cp(Sgv[64:128, :, 1, :], Sg[64:128, :, :], [Sg_r], [Sgbd_r], eng="pool")
                for h in range(8):
                    p, s2 = h // 2, h % 2
                    o = ps[2][s2 * 64:(s2 + 1) * 64, c * 256 + p * 64:c * 256 + (p + 1) * 64]
                    mm(o, Hbd[:, p, s2 * 64:(s2 + 1) * 64], AR[:, p, 1, tkc], True, False, [Hbd_r, AR_r[p]], [ps_r[2]])
                    mm(o, Ub[:, h * 64:(h + 1) * 64], Sball[:, h, 1, cs], False, False, [Ub_r, Sball_r[h]], [ps_r[2]])
                    mm(o, tokV[:, p * 128 + s2 * 64:p * 128 + (s2 + 1) * 64], Sball[:, h, 3, cs], False, True,
                       [tokV_r, Sball_r[h]], [ps_r[2]])
                for h in range(8):
                    p, s2 = h // 2, h % 2
                    o = ps[3][s2 * 64:(s2 + 1) * 64, p * 64:(p + 1) * 64]
                    cb = p * 128 + s2 * 64
                    mm(o, tokBK[cs, cb:cb + 64], Ub[cs, h * 64:(h + 1) * 64], True, False, [tokBK_r, Ub_r], [ps_r[3]])
                    mm(o, tokBK[cs, 512 + cb:512 + cb + 64], tokV[cs, cb:cb + 64], False, True, [tokBK_r, tokV_r], [ps_r[3]])
                Hs2 = Hs[:, :, :].rearrange("p a b -> p (a b)")
                tt(tmpH[:, :], ps[3][:, 0:256], Hs2, ALU.add, [ps_r[3], Hs_r], [tmpH_r])
                tt(Hs[:, :, :], tmpH[:, :].rearrange("p (a b) -> p a b", a=4),
                   gamCr[:, :, ci:ci + 1].to_broadcast([128, 4, 64]), ALU.mult, [tmpH_r, gamCr_r], [Hs_r])
                cp(Hbd[0:64, :, 0:64], Hs[0:64, :, :], [Hs_r], [Hbd_r], eng="pool")
                cp(Hbd[64:128, :, 64:128], Hs[64:128, :, :], [Hs_r], [Hbd_r])
                cp(aT[:, 0:8, :].bitcast(F32).rearrange("p a t -> p (a t)").rearrange("p (a t) -> p a t", a=4)[:, :, tkc],
                   ps[2][:, c * 256:(c + 1) * 256].rearrange("p (a b) -> p a b", a=4), [ps_r[2]], Yraw_r)
            Ov = aT[:, 8:16, :].bitcast(F32).rearrange("p a t -> p (a t)").rearrange("p (a t) -> p a t", a=4)
            cp(Ov[:, :, tk], ps[5][:, :].rearrange("p (a b) -> p a b", a=4), [ps_r[5]], Oraw_r)

        Yv4 = aT[:, 0:8, :].bitcast(F32).rearrange("p a t -> p (a t)").rearrange("p (a t) -> p a t", a=4)
        Ov4 = aT[:, 8:16, :].bitcast(F32).rearrange("p a t -> p (a t)").rearrange("p (a t) -> p a t", a=4)
        for p in range(4):
            y = Yv4[:, p, :]
            cp(sq1[:, :], y, Yraw_r, [sq1_r])
            bkm = nextbank()
            mm(ps[bkm][:, :], blkones, sq1[:, :], True, True, [cm_r, sq1_r], [ps_r[bkm]])
            ts(F[0][:, :], ps[bkm][:, :], 1.0 / 64.0, None, ALU.mult, None, [ps_r[bkm]], [FR[0]])
            act(sq1[:, :], y, AF.Square, Yraw_r, [sq1_r])
            bke = nextbank()
            mm(ps[bke][:, :], blkones, sq1[:, :], True, True, [cm_r, sq1_r], [ps_r[bke]])
            tt(F[1][:, :], F[0][:, :], F[0][:, :], ALU.mult, [FR[0]], [FR[1]])
            stt(F[2][:, :], ps[bke][:, :], 1.0 / 64.0, F[1][:, :], ALU.mult, ALU.subtract, [ps_r[bke], FR[1]], [FR[2]])
            ts(F[2][:, :], F[2][:, :], 0.0, None, ALU.max, None, [FR[2]], [FR[2]])
            act(F[3][:, :], F[2][:, :], AF.Ln, [FR[2], pc_r], [FR[3]], bias=pcol(PC_EPS + 1))
            act(F[1][:, :], F[3][:, :], AF.Exp, [FR[3]], [FR[1]], scale=-0.5)
            tt(F[2][:, :], y, F[0][:, :], ALU.subtract, Yraw_r + [FR[0]], [FR[2]])
            tt(F[2][:, :], F[2][:, :], F[1][:, :], ALU.mult, [FR[2], FR[1]], [FR[2]])
            ts(F[2][:, :], F[2][:, :], pcol(PC_LW + p), pcol(PC_LB + p), ALU.mult, ALU.add, [FR[2], pc_r], [FR[2]])
            tt(F[2][:, :], F[2][:, :], bonus[:, p, :], ALU.add, [FR[2], bonus_r[p]], [FR[2]])
            tt(mixedT[:, 4 + p, :], F[2][:, :], gater[:, p, :], ALU.mult, [FR[2], gater_r[p]], [mixedT_r[4 + p]])
        for h in range(4):
            o = Ov4[:, h, :]
            act(sq1[:, :], o, AF.Square, Oraw_r, [sq1_r])
            bks = nextbank()
            mm(ps[bks][:, :], ones, sq1[:, :], True, True, [cm_r, sq1_r], [ps_r[bks]])
            act(F[3][:, :], ps[bks][:, :], AF.Ln, [ps_r[bks], pc_r], [FR[3]], bias=pcol(PC_EPS), scale=1.0 / 128.0)
            act(F[1][:, :], F[3][:, :], AF.Exp, [FR[3]], [FR[1]], scale=-0.5)
            tt(F[2][:, :], o, F[1][:, :], ALU.mult, Oraw_r + [FR[1]], [FR[2]])
            stt(mixedT[:, h, :], F[2][:, :], pcol(PC_GN), gateg[:, h, :], ALU.mult, ALU.mult,
                [FR[2], pc_r, gateg_r[h]], [mixedT_r[h]])

        for oc in range(8):
            slot, slot_r = stream_A(sout_s[oc], 1024)
            sv = slot[:, 0:1024].rearrange("p (k c) -> p k c", k=8)
            bk = nextbank()
            for fc in range(8):
                mm(ps[bk][:, :], sv[:, fc, :], mixedT[:, fc, :], fc == 0, fc == 7, [slot_r, mixedT_r[fc]], [ps_r[bk]])
            tt(xb[:, oc, :], ps[bk][:, :], xb[:, oc, :], ALU.add, [ps_r[bk], xr[oc]], [xr[oc]])

    xvs = [a.rearrange("(c p) t -> p c t", p=128) for a in xT_ds]
    ovs = [a.rearrange("(c p) t -> p c t", p=128) for a in out_ds]
    out_ops = []
    for n in range(NT):
        b = n % 2
        xb, xr = xT[b], xT_r[b]
        t0 = n * TT
        xv = xvs[t0 // TH]
        ov = ovs[t0 // TH]
        tl = t0 % TH
        P.add("pool", lambda e, xb=xb, tl=tl, xv=xv: e.dma_start(out=xb[:, :, :], in_=xv[:, :, tl:tl + TT]),
              writes=xr, dkey=("x", b))
        if cfg.get("ffn1", True):
            rmsnorm_to_h(xb, xr, PC_N1, 2)
            ffn(0, xb, xr, 2)
        if cfg.get("mix", True):
            mixer(n, xb, xr)
            if cfg.get("dbg", False):
                P.add("pool", lambda e, t0=t0: e.dma_start(out=dbg_d[:, :, t0:t0 + TT], in_=mixedT[:, :, :]),
                      reads=mixedT_r, dkey=("dbg", 0))
        if cfg.get("ffn2", True):
            rmsnorm_to_h(xb, xr, PC_N2, 2)
            ffn(1, xb, xr, 2)
        final_norm(xb, xr)
        op = P.add("pool", lambda e, xb=xb, tl=tl, ov=ov: e.dma_start(out=ov[:, :, tl:tl + TT], in_=xb[:, :, :]),
                   reads=xr, dkey=("o", b))
        out_ops.append(op)
    P.add("pool", lambda e: e.nop(), extra=out_ops[-2:])

    P.emit(nc, es)
    es.close()
    return nc


def _prep_shared(inp):
    f32 = np.float32
    m = {}
    for i, tag in ((1, "ffn1"), (2, "ffn2")):
        g = _kblocks(np.asarray(inp[f"{tag}_w_gate"][0], f32), 128)
        u = _kblocks(np.asarray(inp[f"{tag}_w_up"][0], f32), 128)
        m[f"wgu{i}"] = np.ascontiguousarray(np.stack([g, u], axis=2)).reshape(NFC, 128, 2048)
        m[f"wd{i}"] = _kblocks(np.asarray(inp[f"{tag}_w_down"][0], f32), 128).reshape(8, 128, DFF)
    win = np.asarray(inp["w_in"][0], f32)
    m["wing"] = _kblocks(win[:, 0:1536], 128).reshape(12, 128, 1024)
    m["winga"] = _kblocks(win[:, 1536:1552], 16).reshape(128, 128)
    m["winr"] = _kblocks(win[:, 1552:3344], 128).reshape(14, 128, 1024)
    mu = np.asarray(inp["rwkv_mu"][0], f32).reshape(14, 1, 128)
    m["mur"] = np.ascontiguousarray(np.broadcast_to(mu, (14, 128, 128)))
    m["wout"] = _kblocks(np.asarray(inp["w_out"][0], f32), 128).reshape(8, 128, 1024)
    m["aw2"] = np.ascontiguousarray(np.asarray(inp["gla_alpha_w2"][0], f32))
    m["w2"] = np.ascontiguousarray(np.asarray(inp["rwkv_w2"][0], f32))
    m["a2"] = np.ascontiguousarray(np.asarray(inp["rwkv_a2"][0], f32))
    m["g2"] = np.ascontiguousarray(np.asarray(inp["rwkv_g2"][0], f32))
    pc = np.zeros((128, PC_TOT), f32)
    pc[:, PC_N1:PC_N1 + 8] = _cols(np.asarray(inp["ffn1_norm"][0], f32), 8)
    pc[:, PC_NM:PC_NM + 8] = _cols(np.asarray(inp["mix_norm"][0], f32), 8)
    pc[:, PC_N2:PC_N2 + 8] = _cols(np.asarray(inp["ffn2_norm"][0], f32), 8)
    pc[:, PC_NF:PC_NF + 8] = _cols(np.asarray(inp["final_norm"], f32), 8)
    pc[:, PC_AB:PC_AB + 2] = _cols(np.asarray(inp["gla_alpha_b"][0], f32), 2)
    pc[:, PC_GN] = np.asarray(inp["gla_norm"][0], f32)
    for col, key in ((PC_W0, "rwkv_w0"), (PC_A0, "rwkv_a0"), (PC_KK, "rwkv_k_k"), (PC_KA, "rwkv_k_a"),
                     (PC_LW, "rwkv_ln_w"), (PC_LB, "rwkv_ln_b")):
        pc[:, col:col + 4] = _cols(np.asarray(inp[key][0], f32), 4)
    pc[:, PC_RK:PC_RK + 4] = _cols(np.asarray(inp["rwkv_r_k"][0], f32).reshape(512), 4)
    pc[:, PC_EPS] = EPS
    pc[:, PC_EPS + 1] = GN_EPS
    pc[:, PC_EPS + 2] = 1e-24
    pc[:, PC_EPS + 3] = 1.0
    m["pc"] = pc
    c = _host_consts()
    m["cm"] = c["cm"].reshape(128, 12 * 128)
    m["rst"] = c["rst"]
    return m


_CFG = {"ffn1": True, "mix": True, "ffn2": True}


def kernel(**inputs):
    x = np.asarray(inputs["x"], np.float32)
    B, T, _ = x.shape
    shared = _prep_shared(inputs)
    nc = build_program(T, _CFG)
    in_maps = []
    for b in range(B):
        m = dict(shared)
        xs = 2 if (T // TT) % 2 == 0 else 1
        th = T // xs
        for i in range(xs):
            m[f"xT{i}"] = np.ascontiguousarray(x[b, i * th:(i + 1) * th].T)
        in_maps.append(m)
    res = run_bass_kernel_spmd(nc, in_maps, core_ids=list(range(B)))
    xs = 2 if (T // TT) % 2 == 0 else 1
    out = np.stack([np.concatenate([r[f"outT{i}"].T for i in range(xs)], axis=0) for r in res.results], axis=0)
    return out.astype(np.float32)
```

```python
import numpy as np
from contextlib import ExitStack
import concourse.bass as bass
import concourse.mybir as mybir
from concourse.bass_utils import run_bass_kernel_spmd

F32 = mybir.dt.float32
BF16 = mybir.dt.bfloat16
ALU = mybir.AluOpType
AF = mybir.ActivationFunctionType

D = 1024
DFF = 2816
NFC = DFF // 128
TT = 512
SEQ = 8192
NCORES = 8
EPS = 1e-6
GN_EPS = 64e-5
GLA_W = 1552
RW_W = 1792
PROJ = 3344

ENGS = ("pe", "act", "dve", "pool", "sp")
SEM_CH = 30000


class Res:
    __slots__ = ("name", "last_w", "readers", "excl")

    def __init__(self, name, excl=False):
        self.name = name
        self.last_w = None
        self.readers = []
        self.excl = excl


class Op:
    __slots__ = ("eng", "fn", "deps", "dkey", "needs_sig", "sig", "n")

    def __init__(self, eng, fn, dkey):
        self.eng = eng
        self.fn = fn
        self.deps = []
        self.dkey = dkey
        self.needs_sig = dkey is not None
        self.sig = None
        self.n = None


class Prog:
    def __init__(self):
        self.ops = {e: [] for e in ENGS}
        self.nops = 0

    def add(self, eng, fn, reads=(), writes=(), dkey=None, extra=()):
        op = Op(eng, fn, dkey)
        if any(r.excl for r in reads):
            writes = list(writes) + [r for r in reads if r.excl]
            reads = [r for r in reads if not r.excl]
        deps = {}
        for r in reads:
            w = r.last_w
            if w is not None:
                deps[id(w)] = (w, True)
        for wr in writes:
            w = wr.last_w
            if w is not None and id(w) not in deps:
                deps[id(w)] = (w, False)
            for rd in wr.readers:
                if id(rd) not in deps:
                    deps[id(rd)] = (rd, False)
        for e in extra:
            deps[id(e)] = (e, True)
        for d, raw in deps.values():
            if d is op:
                continue
            if d.eng == eng and d.dkey is None:
                if eng == "pe" or not raw:
                    continue
            op.deps.append(d)
            d.needs_sig = True
        for r in reads:
            if dkey is None:
                r.readers = [x for x in r.readers if not (x.eng == eng and x.dkey is None)]
            r.readers.append(op)
        for wr in writes:
            wr.last_w = op
            wr.readers = []
        self.ops[eng].append(op)
        self.nops += 1
        return op

    def emit(self, nc, es):
        nsig = {e: 0 for e in ENGS}
        dcount = {}
        for e in ENGS:
            for op in self.ops[e]:
                if op.dkey is not None:
                    dcount[op.dkey] = dcount.get(op.dkey, 0) + 1
                    op.sig = ("d", op.dkey, 16 * dcount[op.dkey])
                elif op.needs_sig:
                    op.sig = ("e", e, nsig[e])
                    nsig[e] += 1
        esems = {}
        for e in ENGS:
            nch = (nsig[e] + SEM_CH - 1) // SEM_CH
            esems[e] = [es.enter_context(nc.semaphore(f"s_{e}_{i}")) for i in range(nch)]
        dsems = {k: es.enter_context(nc.semaphore("d_" + "_".join(str(x) for x in k))) for k in dcount}
        self.nsig = nsig
        block = es.enter_context(nc.Block())
        prog = self

        def run(engname, eng):
            known_e = {e: -1 for e in ENGS}
            known_d = {}
            fuse = engname in ("act", "dve")
            for op in prog.ops[engname]:
                waits = []
                for d in op.deps:
                    s = d.sig
                    if s[0] == "e":
                        if known_e[s[1]] >= s[2]:
                            continue
                        known_e[s[1]] = s[2]
                        waits.append((esems[s[1]][s[2] // SEM_CH], s[2] % SEM_CH + 1))
                    else:
                        if known_d.get(s[1], 0) >= s[2]:
                            continue
                        known_d[s[1]] = s[2]
                        waits.append((dsems[s[1]], s[2]))
                last = waits.pop() if (fuse and waits and op.dkey is None) else None
                for sm, v in waits:
                    eng.wait_ge(sm, v)
                ins = op.fn(eng)
                if last is not None:
                    ins._wait_ge(last[0], last[1])
                s = op.sig
                if s is not None:
                    if s[0] == "e":
                        ins.then_inc(esems[s[1]][s[2] // SEM_CH], 1)
                    else:
                        ins.then_inc(dsems[s[1]], 16)

        @block.tensor
        def _(e):
            run("pe", e)

        @block.scalar
        def _(e):
            run("act", e)

        @block.vector
        def _(e):
            run("dve", e)

        @block.gpsimd
        def _(e):
            run("pool", e)

        @block.sync
        def _(e):
            run("sp", e)


def _kblocks(w, cb):
    K, C = w.shape
    return np.ascontiguousarray(w.reshape(K // 128, 128, C // cb, cb).transpose(2, 1, 0, 3))


def _cols(v, n):
    return np.ascontiguousarray(v.reshape(n, 128).T)


def _host_consts():
    i = np.arange(128)
    same = (i[:, None] // 64) == (i[None, :] // 64)
    strictT = (same & (i[:, None] < i[None, :])).astype(np.float32)
    inclT = (same & (i[:, None] <= i[None, :])).astype(np.float32)
    strict = (same & (i[:, None] > i[None, :])).astype(np.float32)
    ident = np.eye(128, dtype=np.float32)
    blk = same.astype(np.float32)
    ones = np.ones((128, 128), np.float32)
    c = {}
    c["cm"] = np.ascontiguousarray(np.stack([strictT, inclT, strictT, inclT, strict, ident, blk, ones,
                                             inclT, inclT, inclT, inclT], axis=1))
    t = np.arange(TT)
    c["rst"] = np.ascontiguousarray(np.broadcast_to((t % 64 != 0).astype(np.float32)[None, :], (128, TT)))
    return c


PC_N1, PC_NM, PC_N2, PC_NF = 0, 8, 16, 24
PC_AB, PC_GN = 32, 34
PC_W0, PC_A0, PC_KK, PC_KA, PC_RK, PC_LW, PC_LB = 35, 39, 43, 47, 51, 55, 59
PC_NAB = 63
PC_EPS = 65
PC_TOT = 72


def build_program(T, cfg):
    NT = T // TT
    nc = bass.Bass("TRN2", target_bir_lowering=False)
    P = Prog()
    es = ExitStack()

    def din(name, shape, dt=F32):
        return nc.dram_tensor(name, list(shape), dt, kind="ExternalInput").ap()

    def dscr(name, shape, dt=BF16):
        return nc.dram_tensor(name, list(shape), dt, kind="Internal").ap()

    def sb(name, shape, dt):
        return es.enter_context(nc.sbuf_tensor("sb_" + name, list(shape), dt))

    XS = 2 if NT % 2 == 0 else 1
    TH = T // XS
    xT_ds = [din(f"xT{i}", [D, TH]) for i in range(XS)]
    out_ds = [nc.dram_tensor(f"outT{i}", [D, TH], F32, kind="ExternalOutput").ap() for i in range(XS)]
    wgu_d = [din(f"wgu{i}", [NFC, 128, 2048]) for i in (1, 2)]
    wd_d = [din(f"wd{i}", [8, 128, DFF]) for i in (1, 2)]
    wing_d = din("wing", [12, 128, 1024])
    winga_d = din("winga", [128, 128])
    winr_d = din("winr", [14, 128, 1024])
    mur_d = din("mur", [14, 128, 128])
    wout_d = din("wout", [8, 128, 1024])
    aw2_d = din("aw2", [16, 256])
    w2_d = din("w2", [64, 512])
    a2_d = din("a2", [64, 512])
    g2_d = din("g2", [128, 512])
    pc_d = din("pc", [128, PC_TOT])
    cm_d = din("cm", [128, 12 * 128])
    rst_d = din("rst", [128, TT])

    if cfg.get("dbg", False):
        dbg_d = nc.dram_tensor("dbg", [128, 8, T], BF16, kind="ExternalOutput").ap()
    sgu_s = [dscr(f"sgu{i}", [NFC, 128, 2048]) for i in (1, 2)]
    sd_s = [dscr(f"sd{i}", [8, 128, DFF]) for i in (1, 2)]
    sing_s = dscr("sing", [12, 128, 1024])
    singa_s = dscr("singa", [128, 128])
    sinr_s = dscr("sinr", [14, 128, 2048])
    sout_s = dscr("sout", [8, 128, 1024])

    xT = [sb(f"xT{i}", [128, 8, TT], F32) for i in range(2)]
    xT_r = [[Res(f"xT{i}_{c}") for c in range(8)] for i in range(2)]
    hT = sb("hT", [128, 8, TT + 2], BF16)
    hT_r = Res("hT")
    aT = sb("aT", [128, NFC, TT], BF16)
    aT_r = [Res(f"aT{j}") for j in range(NFC)]
    NRA, NRB = 4, 2
    ringA = [sb(f"ringA{i}", [128, 2048], BF16) for i in range(NRA)]
    ringA_r = [Res(f"ringA{i}") for i in range(NRA)]
    ringB = [sb(f"ringB{i}", [128, DFF], BF16) for i in range(NRB)]
    ringB_r = [Res(f"ringB{i}") for i in range(NRB)]
    pc = sb("pc", [128, PC_TOT], F32)
    pc_r = Res("pc")
    cmb = sb("cmb", [128, 12, 128], BF16)
    cm_r = Res("cm")
    rst = sb("rst", [128, TT], F32)
    NFT = 14
    ftmp = [sb(f"ftmp{i}", [128, TT], F32) for i in range(NFT)]
    ftmp_r = [Res(f"ftmp{i}") for i in range(NFT)]
    sqT = aT[:, 0:8, :]

    ps = [es.enter_context(nc.psum_tensor(f"ps{i}", [128, 512], F32)) for i in range(8)]
    ps_r = [Res(f"ps{i}", excl=True) for i in range(8)]

    ident = cmb[:, 5, :]
    blkones = cmb[:, 6, :]
    ones = cmb[:, 7, :]

    pro_stores = []

    last_by_key = {}
    scr_ops = {}

    def id_of(ap_):
        return (ap_.tensor.name, ap_.offset)

    def dram_cast(dst, src, key):
        prev = last_by_key.get(key)
        op = P.add("pool", lambda e, d=dst, s=src: e.dma_start(out=d, in_=s), dkey=key,
                   extra=(prev,) if prev is not None else ())
        last_by_key[key] = op
        pro_stores.append(op)
        scr_ops.setdefault(id_of(dst), []).append(op)

    kctr = [0]

    def nkey(base):
        kctr[0] += 1
        return (base, kctr[0] % 4)

    def cast_ffn(f):
        for j in range(NFC):
            dram_cast(sgu_s[f][j], wgu_d[f][j], nkey("pc"))
        for o in range(8):
            dram_cast(sd_s[f][o], wd_d[f][o], nkey("pc"))

    cast_ffn(0)
    dram_cast(singa_s, winga_d, nkey("pc"))
    for b in range(12):
        dram_cast(sing_s[b], wing_d[b], nkey("pc"))

    P.add("sp", lambda e: e.dma_start(out=pc[:, :], in_=pc_d), writes=[pc_r], dkey=("c", 0))
    P.add("sp", lambda e: e.dma_start(out=rst[:, :], in_=rst_d), writes=[cm_r], dkey=("c", 1))
    for h in range(3):
        P.add("sp", lambda e, h=h: e.dma_start(out=ftmp[h][:, :], in_=cm_d[:, h * 512:(h + 1) * 512]),
              writes=[ftmp_r[h]], dkey=("c", 2 + h))
        P.add("dve", lambda e, h=h: e.tensor_copy(cmb[:, 4 * h:4 * h + 4, :],
                                                  ftmp[h][:, :].rearrange("p (a b) -> p a b", a=4)),
              reads=[ftmp_r[h]], writes=[cm_r])
    P.add("dve", lambda e: e.tensor_scalar(pc[:, PC_NAB:PC_NAB + 2], pc[:, PC_AB:PC_AB + 2], -1.0, None, ALU.mult),
          reads=[pc_r], writes=[pc_r])

    ra = [0]
    rb = [0]
    first_stream = [True]

    def stream_A(src, ncols=2048):
        k = ra[0] % NRA
        ra[0] += 1
        extra = scr_ops.get(id_of(src), pro_stores)
        P.add("sp", lambda e, k=k, s=src, n=ncols: e.dma_start(out=ringA[k][:, 0:n], in_=s),
              writes=[ringA_r[k]], dkey=("ra", k), extra=extra)
        return ringA[k], ringA_r[k]

    def stream_B(src):
        k = rb[0] % NRB
        rb[0] += 1
        P.add("sp", lambda e, k=k, s=src: e.dma_start(out=ringB[k][:, :], in_=s),
              writes=[ringB_r[k]], dkey=("rb", k), extra=scr_ops.get(id_of(src), pro_stores))
        return ringB[k], ringB_r[k]

    def rmsnorm_to_h(xb, xr, gcol, hoff):
        P.add("act", lambda e: e.activation(sqT, xb[:, :, :], AF.Square), reads=xr, writes=aT_r[0:8])
        for c in range(8):
            P.add("pe", lambda e, c=c: e.matmul(ps[6][:, :], ones, sqT[:, c, :], start=(c == 0), stop=(c == 7)),
                  reads=aT_r[0:8] + [cm_r], writes=[ps_r[6]])
        P.add("act", lambda e: e.activation(ftmp[2][:, :], ps[6][:, :], AF.Ln, bias=pc[:, PC_EPS:PC_EPS + 1],
                                            scale=1.0 / D), reads=[ps_r[6], pc_r], writes=[ftmp_r[2]])
        P.add("act", lambda e: e.activation(ftmp[3][:, :], ftmp[2][:, :], AF.Exp, scale=-0.5),
              reads=[ftmp_r[2]], writes=[ftmp_r[3]])
        for c in range(8):
            P.add("dve", lambda e, c=c: e.scalar_tensor_tensor(
                hT[:, c, hoff:hoff + TT], xb[:, c, :], pc[:, gcol + c:gcol + c + 1], ftmp[3][:, :],
                ALU.mult, ALU.mult), reads=[xr[c], ftmp_r[3], pc_r], writes=[hT_r])

    def ffn(f, xb, xr, hoff):
        for j in range(NFC):
            slot, slot_r = stream_A(sgu_s[f][j])
            sv = slot[:, :].rearrange("p (g k c) -> p g k c", g=2, k=8)
            pg, pu = 0 + (j % 2), 2 + (j % 2)
            for kc in range(8):
                P.add("pe", lambda e, kc=kc, sv=sv, pg=pg: e.matmul(
                    ps[pg][:, :], sv[:, 0, kc, :], hT[:, kc, hoff:hoff + TT], start=(kc == 0), stop=(kc == 7)),
                    reads=[slot_r, hT_r], writes=[ps_r[pg]])
            for kc in range(8):
                P.add("pe", lambda e, kc=kc, sv=sv, pu=pu: e.matmul(
                    ps[pu][:, :], sv[:, 1, kc, :], hT[:, kc, hoff:hoff + TT], start=(kc == 0), stop=(kc == 7)),
                    reads=[slot_r, hT_r], writes=[ps_r[pu]])
            ft = j % 2
            P.add("act", lambda e, ft=ft, pg=pg: e.activation(ftmp[ft][:, :], ps[pg][:, :], AF.Silu),
                  reads=[ps_r[pg]], writes=[ftmp_r[ft]])
            P.add("dve", lambda e, ft=ft, pu=pu, j=j: e.tensor_tensor(aT[:, j, :], ftmp[ft][:, :], ps[pu][:, :], ALU.mult),
                  reads=[ftmp_r[ft], ps_r[pu]], writes=[aT_r[j]])
        for o in range(8):
            slot, slot_r = stream_B(sd_s[f][o])
            sv = slot[:, :].rearrange("p (j c) -> p j c", j=NFC)
            pd = 4 + (o % 2)
            for j in range(NFC):
                P.add("pe", lambda e, j=j, sv=sv, pd=pd: e.matmul(
                    ps[pd][:, :], sv[:, j, :], aT[:, j, :], start=(j == 0), stop=(j == NFC - 1)),
                    reads=[slot_r, aT_r[j]], writes=[ps_r[pd]])
            P.add("dve", lambda e, o=o, pd=pd: e.scalar_tensor_tensor(
                xb[:, o, :], ps[pd][:, :], 0.5, xb[:, o, :], ALU.mult, ALU.add),
                reads=[ps_r[pd], xr[o]], writes=[xr[o]])

    def final_norm(xb, xr):
        P.add("act", lambda e: e.activation(sqT, xb[:, :, :], AF.Square), reads=xr, writes=aT_r[0:8])
        for c in range(8):
            P.add("pe", lambda e, c=c: e.matmul(ps[6][:, :], ones, sqT[:, c, :], start=(c == 0), stop=(c == 7)),
                  reads=aT_r[0:8] + [cm_r], writes=[ps_r[6]])
        P.add("act", lambda e: e.activation(ftmp[2][:, :], ps[6][:, :], AF.Ln, bias=pc[:, PC_EPS:PC_EPS + 1],
                                            scale=1.0 / D), reads=[ps_r[6], pc_r], writes=[ftmp_r[2]])
        P.add("act", lambda e: e.activation(ftmp[3][:, :], ftmp[2][:, :], AF.Exp, scale=-0.5),
              reads=[ftmp_r[2]], writes=[ftmp_r[3]])
        for c in range(8):
            P.add("dve", lambda e, c=c: e.scalar_tensor_tensor(
                xb[:, c, :], xb[:, c, :], pc[:, PC_NF + c:PC_NF + c + 1], ftmp[3][:, :],
                ALU.mult, ALU.mult), reads=[xr[c], ftmp_r[3], pc_r], writes=[xr[c]])

    CDEC = 0.6065306597126334

    def RL(name, n):
        return [Res(f"{name}{i}") for i in range(n)]

    hprev = sb("hprev", [128, 8, 1], BF16); hprev_r = Res("hprev")
    aw2b = sb("aw2b", [16, 256], BF16)
    w2a2b = sb("w2a2b", [128, 512], BF16)
    g2b = sb("g2b", [128, 512], BF16)
    smallw_r = Res("smallw")
    qkg = sb("qkg", [128, 4, TT], BF16); qkg_r = RL("qkg", 4)
    vTg = sb("vTg", [128, 4, TT], BF16); vTg_r = RL("vTg", 4)
    gateg = sb("gateg", [128, 4, TT], BF16); gateg_r = RL("gateg", 4)
    gamCg = sb("gamCg", [128, 2, 8], F32); gamCg_r = Res("gamCg")
    alr = sb("alr", [16, TT], BF16); alr_r = Res("alr")
    AR = sb("AR", [128, 4, 2, TT], BF16); AR_r = RL("AR", 4)
    Bt = sb("Bt", [128, 4, TT], BF16); Bt_r = RL("Bt", 4)
    Kt = sb("Kt", [128, 4, TT], BF16); Kt_r = RL("Kt", 4)
    vTr = sb("vTr", [128, 4, TT], BF16); vTr_r = RL("vTr", 4)
    bonus = sb("bonus", [128, 4, TT], BF16); bonus_r = RL("bonus", 4)
    gater = sb("gater", [128, 4, TT], BF16); gater_r = RL("gater", 4)
    gamCr = sb("gamCr", [128, 4, 8], F32); gamCr_r = Res("gamCr")
    wa = sb("wa", [128, TT], BF16); wa_r = Res("wa")
    sgl = sb("sgl", [128, TT], BF16); sgl_r = Res("sgl")
    sq1 = sb("sq1", [128, TT], BF16); sq1_r = Res("sq1")
    Hs = sb("Hs", [128, 4, 64], F32); Hs_r = Res("Hs")
    Hbd = sb("Hbd", [128, 4, 128], BF16); Hbd_r = Res("Hbd")
    Sg = sb("Sg", [128, 2, 128], F32); Sg_r = Res("Sg")
    Sgbd = sb("Sgbd", [128, 4, 128], BF16); Sgbd_r = Res("Sgbd")
    Wb = sb("Wb", [128, 512], BF16); Wb_r = Res("Wb")
    Ub = sb("Ub", [128, 512], BF16); Ub_r = Res("Ub")
    tokBK = sb("tokBK", [128, 1024], BF16); tokBK_r = Res("tokBK")
    tokV = sb("tokV", [128, 1024], BF16); tokV_r = Res("tokV")
    tokKg = sb("tokKg", [128, 256], BF16); tokKg_r = Res("tokKg")
    Sball = sb("Sball", [128, 8, 4, 128], BF16); Sball_r = RL("Sball", 8)
    TTall = sb("TTall", [128, 8, 128], BF16); TTall_r = [Res("TTall")]
    Pb = [[sb(f"Pb{a}{b}", [128, 512], BF16) for b in range(2)] for a in range(4)]
    Pb_r = [[Res(f"Pb{a}{b}") for b in range(2)] for a in range(4)]
    STb = sb("STb", [128, 512], BF16); STb_r = Res("STb")
    tmpH = ftmp[0][:, 0:256]; tmpH_r = ftmp_r[0]
    tmpS = ftmp[1][:, 0:256]; tmpS_r = ftmp_r[1]
    mixedT = sb("mixedT", [128, 8, TT], BF16); mixedT_r = RL("mixedT", 8)
    Yraw = aT[:, 0:8, :].bitcast(F32)
    Oraw = aT[:, 8:16, :].bitcast(F32)
    Yraw_r = aT_r[0:8]
    Oraw_r = aT_r[8:16]

    def mm(out, lhsT, rhs, start, stop, reads, writes):
        return P.add("pe", lambda e: e.matmul(out, lhsT, rhs, start=start, stop=stop), reads, writes)

    def tr(out, in_, reads, writes):
        return P.add("pe", lambda e: e.transpose(out, in_, ident), list(reads) + [cm_r], writes)

    def act(out, in_, func, reads, writes, bias=None, scale=None):
        kw = {}
        if bias is not None:
            kw["bias"] = bias
        if scale is not None:
            kw["scale"] = scale
        return P.add("act", lambda e: e.activation(out, in_, func, **kw), reads, writes)

    def tt(out, in0, in1, op, reads, writes, eng="dve"):
        return P.add(eng, lambda e: e.tensor_tensor(out, in0, in1, op), reads, writes)

    def ts(out, in0, s1, s2, op0, op1, reads, writes, eng="dve"):
        if s2 is None:
            return P.add(eng, lambda e: e.tensor_scalar(out, in0, s1, None, op0), reads, writes)
        return P.add(eng, lambda e: e.tensor_scalar(out, in0, s1, s2, op0, op1), reads, writes)

    def stt(out, in0, sc, in1, op0, op1, reads, writes):
        return P.add("dve", lambda e: e.scalar_tensor_tensor(out, in0, sc, in1, op0, op1), reads, writes)

    def cp(out, in_, reads, writes, eng="act"):
        if eng == "act":
            return P.add("act", lambda e: e.activation(out, in_, AF.Copy), reads, writes)
        return P.add(eng, lambda e: e.tensor_copy(out, in_), reads, writes)

    def scan(out, d0, d1, reads, writes):
        return P.add("dve", lambda e: e.tensor_tensor_scan(out, d0, d1, 0.0, ALU.mult, ALU.add), reads, writes)

    def pcol(c):
        return pc[:, c:c + 1]

    def load_small(dst, src_d, rows, cols, ft, prow=0):
        P.add("sp", lambda e: e.dma_start(out=ftmp[ft][prow:prow + rows, 0:cols], in_=src_d),
              writes=[ftmp_r[ft]], dkey=("c", 5 + ft))
        cp(dst, ftmp[ft][prow:prow + rows, 0:cols], [ftmp_r[ft]], [smallw_r], eng="dve")

    load_small(aw2b[0:16, :], aw2_d, 16, 256, 3)
    load_small(w2a2b[0:64, :], w2_d, 64, 512, 4)
    load_small(w2a2b[64:128, :], a2_d, 64, 512, 5, prow=64)
    load_small(g2b[:, :], g2_d, 128, 512, 6)
    for tl, tr_ in ((Hs, Hs_r), (Hbd, Hbd_r), (Sg, Sg_r), (Sgbd, Sgbd_r), (Wb, Wb_r), (Ub, Ub_r), (hprev, hprev_r)):
        ap_ = tl[:, :, :] if len(tl.shape) == 3 else tl[:, :]
        P.add("pool", lambda e, a=ap_: e.memset(a, 0.0), writes=[tr_])
    stg_r = [Res("stgA"), Res("stgB")]
    for blk in range(14):
        s2 = blk % 2
        fW, fM, fO = ftmp[7 + 3 * s2], ftmp[8 + 3 * s2], ftmp[9 + 3 * s2]
        rW, rM, rO = ftmp_r[7 + 3 * s2], ftmp_r[8 + 3 * s2], ftmp_r[9 + 3 * s2]
        for half in range(2):
            P.add("sp", lambda e, blk=blk, half=half, fW=fW: e.dma_start(
                out=fW[:, :], in_=winr_d[blk][:, half * 512:(half + 1) * 512]), writes=[rW], dkey=("pw", s2))
            if half == 0:
                P.add("sp", lambda e, blk=blk, fM=fM: e.dma_start(out=fM[:, 0:128], in_=mur_d[blk]),
                      writes=[rM], dkey=("pm", s2))
                ts(fM[:, 128:256], fM[:, 0:128], -1.0, 1.0, ALU.mult, ALU.add, [rM], [rM])
            fWv = fW[:, :].rearrange("p (k c) -> p k c", k=4)
            fOv = fO[:, :].bitcast(BF16).rearrange("p (k c) -> p k c", k=8)
            tt(fOv[:, 0:4, :], fWv, fM[:, 128:256].unsqueeze(1).to_broadcast([128, 4, 128]), ALU.mult, [rW, rM], [rO])
            tt(fOv[:, 4:8, :], fWv, fM[:, 0:128].unsqueeze(1).to_broadcast([128, 4, 128]), ALU.mult, [rW, rM], [rO])
            dv = sinr_s[blk].rearrange("p (k c) -> p k c", k=16)
            op1 = P.add("pool", lambda e, dv=dv, fOv=fOv, half=half: e.dma_start(
                out=dv[:, half * 4:half * 4 + 4, :], in_=fOv[:, 0:4, :]), reads=[rO], dkey=("ps1", s2))
            op2 = P.add("pool", lambda e, dv=dv, fOv=fOv, half=half: e.dma_start(
                out=dv[:, 8 + half * 4:8 + half * 4 + 4, :], in_=fOv[:, 4:8, :]), reads=[rO], dkey=("ps2", s2))
            pro_stores.append(op1)
            pro_stores.append(op2)
            scr_ops.setdefault(id_of(sinr_s[blk]), []).extend([op1, op2])
    for o in range(8):
        dram_cast(sout_s[o], wout_d[o], nkey("pc"))
    cast_ffn(1)

    bank_rr = [0]

    def nextbank():
        b_ = bank_rr[0] % 8
        bank_rr[0] += 1
        return b_

    def proj_block(src, ncols, K16, M=128):
        slot, slot_r = stream_A(src, ncols)
        nk = 16 if K16 else 8
        sv = slot[:, 0:ncols].rearrange("p (k c) -> p k c", k=nk)
        bk = nextbank()
        for kc in range(nk):
            rhs = hT[:, kc, 2:TT + 2] if kc < 8 else hT[:, kc - 8, 1:TT + 1]
            mm(ps[bk][0:M, :], sv[:, kc, :], rhs, kc == 0, kc == nk - 1, [slot_r, hT_r], [ps_r[bk]])
        return bk

    def mixer(n, xb, xr):
        rmsnorm_to_h(xb, xr, PC_NM, 2)
        cp(hT[:, :, 1:2], hprev[:, :, :], [hprev_r], [hT_r], eng="pool")
        cp(hprev[:, :, :], hT[:, :, TT + 1:TT + 2], [hT_r], [hprev_r], eng="pool")

        stage = float(cfg.get("stage", 99))
        if stage < 1:
            return
        bk = proj_block(singa_s, 128, False, M=16)
        cp(alr[0:16, :], ps[bk][0:16, :], [ps_r[bk]], [alr_r])
        if stage < 0.5:
            return
        for c in range(2):
            bk = nextbank()
            mm(ps[bk][:, :], aw2b[0:16, c * 128:(c + 1) * 128], alr[0:16, :], True, True, [smallw_r, alr_r], [ps_r[bk]])
            act(ftmp[8][:, :], ps[bk][:, :], AF.Exp, [ps_r[bk], pc_r], [ftmp_r[8]], bias=pcol(PC_NAB + c), scale=-1.0)
            act(ftmp[9][:, :], ftmp[8][:, :], AF.Ln, [ftmp_r[8], pc_r], [ftmp_r[9]], bias=pcol(PC_EPS + 3))
            scan(ftmp[10][:, :], rst[:, :], ftmp[9][:, :], [ftmp_r[9], cm_r], [ftmp_r[10]])
            act(ftmp[4 + c][:, :], ftmp[10][:, :], AF.Exp, [ftmp_r[10]], [ftmp_r[4 + c]], scale=-1.0 / 16.0)
            act(ftmp[6 + c][:, :], ftmp[10][:, :], AF.Exp, [ftmp_r[10]], [ftmp_r[6 + c]], scale=1.0 / 16.0)
            cp(gamCg[:, c, :], ftmp[4 + c][:, :].rearrange("p (a b) -> p a b", b=64)[:, :, 63],
               [ftmp_r[4 + c]], [gamCg_r], eng="dve")
        if stage < 0.7:
            return
        for c in range(2):
            bk = proj_block(sing_s[c], 1024, False)
            stt(qkg[:, c, :], ps[bk][:, :], 0.125, ftmp[4 + c][:, :], ALU.mult, ALU.mult,
                [ps_r[bk], ftmp_r[4 + c]], [qkg_r[c]])
        for c in range(2):
            bk = proj_block(sing_s[2 + c], 1024, False)
            tt(qkg[:, 2 + c, :], ps[bk][:, :], ftmp[6 + c][:, :], ALU.mult, [ps_r[bk], ftmp_r[6 + c]], [qkg_r[2 + c]])
        for h in range(4):
            bk = proj_block(sing_s[4 + h], 1024, False)
            cp(vTg[:, h, :], ps[bk][:, :], [ps_r[bk]], [vTg_r[h]])
        for h in range(4):
            bk = proj_block(sing_s[8 + h], 1024, False)
            act(gateg[:, h, :], ps[bk][:, :], AF.Silu, [ps_r[bk]], [gateg_r[h]])

        if stage < 2:
            return
        bk = proj_block(sinr_s[12], 2048, True)
        act(wa[0:64, :], ps[bk][0:64, :], AF.Tanh, [ps_r[bk]], [wa_r])
        cp(wa[64:128, :], ps[bk][64:128, :], [ps_r[bk]], [wa_r])
        bk = proj_block(sinr_s[13], 2048, True)
        act(sgl[:, :], ps[bk][:, :], AF.Sigmoid, [ps_r[bk]], [sgl_r])
        for p in range(4):
            bk = nextbank()
            mm(ps[bk][:, :], g2b[:, p * 128:(p + 1) * 128], sgl[:, :], True, True, [smallw_r, sgl_r], [ps_r[bk]])
            cp(gater[:, p, :], ps[bk][:, :], [ps_r[bk]], [gater_r[p]])
        F = ftmp
        FR = ftmp_r
        if stage < 1.5:
            return
        for p in range(4):
            bk = nextbank()
            mm(ps[bk][:, :], w2a2b[0:64, p * 128:(p + 1) * 128], wa[0:64, :], True, True, [smallw_r, wa_r], [ps_r[bk]])
            act(F[8][:, :], ps[bk][:, :], AF.Sigmoid, [ps_r[bk], pc_r], [FR[8]], bias=pcol(PC_W0 + p))
            if stage < 1.6:
                continue
            bk = nextbank()
            mm(ps[bk][:, :], w2a2b[64:128, p * 128:(p + 1) * 128], wa[64:128, :], True, True, [smallw_r, wa_r], [ps_r[bk]])
            act(F[9][:, :], ps[bk][:, :], AF.Sigmoid, [ps_r[bk], pc_r], [FR[9]], bias=pcol(PC_A0 + p))
            if stage < 1.7:
                continue
            scan(F[10][:, :], rst[:, :], F[8][:, :], [FR[8], cm_r], [FR[10]])
            act(F[0][:, :], F[10][:, :], AF.Exp, [FR[10]], [FR[0]], scale=-CDEC)
            act(F[1][:, :], F[10][:, :], AF.Exp, [FR[10]], [FR[1]], scale=CDEC)
            tt(F[11][:, :], F[10][:, :], F[8][:, :], ALU.subtract, [FR[10], FR[8]], [FR[11]])
            act(F[2][:, :], F[11][:, :], AF.Exp, [FR[11]], [FR[2]], scale=-CDEC)
            cp(gamCr[:, p, :], F[0][:, :].rearrange("p (a b) -> p a b", b=64)[:, :, 63], [FR[0]], [gamCr_r], eng="dve")
            if stage < 1.8:
                continue
            bkK = proj_block(sinr_s[4 + p], 2048, True)
            ts(F[11][:, :], ps[bkK][:, :], pcol(PC_KK + p), None, ALU.mult, None, [ps_r[bkK], pc_r], [FR[11]])
            act(sq1[:, :], F[11][:, :], AF.Square, [FR[11]], [sq1_r])
            bk = nextbank()
            mm(ps[bk][:, :], blkones, sq1[:, :], True, True, [cm_r, sq1_r], [ps_r[bk]])
            act(F[3][:, :], ps[bk][:, :], AF.Ln, [ps_r[bk], pc_r], [FR[3]], bias=pcol(PC_EPS + 2))
            act(F[12][:, :], F[3][:, :], AF.Exp, [FR[3]], [FR[12]], scale=-0.5)
            tt(F[11][:, :], F[11][:, :], F[12][:, :], ALU.mult, [FR[11], FR[12]], [FR[11]])
            if stage < 1.9:
                continue
            tt(F[3][:, :], F[11][:, :], F[9][:, :], ALU.mult, [FR[11], FR[9]], [FR[3]])
            tt(Bt[:, p, :], F[3][:, :], F[1][:, :], ALU.mult, [FR[3], FR[1]], [Bt_r[p]])
            stt(AR[:, p, 0, :], F[11][:, :], -1.0, F[2][:, :], ALU.mult, ALU.mult, [FR[11], FR[2]], [AR_r[p]])
            ts(F[3][:, :], F[9][:, :], -1.0, pcol(PC_KA + p), ALU.add, ALU.mult, [FR[9], pc_r], [FR[3]])
            stt(F[13][:, :], F[3][:, :], 1.0, ps[bkK][:, :], ALU.add, ALU.mult, [FR[3], ps_r[bkK]], [FR[13]])
            tt(Kt[:, p, :], F[13][:, :], F[1][:, :], ALU.mult, [FR[13], FR[1]], [Kt_r[p]])
            if stage < 1.95:
                continue
            bkR = proj_block(sinr_s[p], 2048, True)
            tt(AR[:, p, 1, :], ps[bkR][:, :], F[0][:, :], ALU.mult, [ps_r[bkR], FR[0]], [AR_r[p]])
            if stage < 1.98:
                continue
            stt(sq1[:, :], ps[bkR][:, :], pcol(PC_RK + p), F[13][:, :], ALU.mult, ALU.mult,
                [ps_r[bkR], pc_r, FR[13]], [sq1_r])
            if stage < 1.985:
                continue
            bk = nextbank()
            mm(ps[bk][:, :], blkones, sq1[:, :], True, True, [cm_r, sq1_r], [ps_r[bk]])
            cp(F[12][:, :], ps[bk][:, :], [ps_r[bk]], [FR[12]])
            if stage < 1.99:
                continue
            bkV = proj_block(sinr_s[8 + p], 2048, True)
            cp(vTr[:, p, :], ps[bkV][:, :], [ps_r[bkV]], [vTr_r[p]])
            if stage < 1.995:
                continue
            tt(bonus[:, p, :], ps[bkV][:, :], F[12][:, :], ALU.mult, [ps_r[bkV], FR[12]], [bonus_r[p]])

        if stage < 3:
            return
        ps0b = ps[0][:, :].bitcast(BF16)
        ps1b = ps[1][:, :].bitcast(BF16)
        Yv = Yraw.rearrange("p a (b t) -> p (a b) t", b=2) if False else None
        for s in range(TT // 128):
            tk = slice(s * 128, (s + 1) * 128)
            for p in range(4):
                tr(ps0b[:, p * 128:(p + 1) * 128], Bt[:, p, tk], [Bt_r[p]], [ps_r[0]])
            for p in range(4):
                tr(ps0b[:, 512 + p * 128:512 + (p + 1) * 128], Kt[:, p, tk], [Kt_r[p]], [ps_r[0]])
            cp(tokBK[:, :], ps0b, [ps_r[0]], [tokBK_r], eng="dve")
            for p in range(4):
                tr(ps1b[:, p * 128:(p + 1) * 128], vTr[:, p, tk], [vTr_r[p]], [ps_r[1]])
            for h in range(4):
                tr(ps1b[:, 512 + h * 128:512 + (h + 1) * 128], vTg[:, h, tk], [vTg_r[h]], [ps_r[1]])
            cp(tokV[:, :], ps1b, [ps_r[1]], [tokV_r])
            for c in range(2):
                tr(ps0b[:, c * 128:(c + 1) * 128], qkg[:, 2 + c, tk], [qkg_r[2 + c]], [ps_r[0]])
            cp(tokKg[:, :], ps0b[:, 0:256], [ps_r[0]], [tokKg_r], eng="dve")

            TTv = TTall[:, :, :].rearrange("q (p s) c -> q s p c", s=2)
            order = [(p_, 0) for p_ in range(4)] + [(p_, 1) for p_ in range(4)]
            for idx, (p, s2) in enumerate(order):
                h = 2 * p + s2
                hp = slice(s2 * 64, (s2 + 1) * 64)
                SB = (0, 1, 6, 7)[idx % 4]
                bi = s2 * 2 + p // 2
                PB = 2 + bi
                c0 = (p % 2) * 256
                arv = AR[hp, p, :, tk]
                mm(ps[SB][:, 0:256].rearrange("p (a b) -> p a b", a=2), Bt[hp, p, tk], arv, True, True,
                   [Bt_r[p], AR_r[p]], [ps_r[SB]])
                mm(ps[SB][:, 256:512].rearrange("p (a b) -> p a b", a=2), Kt[hp, p, tk], arv, True, True,
                   [Kt_r[p], AR_r[p]], [ps_r[SB]])
                mm(ps[PB][:, c0:c0 + 128], AR[hp, p, 0, tk], Bt[hp, p, tk], True, True, [Bt_r[p], AR_r[p]], [ps_r[PB]])
                tt(Sball[:, h, :, :], ps[SB][:, :].rearrange("p (a b) -> p a b", a=4), cmb[:, 0:4, :], ALU.mult,
                   [ps_r[SB], cm_r], [Sball_r[h]])
                tt(Pb[bi][0][:, c0:c0 + 128], ps[PB][:, c0:c0 + 128], cmb[:, 4, :], ALU.mult, [ps_r[PB], cm_r], [Pb_r[bi][0]])
                cp(Pb[bi][0][:, c0 + 128:c0 + 256], Sball[:, h, 0, :], [Sball_r[h]], [Pb_r[bi][0]], eng="pool")
                tt(TTall[:, h, :], Sball[:, h, 0, :], ident, ALU.add, [Sball_r[h], cm_r], TTall_r, eng="pool")
            for lv in range(5):
                for bi in range(4):
                    cur, cur_r = Pb[bi][lv % 2], Pb_r[bi][lv % 2]
                    for c0 in (0, 256):
                        mm(ps[2 + bi][:, c0:c0 + 128], cur[:, c0 + 128:c0 + 256], cur[:, c0:c0 + 128], True, True,
                           [cur_r], [ps_r[2 + bi]])
                        if lv < 4:
                            mm(ps[2 + bi][:, c0 + 128:c0 + 256], cur[:, c0:c0 + 128], cur[:, c0 + 128:c0 + 256], True, True,
                               [cur_r], [ps_r[2 + bi]])
                for bi in range(4):
                    nxt, nxt_r = Pb[bi][(lv + 1) % 2], Pb_r[bi][(lv + 1) % 2]
                    ev_eng = "dve" if bi == 3 else "act"
                    if lv < 4:
                        cp(nxt[:, :], ps[2 + bi][:, :], [ps_r[2 + bi]], [nxt_r], eng=ev_eng)
                    else:
                        cp(nxt[:, :].rearrange("q (a b) -> q a b", a=2)[:, :, 0:128],
                           ps[2 + bi][:, :].rearrange("q (a b) -> q a b", a=2)[:, :, 0:128], [ps_r[2 + bi]], [nxt_r], eng=ev_eng)
                for s2 in range(2):
                    for p in range(4):
                        h = 2 * p + s2
                        bi = s2 * 2 + p // 2
                        c0 = (p % 2) * 256
                        nxt, nxt_r = Pb[bi][(lv + 1) % 2], Pb_r[bi][(lv + 1) % 2]
                        mm(ps[6 + s2][:, p * 128:(p + 1) * 128], nxt[:, c0:c0 + 128], TTall[:, h, :], True, True,
                           [nxt_r] + TTall_r, [ps_r[6 + s2]])
                for s2 in range(2):
                    tt(TTv[:, s2, :, :], TTv[:, s2, :, :], ps[6 + s2][:, :].rearrange("q (p c) -> q p c", p=4), ALU.add,
                       TTall_r + [ps_r[6 + s2]], TTall_r)

            for h in range(4):
                p, s2 = h // 2, h % 2
                hp = slice(s2 * 64, (s2 + 1) * 64)
                mm(ps[4 + s2][:, p * 128:(p + 1) * 128], qkg[hp, 2 + p, tk], qkg[hp, p, tk], True, True,
                   [qkg_r[p], qkg_r[2 + p]], [ps_r[4 + s2]])
            STv = STb[:, :].rearrange("p (a b c) -> p a b c", a=2, b=2)
            for s2 in range(2):
                tt(STv[:, :, s2, :], ps[4 + s2][:, 0:256].rearrange("p (a c) -> p a c", a=2), cmb[:, 8:10, :], ALU.mult,
                   [ps_r[4 + s2], cm_r], [STb_r])

            for c in range(2):
                cs = slice(c * 64, (c + 1) * 64)
                tkc = slice(s * 128 + c * 64, s * 128 + (c + 1) * 64)
                ci = s * 2 + c
                for h in range(8):
                    p, s2 = h // 2, h % 2
                    o = ps[6][cs, h * 64:(h + 1) * 64]
                    mm(o, AR[:, p, 0, tkc], Hbd[:, p, s2 * 64:(s2 + 1) * 64], True, False, [AR_r[p], Hbd_r], [ps_r[6]])
                    mm(o, Sball[:, h, 2, cs], tokV[:, p * 128 + s2 * 64:p * 128 + (s2 + 1) * 64], False, True,
                       [Sball_r[h], tokV_r], [ps_r[6]])
                for h in range(4):
                    p = h // 2
                    o = ps[5][:, h * 128 + c * 64:h * 128 + (c + 1) * 64]
                    mm(o, tokV[:, 512 + h * 128:512 + (h + 1) * 128], STb[:, h * 128 + c * 64:h * 128 + (c + 1) * 64],
                       True, False, [tokV_r, STb_r], [ps_r[5]])
                    mm(o, Sgbd[:, h, :], qkg[:, p, tkc], False, True, [Sgbd_r, qkg_r[p]], [ps_r[5]])
                cp(Wb[cs, :], ps[6][cs, :], [ps_r[6]], [Wb_r])
                for h in range(4):
                    p, s2 = h // 2, h % 2
                    mm(ps[4][s2 * 64:(s2 + 1) * 64, p * 128:(p + 1) * 128], tokKg[cs, h * 64:(h + 1) * 64],
                       tokV[cs, 512 + h * 128:512 + (h + 1) * 128], True, True, [tokKg_r, tokV_r], [ps_r[4]])
                for h in range(8):
                    mm(ps[7][cs, h * 64:(h + 1) * 64], TTall[:, h, cs], Wb[:, h * 64:(h + 1) * 64], True, True,
                       [TTall_r[0], Wb_r], [ps_r[7]])
                tt(tmpS[:, :], ps[4][:, 0:256], Sg[:, :, :].rearrange("p a b -> p (a b)"), ALU.add, [ps_r[4], Sg_r], [tmpS_r])
                cp(Ub[cs, :], ps[7][cs, :], [ps_r[7]], [Ub_r], eng="dve")
                tt(Sg[:, :, :], tmpS[:, :].rearrange("p (a b) -> p a b", a=2),
                   gamCg[:, :, ci:ci + 1].to_broadcast([128, 2, 128]), ALU.mult, [tmpS_r, gamCg_r], [Sg_r])
                Sgv = Sgbd[:, :, :].rearrange("p (a b) v -> p a b v", b=2)
                cp(Sgv[0:64, :, 0, :], Sg[0:64, :, :], [Sg_r], [Sgbd_r], eng="dve")
                cp(Sgv[64:128, :, 1, :], Sg[64:128, :, :], [Sg_r], [Sgbd_r])
                for h in range(8):
                    p, s2 = h // 2, h % 2
                    o = ps[2][s2 * 64:(s2 + 1) * 64, c * 256 + p * 64:c * 256 + (p + 1) * 64]
                    mm(o, Hbd[:, p, s2 * 64:(s2 + 1) * 64], AR[:, p, 1, tkc], True, False, [Hbd_r, AR_r[p]], [ps_r[2]])
                    mm(o, Ub[:, h * 64:(h + 1) * 64], Sball[:, h, 1, cs], False, False, [Ub_r, Sball_r[h]], [ps_r[2]])
                    mm(o, tokV[:, p * 128 + s2 * 64:p * 128 + (s2 + 1) * 64], Sball[:, h, 3, cs], False, True,
                       [tokV_r, Sball_r[h]], [ps_r[2]])
                for h in range(8):
                    p, s2 = h // 2, h % 2
                    o = ps[3][s2 * 64:(s2 + 1) * 64, p * 64:(p + 1) * 64]
                    cb = p * 128 + s2 * 64
                    mm(o, tokBK[cs, cb:cb + 64], Ub[cs, h * 64:(h + 1) * 64], True, False, [tokBK_r, Ub_r], [ps_r[3]])
                    mm(o, tokBK[cs, 512 + cb:512 + cb + 64], tokV[cs, cb:cb + 64], False, True, [tokBK_r, tokV_r], [ps_r[3]])
                Hs2 = Hs[:, :, :].rearrange("p a b -> p (a b)")
                tt(tmpH[:, :], ps[3][:, 0:256], Hs2, ALU.add, [ps_r[3], Hs_r], [tmpH_r])
                tt(Hs[:, :, :], tmpH[:, :].rearrange("p (a b) -> p a b", a=4),
                   gamCr[:, :, ci:ci + 1].to_broadcast([128, 4, 64]), ALU.mult, [tmpH_r, gamCr_r], [Hs_r])
                cp(Hbd[0:64, :, 0:64], Hs[0:64, :, :], [Hs_r], [Hbd_r], eng="dve")
                cp(Hbd[64:128, :, 64:128], Hs[64:128, :, :], [Hs_r], [Hbd_r])
                cp(aT[:, 0:8, :].bitcast(F32).rearrange("p a t -> p (a t)").rearrange("p (a t) -> p a t", a=4)[:, :, tkc],
                   ps[2][:, c * 256:(c + 1) * 256].rearrange("p (a b) -> p a b", a=4), [ps_r[2]], Yraw_r)
            Ov = aT[:, 8:16, :].bitcast(F32).rearrange("p a t -> p (a t)").rearrange("p (a t) -> p a t", a=4)
            cp(Ov[:, :, tk], ps[5][:, :].rearrange("p (a b) -> p a b", a=4), [ps_r[5]], Oraw_r)

        Yv4 = aT[:, 0:8, :].bitcast(F32).rearrange("p a t -> p (a t)").rearrange("p (a t) -> p a t", a=4)
        Ov4 = aT[:, 8:16, :].bitcast(F32).rearrange("p a t -> p (a t)").rearrange("p (a t) -> p a t", a=4)
        for p in range(4):
            y = Yv4[:, p, :]
            cp(sq1[:, :], y, Yraw_r, [sq1_r])
            bkm = nextbank()
            mm(ps[bkm][:, :], blkones, sq1[:, :], True, True, [cm_r, sq1_r], [ps_r[bkm]])
            ts(F[0][:, :], ps[bkm][:, :], 1.0 / 64.0, None, ALU.mult, None, [ps_r[bkm]], [FR[0]])
            act(sq1[:, :], y, AF.Square, Yraw_r, [sq1_r])
            bke = nextbank()
            mm(ps[bke][:, :], blkones, sq1[:, :], True, True, [cm_r, sq1_r], [ps_r[bke]])
            tt(F[1][:, :], F[0][:, :], F[0][:, :], ALU.mult, [FR[0]], [FR[1]])
            stt(F[2][:, :], ps[bke][:, :], 1.0 / 64.0, F[1][:, :], ALU.mult, ALU.subtract, [ps_r[bke], FR[1]], [FR[2]])
            ts(F[2][:, :], F[2][:, :], 0.0, None, ALU.max, None, [FR[2]], [FR[2]])
            act(F[3][:, :], F[2][:, :], AF.Ln, [FR[2], pc_r], [FR[3]], bias=pcol(PC_EPS + 1))
            act(F[1][:, :], F[3][:, :], AF.Exp, [FR[3]], [FR[1]], scale=-0.5)
            tt(F[2][:, :], y, F[0][:, :], ALU.subtract, Yraw_r + [FR[0]], [FR[2]])
            tt(F[2][:, :], F[2][:, :], F[1][:, :], ALU.mult, [FR[2], FR[1]], [FR[2]])
            ts(F[2][:, :], F[2][:, :], pcol(PC_LW + p), pcol(PC_LB + p), ALU.mult, ALU.add, [FR[2], pc_r], [FR[2]])
            tt(F[2][:, :], F[2][:, :], bonus[:, p, :], ALU.add, [FR[2], bonus_r[p]], [FR[2]])
            tt(mixedT[:, 4 + p, :], F[2][:, :], gater[:, p, :], ALU.mult, [FR[2], gater_r[p]], [mixedT_r[4 + p]])
        for h in range(4):
            o = Ov4[:, h, :]
            act(sq1[:, :], o, AF.Square, Oraw_r, [sq1_r])
            bks = nextbank()
            mm(ps[bks][:, :], ones, sq1[:, :], True, True, [cm_r, sq1_r], [ps_r[bks]])
            act(F[3][:, :], ps[bks][:, :], AF.Ln, [ps_r[bks], pc_r], [FR[3]], bias=pcol(PC_EPS), scale=1.0 / 128.0)
            act(F[1][:, :], F[3][:, :], AF.Exp, [FR[3]], [FR[1]], scale=-0.5)
            tt(F[2][:, :], o, F[1][:, :], ALU.mult, Oraw_r + [FR[1]], [FR[2]])
            stt(mixedT[:, h, :], F[2][:, :], pcol(PC_GN), gateg[:, h, :], ALU.mult, ALU.mult,
                [FR[2], pc_r, gateg_r[h]], [mixedT_r[h]])

        for oc in range(8):
            slot, slot_r = stream_A(sout_s[oc], 1024)
            sv = slot[:, 0:1024].rearrange("p (k c) -> p k c", k=8)
            bk = nextbank()
            for fc in range(8):
                mm(ps[bk][:, :], sv[:, fc, :], mixedT[:, fc, :], fc == 0, fc == 7, [slot_r, mixedT_r[fc]], [ps_r[bk]])
            tt(xb[:, oc, :], ps[bk][:, :], xb[:, oc, :], ALU.add, [ps_r[bk], xr[oc]], [xr[oc]])

    xvs = [a.rearrange("(c p) t -> p c t", p=128) for a in xT_ds]
    ovs = [a.rearrange("(c p) t -> p c t", p=128) for a in out_ds]
    out_ops = []
    for n in range(NT):
        b = n % 2
        xb, xr = xT[b], xT_r[b]
        t0 = n * TT
        xv = xvs[t0 // TH]
        ov = ovs[t0 // TH]
        tl = t0 % TH
        P.add("pool", lambda e, xb=xb, tl=tl, xv=xv: e.dma_start(out=xb[:, :, :], in_=xv[:, :, tl:tl + TT]),
              writes=xr, dkey=("x", b))
        if cfg.get("ffn1", True):
            rmsnorm_to_h(xb, xr, PC_N1, 2)
            ffn(0, xb, xr, 2)
        if cfg.get("mix", True):
            mixer(n, xb, xr)
            if cfg.get("dbg", False):
                P.add("pool", lambda e, t0=t0: e.dma_start(out=dbg_d[:, :, t0:t0 + TT], in_=mixedT[:, :, :]),
                      reads=mixedT_r, dkey=("dbg", 0))
        if cfg.get("ffn2", True):
            rmsnorm_to_h(xb, xr, PC_N2, 2)
            ffn(1, xb, xr, 2)
        final_norm(xb, xr)
        op = P.add("pool", lambda e, xb=xb, tl=tl, ov=ov: e.dma_start(out=ov[:, :, tl:tl + TT], in_=xb[:, :, :]),
                   reads=xr, dkey=("o", b))
        out_ops.append(op)
    P.add("pool", lambda e: e.nop(), extra=out_ops[-2:])

    P.emit(nc, es)
    es.close()
    return nc


def _prep_shared(inp):
    f32 = np.float32
    m = {}
    for i, tag in ((1, "ffn1"), (2, "ffn2")):
        g = _kblocks(np.asarray(inp[f"{tag}_w_gate"][0], f32), 128)
        u = _kblocks(np.asarray(inp[f"{tag}_w_up"][0], f32), 128)
        m[f"wgu{i}"] = np.ascontiguousarray(np.stack([g, u], axis=2)).reshape(NFC, 128, 2048)
        m[f"wd{i}"] = _kblocks(np.asarray(inp[f"{tag}_w_down"][0], f32), 128).reshape(8, 128, DFF)
    win = np.asarray(inp["w_in"][0], f32)
    m["wing"] = _kblocks(win[:, 0:1536], 128).reshape(12, 128, 1024)
    m["winga"] = _kblocks(win[:, 1536:1552], 16).reshape(128, 128)
    m["winr"] = _kblocks(win[:, 1552:3344], 128).reshape(14, 128, 1024)
    mu = np.asarray(inp["rwkv_mu"][0], f32).reshape(14, 1, 128)
    m["mur"] = np.ascontiguousarray(np.broadcast_to(mu, (14, 128, 128)))
    m["wout"] = _kblocks(np.asarray(inp["w_out"][0], f32), 128).reshape(8, 128, 1024)
    m["aw2"] = np.ascontiguousarray(np.asarray(inp["gla_alpha_w2"][0], f32))
    m["w2"] = np.ascontiguousarray(np.asarray(inp["rwkv_w2"][0], f32))
    m["a2"] = np.ascontiguousarray(np.asarray(inp["rwkv_a2"][0], f32))
    m["g2"] = np.ascontiguousarray(np.asarray(inp["rwkv_g2"][0], f32))
    pc = np.zeros((128, PC_TOT), f32)
    pc[:, PC_N1:PC_N1 + 8] = _cols(np.asarray(inp["ffn1_norm"][0], f32), 8)
    pc[:, PC_NM:PC_NM + 8] = _cols(np.asarray(inp["mix_norm"][0], f32), 8)
    pc[:, PC_N2:PC_N2 + 8] = _cols(np.asarray(inp["ffn2_norm"][0], f32), 8)
    pc[:, PC_NF:PC_NF + 8] = _cols(np.asarray(inp["final_norm"], f32), 8)
    pc[:, PC_AB:PC_AB + 2] = _cols(np.asarray(inp["gla_alpha_b"][0], f32), 2)
    pc[:, PC_GN] = np.asarray(inp["gla_norm"][0], f32)
    for col, key in ((PC_W0, "rwkv_w0"), (PC_A0, "rwkv_a0"), (PC_KK, "rwkv_k_k"), (PC_KA, "rwkv_k_a"),
                     (PC_LW, "rwkv_ln_w"), (PC_LB, "rwkv_ln_b")):
        pc[:, col:col + 4] = _cols(np.asarray(inp[key][0], f32), 4)
    pc[:, PC_RK:PC_RK + 4] = _cols(np.asarray(inp["rwkv_r_k"][0], f32).reshape(512), 4)
    pc[:, PC_EPS] = EPS
    pc[:, PC_EPS + 1] = GN_EPS
    pc[:, PC_EPS + 2] = 1e-24
    pc[:, PC_EPS + 3] = 1.0
    m["pc"] = pc
    c = _host_consts()
    m["cm"] = c["cm"].reshape(128, 12 * 128)
    m["rst"] = c["rst"]
    return m


_CFG = {"ffn1": True, "mix": True, "ffn2": True}


def kernel(**inputs):
    x = np.asarray(inputs["x"], np.float32)
    B, T, _ = x.shape
    shared = _prep_shared(inputs)
    nc = build_program(T, _CFG)
    in_maps = []
    for b in range(B):
        m = dict(shared)
        xs = 2 if (T // TT) % 2 == 0 else 1
        th = T // xs
        for i in range(xs):
            m[f"xT{i}"] = np.ascontiguousarray(x[b, i * th:(i + 1) * th].T)
        in_maps.append(m)
    res = run_bass_kernel_spmd(nc, in_maps, core_ids=list(range(B)))
    xs = 2 if (T // TT) % 2 == 0 else 1
    out = np.stack([np.concatenate([r[f"outT{i}"].T for i in range(xs)], axis=0) for r in res.results], axis=0)
    return out.astype(np.float32)
```

```python
import numpy as np
from contextlib import ExitStack
import concourse.bass as bass
import concourse.mybir as mybir
from concourse.bass_utils import run_bass_kernel_spmd

F32 = mybir.dt.float32
BF16 = mybir.dt.bfloat16
ALU = mybir.AluOpType
AF = mybir.ActivationFunctionType

D = 1024
DFF = 2816
NFC = DFF // 128
TT = 512
SEQ = 8192
NCORES = 8
EPS = 1e-6
GN_EPS = 64e-5
GLA_W = 1552
RW_W = 1792
PROJ = 3344

ENGS = ("pe", "act", "dve", "pool", "sp")
SEM_CH = 30000


class Res:
    __slots__ = ("name", "last_w", "readers", "excl")

    def __init__(self, name, excl=False):
        self.name = name
        self.last_w = None
        self.readers = []
        self.excl = excl


class Op:
    __slots__ = ("eng", "fn", "deps", "dkey", "needs_sig", "sig", "n")

    def __init__(self, eng, fn, dkey):
        self.eng = eng
        self.fn = fn
        self.deps = []
        self.dkey = dkey
        self.needs_sig = dkey is not None
        self.sig = None
        self.n = None


class Prog:
    def __init__(self):
        self.ops = {e: [] for e in ENGS}
        self.nops = 0

    def add(self, eng, fn, reads=(), writes=(), dkey=None, extra=()):
        op = Op(eng, fn, dkey)
        if any(r.excl for r in reads):
            writes = list(writes) + [r for r in reads if r.excl]
            reads = [r for r in reads if not r.excl]
        deps = {}
        for r in reads:
            w = r.last_w
            if w is not None:
                deps[id(w)] = (w, True)
        for wr in writes:
            w = wr.last_w
            if w is not None and id(w) not in deps:
                deps[id(w)] = (w, False)
            for rd in wr.readers:
                if id(rd) not in deps:
                    deps[id(rd)] = (rd, False)
        for e in extra:
            deps[id(e)] = (e, True)
        for d, raw in deps.values():
            if d is op:
                continue
            if d.eng == eng and d.dkey is None:
                if eng == "pe" or not raw:
                    continue
            op.deps.append(d)
            d.needs_sig = True
        for r in reads:
            if dkey is None:
                r.readers = [x for x in r.readers if not (x.eng == eng and x.dkey is None)]
            r.readers.append(op)
        for wr in writes:
            wr.last_w = op
            wr.readers = []
        self.ops[eng].append(op)
        self.nops += 1
        return op

    def emit(self, nc, es):
        nsig = {e: 0 for e in ENGS}
        dcount = {}
        for e in ENGS:
            for op in self.ops[e]:
                if op.dkey is not None:
                    dcount[op.dkey] = dcount.get(op.dkey, 0) + 1
                    op.sig = ("d", op.dkey, 16 * dcount[op.dkey])
                elif op.needs_sig:
                    op.sig = ("e", e, nsig[e])
                    nsig[e] += 1
        esems = {}
        for e in ENGS:
            nch = (nsig[e] + SEM_CH - 1) // SEM_CH
            esems[e] = [es.enter_context(nc.semaphore(f"s_{e}_{i}")) for i in range(nch)]
        dsems = {k: es.enter_context(nc.semaphore("d_" + "_".join(str(x) for x in k))) for k in dcount}
        self.nsig = nsig
        block = es.enter_context(nc.Block())
        prog = self

        def run(engname, eng):
            known_e = {e: -1 for e in ENGS}
            known_d = {}
            fuse = engname in ("act", "dve")
            for op in prog.ops[engname]:
                waits = []
                for d in op.deps:
                    s = d.sig
                    if s[0] == "e":
                        if known_e[s[1]] >= s[2]:
                            continue
                        known_e[s[1]] = s[2]
                        waits.append((esems[s[1]][s[2] // SEM_CH], s[2] % SEM_CH + 1))
                    else:
                        if known_d.get(s[1], 0) >= s[2]:
                            continue
                        known_d[s[1]] = s[2]
                        waits.append((dsems[s[1]], s[2]))
                last = waits.pop() if (fuse and waits and op.dkey is None) else None
                for sm, v in waits:
                    eng.wait_ge(sm, v)
                ins = op.fn(eng)
                if last is not None:
                    ins._wait_ge(last[0], last[1])
                s = op.sig
                if s is not None:
                    if s[0] == "e":
                        ins.then_inc(esems[s[1]][s[2] // SEM_CH], 1)
                    else:
                        ins.then_inc(dsems[s[1]], 16)

        @block.tensor
        def _(e):
            run("pe", e)

        @block.scalar
        def _(e):
            run("act", e)

        @block.vector
        def _(e):
            run("dve", e)

        @block.gpsimd
        def _(e):
            run("pool", e)

        @block.sync
        def _(e):
            run("sp", e)


def _kblocks(w, cb):
    K, C = w.shape
    return np.ascontiguousarray(w.reshape(K // 128, 128, C // cb, cb).transpose(2, 1, 0, 3))


def _cols(v, n):
    return np.ascontiguousarray(v.reshape(n, 128).T)


def _host_consts():
    i = np.arange(128)
    same = (i[:, None] // 64) == (i[None, :] // 64)
    strictT = (same & (i[:, None] < i[None, :])).astype(np.float32)
    inclT = (same & (i[:, None] <= i[None, :])).astype(np.float32)
    strict = (same & (i[:, None] > i[None, :])).astype(np.float32)
    ident = np.eye(128, dtype=np.float32)
    blk = same.astype(np.float32)
    ones = np.ones((128, 128), np.float32)
    c = {}
    c["cm"] = np.ascontiguousarray(np.stack([strictT, inclT, strictT, inclT, strict, ident, blk, ones,
                                             inclT, inclT, inclT, inclT], axis=1))
    t = np.arange(TT)
    c["rst"] = np.ascontiguousarray(np.broadcast_to((t % 64 != 0).astype(np.float32)[None, :], (128, TT)))
    return c


PC_N1, PC_NM, PC_N2, PC_NF = 0, 8, 16, 24
PC_AB, PC_GN = 32, 34
PC_W0, PC_A0, PC_KK, PC_KA, PC_RK, PC_LW, PC_LB = 35, 39, 43, 47, 51, 55, 59
PC_NAB = 63
PC_EPS = 65
PC_TOT = 72


def build_program(T, cfg):
    NT = T // TT
    nc = bass.Bass("TRN2", target_bir_lowering=False)
    P = Prog()
    es = ExitStack()

    def din(name, shape, dt=F32):
        return nc.dram_tensor(name, list(shape), dt, kind="ExternalInput").ap()

    def dscr(name, shape, dt=BF16):
        return nc.dram_tensor(name, list(shape), dt, kind="Internal").ap()

    def sb(name, shape, dt):
        return es.enter_context(nc.sbuf_tensor("sb_" + name, list(shape), dt))

    XS = 2 if NT % 2 == 0 else 1
    TH = T // XS
    xT_ds = [din(f"xT{i}", [D, TH]) for i in range(XS)]
    out_ds = [nc.dram_tensor(f"outT{i}", [D, TH], F32, kind="ExternalOutput").ap() for i in range(XS)]
    wgu_d = [din(f"wgu{i}", [NFC, 128, 2048]) for i in (1, 2)]
    wd_d = [din(f"wd{i}", [8, 128, DFF]) for i in (1, 2)]
    wing_d = din("wing", [12, 128, 1024])
    winga_d = din("winga", [128, 128])
    winr_d = din("winr", [14, 128, 1024])
    mur_d = din("mur", [14, 128, 128])
    wout_d = din("wout", [8, 128, 1024])
    aw2_d = din("aw2", [16, 256])
    w2_d = din("w2", [64, 512])
    a2_d = din("a2", [64, 512])
    g2_d = din("g2", [128, 512])
    pc_d = din("pc", [128, PC_TOT])
    cm_d = din("cm", [128, 12 * 128])
    rst_d = din("rst", [128, TT])

    if cfg.get("dbg", False):
        dbg_d = nc.dram_tensor("dbg", [128, 8, T], BF16, kind="ExternalOutput").ap()
    sgu_s = [dscr(f"sgu{i}", [NFC, 128, 2048]) for i in (1, 2)]
    sd_s = [dscr(f"sd{i}", [8, 128, DFF]) for i in (1, 2)]
    sing_s = dscr("sing", [12, 128, 1024])
    singa_s = dscr("singa", [128, 128])
    sinr_s = dscr("sinr", [14, 128, 2048])
    sout_s = dscr("sout", [8, 128, 1024])

    xT = [sb(f"xT{i}", [128, 8, TT], F32) for i in range(2)]
    xT_r = [[Res(f"xT{i}_{c}") for c in range(8)] for i in range(2)]
    hT = sb("hT", [128, 8, TT + 2], BF16)
    hT_r = Res("hT")
    aT = sb("aT", [128, NFC, TT], BF16)
    aT_r = [Res(f"aT{j}") for j in range(NFC)]
    NRA, NRB = 4, 2
    ringA = [sb(f"ringA{i}", [128, 2048], BF16) for i in range(NRA)]
    ringA_r = [Res(f"ringA{i}") for i in range(NRA)]
    ringB = [sb(f"ringB{i}", [128, DFF], BF16) for i in range(NRB)]
    ringB_r = [Res(f"ringB{i}") for i in range(NRB)]
    pc = sb("pc", [128, PC_TOT], F32)
    pc_r = Res("pc")
    cmb = sb("cmb", [128, 12, 128], BF16)
    cm_r = Res("cm")
    rst = sb("rst", [128, TT], F32)
    NFT = 14
    ftmp = [sb(f"ftmp{i}", [128, TT], F32) for i in range(NFT)]
    ftmp_r = [Res(f"ftmp{i}") for i in range(NFT)]
    sqT = aT[:, 0:8, :]

    ps = [es.enter_context(nc.psum_tensor(f"ps{i}", [128, 512], F32)) for i in range(8)]
    ps_r = [Res(f"ps{i}", excl=True) for i in range(8)]

    ident = cmb[:, 5, :]
    blkones = cmb[:, 6, :]
    ones = cmb[:, 7, :]

    pro_stores = []

    last_by_key = {}
    scr_ops = {}

    def id_of(ap_):
        return (ap_.tensor.name, ap_.offset)

    def dram_cast(dst, src, key):
        prev = last_by_key.get(key)
        op = P.add("pool", lambda e, d=dst, s=src: e.dma_start(out=d, in_=s), dkey=key,
                   extra=(prev,) if prev is not None else ())
        last_by_key[key] = op
        pro_stores.append(op)
        scr_ops.setdefault(id_of(dst), []).append(op)

    kctr = [0]

    def nkey(base):
        kctr[0] += 1
        return (base, kctr[0] % 4)

    def cast_ffn(f):
        for j in range(NFC):
            dram_cast(sgu_s[f][j], wgu_d[f][j], nkey("pc"))
        for o in range(8):
            dram_cast(sd_s[f][o], wd_d[f][o], nkey("pc"))

    cast_ffn(0)
    dram_cast(singa_s, winga_d, nkey("pc"))
    for b in range(12):
        dram_cast(sing_s[b], wing_d[b], nkey("pc"))

    P.add("sp", lambda e: e.dma_start(out=pc[:, :], in_=pc_d), writes=[pc_r], dkey=("c", 0))
    P.add("sp", lambda e: e.dma_start(out=rst[:, :], in_=rst_d), writes=[cm_r], dkey=("c", 1))
    for h in range(3):
        P.add("sp", lambda e, h=h: e.dma_start(out=ftmp[h][:, :], in_=cm_d[:, h * 512:(h + 1) * 512]),
              writes=[ftmp_r[h]], dkey=("c", 2 + h))
        P.add("dve", lambda e, h=h: e.tensor_copy(cmb[:, 4 * h:4 * h + 4, :],
                                                  ftmp[h][:, :].rearrange("p (a b) -> p a b", a=4)),
              reads=[ftmp_r[h]], writes=[cm_r])
    P.add("dve", lambda e: e.tensor_scalar(pc[:, PC_NAB:PC_NAB + 2], pc[:, PC_AB:PC_AB + 2], -1.0, None, ALU.mult),
          reads=[pc_r], writes=[pc_r])

    ra = [0]
    rb = [0]
    first_stream = [True]

    def stream_A(src, ncols=2048):
        k = ra[0] % NRA
        ra[0] += 1
        extra = scr_ops.get(id_of(src), pro_stores)
        P.add("sp", lambda e, k=k, s=src, n=ncols: e.dma_start(out=ringA[k][:, 0:n], in_=s),
              writes=[ringA_r[k]], dkey=("ra", k), extra=extra)
        return ringA[k], ringA_r[k]

    def stream_B(src):
        k = rb[0] % NRB
        rb[0] += 1
        P.add("sp", lambda e, k=k, s=src: e.dma_start(out=ringB[k][:, :], in_=s),
              writes=[ringB_r[k]], dkey=("rb", k), extra=scr_ops.get(id_of(src), pro_stores))
        return ringB[k], ringB_r[k]

    def rmsnorm_to_h(xb, xr, gcol, hoff):
        P.add("act", lambda e: e.activation(sqT, xb[:, :, :], AF.Square), reads=xr, writes=aT_r[0:8])
        for c in range(8):
            P.add("pe", lambda e, c=c: e.matmul(ps[6][:, :], ones, sqT[:, c, :], start=(c == 0), stop=(c == 7)),
                  reads=aT_r[0:8] + [cm_r], writes=[ps_r[6]])
        P.add("act", lambda e: e.activation(ftmp[2][:, :], ps[6][:, :], AF.Ln, bias=pc[:, PC_EPS:PC_EPS + 1],
                                            scale=1.0 / D), reads=[ps_r[6], pc_r], writes=[ftmp_r[2]])
        P.add("act", lambda e: e.activation(ftmp[3][:, :], ftmp[2][:, :], AF.Exp, scale=-0.5),
              reads=[ftmp_r[2]], writes=[ftmp_r[3]])
        for c in range(8):
            P.add("dve", lambda e, c=c: e.scalar_tensor_tensor(
                hT[:, c, hoff:hoff + TT], xb[:, c, :], pc[:, gcol + c:gcol + c + 1], ftmp[3][:, :],
                ALU.mult, ALU.mult), reads=[xr[c], ftmp_r[3], pc_r], writes=[hT_r])

    def ffn(f, xb, xr, hoff):
        for j in range(NFC):
            slot, slot_r = stream_A(sgu_s[f][j])
            sv = slot[:, :].rearrange("p (g k c) -> p g k c", g=2, k=8)
            pg, pu = 0 + (j % 2), 2 + (j % 2)
            for kc in range(8):
                P.add("pe", lambda e, kc=kc, sv=sv, pg=pg: e.matmul(
                    ps[pg][:, :], sv[:, 0, kc, :], hT[:, kc, hoff:hoff + TT], start=(kc == 0), stop=(kc == 7)),
                    reads=[slot_r, hT_r], writes=[ps_r[pg]])
            for kc in range(8):
                P.add("pe", lambda e, kc=kc, sv=sv, pu=pu: e.matmul(
                    ps[pu][:, :], sv[:, 1, kc, :], hT[:, kc, hoff:hoff + TT], start=(kc == 0), stop=(kc == 7)),
                    reads=[slot_r, hT_r], writes=[ps_r[pu]])
            ft = j % 2
            P.add("act", lambda e, ft=ft, pg=pg: e.activation(ftmp[ft][:, :], ps[pg][:, :], AF.Silu),
                  reads=[ps_r[pg]], writes=[ftmp_r[ft]])
            P.add("dve", lambda e, ft=ft, pu=pu, j=j: e.tensor_tensor(aT[:, j, :], ftmp[ft][:, :], ps[pu][:, :], ALU.mult),
                  reads=[ftmp_r[ft], ps_r[pu]], writes=[aT_r[j]])
        for o in range(8):
            slot, slot_r = stream_B(sd_s[f][o])
            sv = slot[:, :].rearrange("p (j c) -> p j c", j=NFC)
            pd = 4 + (o % 2)
            for j in range(NFC):
                P.add("pe", lambda e, j=j, sv=sv, pd=pd: e.matmul(
                    ps[pd][:, :], sv[:, j, :], aT[:, j, :], start=(j == 0), stop=(j == NFC - 1)),
                    reads=[slot_r, aT_r[j]], writes=[ps_r[pd]])
            P.add("dve", lambda e, o=o, pd=pd: e.scalar_tensor_tensor(
                xb[:, o, :], ps[pd][:, :], 0.5, xb[:, o, :], ALU.mult, ALU.add),
                reads=[ps_r[pd], xr[o]], writes=[xr[o]])

    def final_norm(xb, xr):
        P.add("act", lambda e: e.activation(sqT, xb[:, :, :], AF.Square), reads=xr, writes=aT_r[0:8])
        for c in range(8):
            P.add("pe", lambda e, c=c: e.matmul(ps[6][:, :], ones, sqT[:, c, :], start=(c == 0), stop=(c == 7)),
                  reads=aT_r[0:8] + [cm_r], writes=[ps_r[6]])
        P.add("act", lambda e: e.activation(ftmp[2][:, :], ps[6][:, :], AF.Ln, bias=pc[:, PC_EPS:PC_EPS + 1],
                                            scale=1.0 / D), reads=[ps_r[6], pc_r], writes=[ftmp_r[2]])
        P.add("act", lambda e: e.activation(ftmp[3][:, :], ftmp[2][:, :], AF.Exp, scale=-0.5),
              reads=[ftmp_r[2]], writes=[ftmp_r[3]])
        for c in range(8):
            P.add("dve", lambda e, c=c: e.scalar_tensor_tensor(
                xb[:, c, :], xb[:, c, :], pc[:, PC_NF + c:PC_NF + c + 1], ftmp[3][:, :],
                ALU.mult, ALU.mult), reads=[xr[c], ftmp_r[3], pc_r], writes=[xr[c]])

    CDEC = 0.6065306597126334

    def RL(name, n):
        return [Res(f"{name}{i}") for i in range(n)]

    hprev = sb("hprev", [128, 8, 1], BF16); hprev_r = Res("hprev")
    aw2b = sb("aw2b", [16, 256], BF16)
    w2a2b = sb("w2a2b", [128, 512], BF16)
    g2b = sb("g2b", [128, 512], BF16)
    smallw_r = Res("smallw")
    qkg = sb("qkg", [128, 4, TT], BF16); qkg_r = RL("qkg", 4)
    vTg = sb("vTg", [128, 4, TT], BF16); vTg_r = RL("vTg", 4)
    gateg = sb("gateg", [128, 4, TT], BF16); gateg_r = RL("gateg", 4)
    gamCg = sb("gamCg", [128, 2, 8], F32); gamCg_r = Res("gamCg")
    alr = sb("alr", [16, TT], BF16); alr_r = Res("alr")
    AR = sb("AR", [128, 4, 2, TT], BF16); AR_r = RL("AR", 4)
    Bt = sb("Bt", [128, 4, TT], BF16); Bt_r = RL("Bt", 4)
    Kt = sb("Kt", [128, 4, TT], BF16); Kt_r = RL("Kt", 4)
    vTr = sb("vTr", [128, 4, TT], BF16); vTr_r = RL("vTr", 4)
    bonus = sb("bonus", [128, 4, TT], BF16); bonus_r = RL("bonus", 4)
    gater = sb("gater", [128, 4, TT], BF16); gater_r = RL("gater", 4)
    gamCr = sb("gamCr", [128, 4, 8], F32); gamCr_r = Res("gamCr")
    wa = sb("wa", [128, TT], BF16); wa_r = Res("wa")
    sgl = sb("sgl", [128, TT], BF16); sgl_r = Res("sgl")
    sq1 = sb("sq1", [128, TT], BF16); sq1_r = Res("sq1")
    Hs = sb("Hs", [128, 4, 64], F32); Hs_r = Res("Hs")
    Hbd = sb("Hbd", [128, 4, 128], BF16); Hbd_r = Res("Hbd")
    Sg = sb("Sg", [128, 2, 128], F32); Sg_r = Res("Sg")
    Sgbd = sb("Sgbd", [128, 4, 128], BF16); Sgbd_r = Res("Sgbd")
    Wb = sb("Wb", [128, 512], BF16); Wb_r = Res("Wb")
    Ub = sb("Ub", [128, 512], BF16); Ub_r = Res("Ub")
    tokBK = sb("tokBK", [128, 1024], BF16); tokBK_r = Res("tokBK")
    tokV = sb("tokV", [128, 1024], BF16); tokV_r = Res("tokV")
    tokKg = sb("tokKg", [128, 256], BF16); tokKg_r = Res("tokKg")
    Sball = sb("Sball", [128, 8, 4, 128], BF16); Sball_r = RL("Sball", 8)
    TTall = sb("TTall", [128, 8, 128], BF16); TTall_r = [Res("TTall")]
    Pb = [[sb(f"Pb{a}{b}", [128, 512], BF16) for b in range(2)] for a in range(4)]
    Pb_r = [[Res(f"Pb{a}{b}") for b in range(2)] for a in range(4)]
    STb = sb("STb", [128, 512], BF16); STb_r = Res("STb")
    tmpH = ftmp[0][:, 0:256]; tmpH_r = ftmp_r[0]
    tmpS = ftmp[1][:, 0:256]; tmpS_r = ftmp_r[1]
    mixedT = sb("mixedT", [128, 8, TT], BF16); mixedT_r = RL("mixedT", 8)
    Yraw = aT[:, 0:8, :].bitcast(F32)
    Oraw = aT[:, 8:16, :].bitcast(F32)
    Yraw_r = aT_r[0:8]
    Oraw_r = aT_r[8:16]

    def mm(out, lhsT, rhs, start, stop, reads, writes):
        return P.add("pe", lambda e: e.matmul(out, lhsT, rhs, start=start, stop=stop), reads, writes)

    def tr(out, in_, reads, writes):
        return P.add("pe", lambda e: e.transpose(out, in_, ident), list(reads) + [cm_r], writes)

    def act(out, in_, func, reads, writes, bias=None, scale=None):
        kw = {}
        if bias is not None:
            kw["bias"] = bias
        if scale is not None:
            kw["scale"] = scale
        return P.add("act", lambda e: e.activation(out, in_, func, **kw), reads, writes)

    def tt(out, in0, in1, op, reads, writes, eng="dve"):
        return P.add(eng, lambda e: e.tensor_tensor(out, in0, in1, op), reads, writes)

    def ts(out, in0, s1, s2, op0, op1, reads, writes, eng="dve"):
        if s2 is None:
            return P.add(eng, lambda e: e.tensor_scalar(out, in0, s1, None, op0), reads, writes)
        return P.add(eng, lambda e: e.tensor_scalar(out, in0, s1, s2, op0, op1), reads, writes)

    def stt(out, in0, sc, in1, op0, op1, reads, writes):
        return P.add("dve", lambda e: e.scalar_tensor_tensor(out, in0, sc, in1, op0, op1), reads, writes)

    def cp(out, in_, reads, writes, eng="act"):
        if eng == "act":
            return P.add("act", lambda e: e.activation(out, in_, AF.Copy), reads, writes)
        return P.add(eng, lambda e: e.tensor_copy(out, in_), reads, writes)

    def scan(out, d0, d1, reads, writes):
        return P.add("dve", lambda e: e.tensor_tensor_scan(out, d0, d1, 0.0, ALU.mult, ALU.add), reads, writes)

    def pcol(c):
        return pc[:, c:c + 1]

    def load_small(dst, src_d, rows, cols, ft, prow=0):
        P.add("sp", lambda e: e.dma_start(out=ftmp[ft][prow:prow + rows, 0:cols], in_=src_d),
              writes=[ftmp_r[ft]], dkey=("c", 5 + ft))
        cp(dst, ftmp[ft][prow:prow + rows, 0:cols], [ftmp_r[ft]], [smallw_r], eng="dve")

    load_small(aw2b[0:16, :], aw2_d, 16, 256, 3)
    load_small(w2a2b[0:64, :], w2_d, 64, 512, 4)
    load_small(w2a2b[64:128, :], a2_d, 64, 512, 5, prow=64)
    load_small(g2b[:, :], g2_d, 128, 512, 6)
    for tl, tr_ in ((Hs, Hs_r), (Hbd, Hbd_r), (Sg, Sg_r), (Sgbd, Sgbd_r), (Wb, Wb_r), (Ub, Ub_r), (hprev, hprev_r)):
        ap_ = tl[:, :, :] if len(tl.shape) == 3 else tl[:, :]
        P.add("pool", lambda e, a=ap_: e.memset(a, 0.0), writes=[tr_])
    stg_r = [Res("stgA"), Res("stgB")]
    for blk in range(14):
        s2 = blk % 2
        fW, fM, fO = ftmp[7 + 3 * s2], ftmp[8 + 3 * s2], ftmp[9 + 3 * s2]
        rW, rM, rO = ftmp_r[7 + 3 * s2], ftmp_r[8 + 3 * s2], ftmp_r[9 + 3 * s2]
        for half in range(2):
            P.add("sp", lambda e, blk=blk, half=half, fW=fW: e.dma_start(
                out=fW[:, :], in_=winr_d[blk][:, half * 512:(half + 1) * 512]), writes=[rW], dkey=("pw", s2))
            if half == 0:
                P.add("sp", lambda e, blk=blk, fM=fM: e.dma_start(out=fM[:, 0:128], in_=mur_d[blk]),
                      writes=[rM], dkey=("pm", s2))
                ts(fM[:, 128:256], fM[:, 0:128], -1.0, 1.0, ALU.mult, ALU.add, [rM], [rM])
            fWv = fW[:, :].rearrange("p (k c) -> p k c", k=4)
            fOv = fO[:, :].bitcast(BF16).rearrange("p (k c) -> p k c", k=8)
            tt(fOv[:, 0:4, :], fWv, fM[:, 128:256].unsqueeze(1).to_broadcast([128, 4, 128]), ALU.mult, [rW, rM], [rO])
            tt(fOv[:, 4:8, :], fWv, fM[:, 0:128].unsqueeze(1).to_broadcast([128, 4, 128]), ALU.mult, [rW, rM], [rO])
            dv = sinr_s[blk].rearrange("p (k c) -> p k c", k=16)
            op1 = P.add("pool", lambda e, dv=dv, fOv=fOv, half=half: e.dma_start(
                out=dv[:, half * 4:half * 4 + 4, :], in_=fOv[:, 0:4, :]), reads=[rO], dkey=("ps1", s2))
            op2 = P.add("pool", lambda e, dv=dv, fOv=fOv, half=half: e.dma_start(
                out=dv[:, 8 + half * 4:8 + half * 4 + 4, :], in_=fOv[:, 4:8, :]), reads=[rO], dkey=("ps2", s2))
            pro_stores.append(op1)
            pro_stores.append(op2)
            scr_ops.setdefault(id_of(sinr_s[blk]), []).extend([op1, op2])
    for o in range(8):
        dram_cast(sout_s[o], wout_d[o], nkey("pc"))
    cast_ffn(1)

    bank_rr = [0]

    def nextbank():
        b_ = bank_rr[0] % 8
        bank_rr[0] += 1
        return b_

    def proj_block(src, ncols, K16, M=128):
        slot, slot_r = stream_A(src, ncols)
        nk = 16 if K16 else 8
        sv = slot[:, 0:ncols].rearrange("p (k c) -> p k c", k=nk)
        bk = nextbank()
        for kc in range(nk):
            rhs = hT[:, kc, 2:TT + 2] if kc < 8 else hT[:, kc - 8, 1:TT + 1]
            mm(ps[bk][0:M, :], sv[:, kc, :], rhs, kc == 0, kc == nk - 1, [slot_r, hT_r], [ps_r[bk]])
        return bk

    def mixer(n, xb, xr):
        rmsnorm_to_h(xb, xr, PC_NM, 2)
        cp(hT[:, :, 1:2], hprev[:, :, :], [hprev_r], [hT_r], eng="pool")
        cp(hprev[:, :, :], hT[:, :, TT + 1:TT + 2], [hT_r], [hprev_r], eng="pool")

        stage = float(cfg.get("stage", 99))
        if stage < 1:
            return
        bk = proj_block(singa_s, 128, False, M=16)
        cp(alr[0:16, :], ps[bk][0:16, :], [ps_r[bk]], [alr_r])
        if stage < 0.5:
            return
        for c in range(2):
            bk = nextbank()
            mm(ps[bk][:, :], aw2b[0:16, c * 128:(c + 1) * 128], alr[0:16, :], True, True, [smallw_r, alr_r], [ps_r[bk]])
            act(ftmp[8][:, :], ps[bk][:, :], AF.Exp, [ps_r[bk], pc_r], [ftmp_r[8]], bias=pcol(PC_NAB + c), scale=-1.0)
            act(ftmp[9][:, :], ftmp[8][:, :], AF.Ln, [ftmp_r[8], pc_r], [ftmp_r[9]], bias=pcol(PC_EPS + 3))
            scan(ftmp[10][:, :], rst[:, :], ftmp[9][:, :], [ftmp_r[9], cm_r], [ftmp_r[10]])
            act(ftmp[4 + c][:, :], ftmp[10][:, :], AF.Exp, [ftmp_r[10]], [ftmp_r[4 + c]], scale=-1.0 / 16.0)
            act(ftmp[6 + c][:, :], ftmp[10][:, :], AF.Exp, [ftmp_r[10]], [ftmp_r[6 + c]], scale=1.0 / 16.0)
            cp(gamCg[:, c, :], ftmp[4 + c][:, :].rearrange("p (a b) -> p a b", b=64)[:, :, 63],
               [ftmp_r[4 + c]], [gamCg_r], eng="dve")
        if stage < 0.7:
            return
        for c in range(2):
            bk = proj_block(sing_s[c], 1024, False)
            stt(qkg[:, c, :], ps[bk][:, :], 0.125, ftmp[4 + c][:, :], ALU.mult, ALU.mult,
                [ps_r[bk], ftmp_r[4 + c]], [qkg_r[c]])
        for c in range(2):
            bk = proj_block(sing_s[2 + c], 1024, False)
            tt(qkg[:, 2 + c, :], ps[bk][:, :], ftmp[6 + c][:, :], ALU.mult, [ps_r[bk], ftmp_r[6 + c]], [qkg_r[2 + c]])
        for h in range(4):
            bk = proj_block(sing_s[4 + h], 1024, False)
            cp(vTg[:, h, :], ps[bk][:, :], [ps_r[bk]], [vTg_r[h]])
        for h in range(4):
            bk = proj_block(sing_s[8 + h], 1024, False)
            act(gateg[:, h, :], ps[bk][:, :], AF.Silu, [ps_r[bk]], [gateg_r[h]])

        if stage < 2:
            return
        bk = proj_block(sinr_s[12], 2048, True)
        act(wa[0:64, :], ps[bk][0:64, :], AF.Tanh, [ps_r[bk]], [wa_r])
        cp(wa[64:128, :], ps[bk][64:128, :], [ps_r[bk]], [wa_r])
        bk = proj_block(sinr_s[13], 2048, True)
        act(sgl[:, :], ps[bk][:, :], AF.Sigmoid, [ps_r[bk]], [sgl_r])
        for p in range(4):
            bk = nextbank()
            mm(ps[bk][:, :], g2b[:, p * 128:(p + 1) * 128], sgl[:, :], True, True, [smallw_r, sgl_r], [ps_r[bk]])
            cp(gater[:, p, :], ps[bk][:, :], [ps_r[bk]], [gater_r[p]])
        F = ftmp
        FR = ftmp_r
        if stage < 1.5:
            return
        for p in range(4):
            bk = nextbank()
            mm(ps[bk][:, :], w2a2b[0:64, p * 128:(p + 1) * 128], wa[0:64, :], True, True, [smallw_r, wa_r], [ps_r[bk]])
            act(F[8][:, :], ps[bk][:, :], AF.Sigmoid, [ps_r[bk], pc_r], [FR[8]], bias=pcol(PC_W0 + p))
            if stage < 1.6:
                continue
            bk = nextbank()
            mm(ps[bk][:, :], w2a2b[64:128, p * 128:(p + 1) * 128], wa[64:128, :], True, True, [smallw_r, wa_r], [ps_r[bk]])
            act(F[9][:, :], ps[bk][:, :], AF.Sigmoid, [ps_r[bk], pc_r], [FR[9]], bias=pcol(PC_A0 + p))
            if stage < 1.7:
                continue
            scan(F[10][:, :], rst[:, :], F[8][:, :], [FR[8], cm_r], [FR[10]])
            act(F[0][:, :], F[10][:, :], AF.Exp, [FR[10]], [FR[0]], scale=-CDEC)
            act(F[1][:, :], F[10][:, :], AF.Exp, [FR[10]], [FR[1]], scale=CDEC)
            tt(F[11][:, :], F[10][:, :], F[8][:, :], ALU.subtract, [FR[10], FR[8]], [FR[11]])
            act(F[2][:, :], F[11][:, :], AF.Exp, [FR[11]], [FR[2]], scale=-CDEC)
            cp(gamCr[:, p, :], F[0][:, :].rearrange("p (a b) -> p a b", b=64)[:, :, 63], [FR[0]], [gamCr_r], eng="dve")
            if stage < 1.8:
                continue
            bkK = proj_block(sinr_s[4 + p], 2048, True)
            ts(F[11][:, :], ps[bkK][:, :], pcol(PC_KK + p), None, ALU.mult, None, [ps_r[bkK], pc_r], [FR[11]])
            act(sq1[:, :], F[11][:, :], AF.Square, [FR[11]], [sq1_r])
            bk = nextbank()
            mm(ps[bk][:, :], blkones, sq1[:, :], True, True, [cm_r, sq1_r], [ps_r[bk]])
            act(F[3][:, :], ps[bk][:, :], AF.Ln, [ps_r[bk], pc_r], [FR[3]], bias=pcol(PC_EPS + 2))
            act(F[12][:, :], F[3][:, :], AF.Exp, [FR[3]], [FR[12]], scale=-0.5)
            tt(F[11][:, :], F[11][:, :], F[12][:, :], ALU.mult, [FR[11], FR[12]], [FR[11]])
            if stage < 1.9:
                continue
            tt(F[3][:, :], F[11][:, :], F[9][:, :], ALU.mult, [FR[11], FR[9]], [FR[3]])
            tt(Bt[:, p, :], F[3][:, :], F[1][:, :], ALU.mult, [FR[3], FR[1]], [Bt_r[p]])
            stt(AR[:, p, 0, :], F[11][:, :], -1.0, F[2][:, :], ALU.mult, ALU.mult, [FR[11], FR[2]], [AR_r[p]])
            ts(F[3][:, :], F[9][:, :], -1.0, pcol(PC_KA + p), ALU.add, ALU.mult, [FR[9], pc_r], [FR[3]])
            stt(F[13][:, :], F[3][:, :], 1.0, ps[bkK][:, :], ALU.add, ALU.mult, [FR[3], ps_r[bkK]], [FR[13]])
            tt(Kt[:, p, :], F[13][:, :], F[1][:, :], ALU.mult, [FR[13], FR[1]], [Kt_r[p]])
            if stage < 1.95:
                continue
            bkR = proj_block(sinr_s[p], 2048, True)
            tt(AR[:, p, 1, :], ps[bkR][:, :], F[0][:, :], ALU.mult, [ps_r[bkR], FR[0]], [AR_r[p]])
            if stage < 1.98:
                continue
            stt(sq1[:, :], ps[bkR][:, :], pcol(PC_RK + p), F[13][:, :], ALU.mult, ALU.mult,
                [ps_r[bkR], pc_r, FR[13]], [sq1_r])
            if stage < 1.985:
                continue
            bk = nextbank()
            mm(ps[bk][:, :], blkones, sq1[:, :], True, True, [cm_r, sq1_r], [ps_r[bk]])
            cp(F[12][:, :], ps[bk][:, :], [ps_r[bk]], [FR[12]])
            if stage < 1.99:
                continue
            bkV = proj_block(sinr_s[8 + p], 2048, True)
            cp(vTr[:, p, :], ps[bkV][:, :], [ps_r[bkV]], [vTr_r[p]])
            if stage < 1.995:
                continue
            tt(bonus[:, p, :], ps[bkV][:, :], F[12][:, :], ALU.mult, [ps_r[bkV], FR[12]], [bonus_r[p]])

        if stage < 3:
            return
        ps0b = ps[0][:, :].bitcast(BF16)
        ps1b = ps[1][:, :].bitcast(BF16)
        Yv = Yraw.rearrange("p a (b t) -> p (a b) t", b=2) if False else None
        for s in range(TT // 128):
            tk = slice(s * 128, (s + 1) * 128)
            for p in range(4):
                tr(ps0b[:, p * 128:(p + 1) * 128], Bt[:, p, tk], [Bt_r[p]], [ps_r[0]])
            for p in range(4):
                tr(ps0b[:, 512 + p * 128:512 + (p + 1) * 128], Kt[:, p, tk], [Kt_r[p]], [ps_r[0]])
            cp(tokBK[:, :], ps0b, [ps_r[0]], [tokBK_r], eng="dve")
            for p in range(4):
                tr(ps1b[:, p * 128:(p + 1) * 128], vTr[:, p, tk], [vTr_r[p]], [ps_r[1]])
            for h in range(4):
                tr(ps1b[:, 512 + h * 128:512 + (h + 1) * 128], vTg[:, h, tk], [vTg_r[h]], [ps_r[1]])
            cp(tokV[:, :], ps1b, [ps_r[1]], [tokV_r])
            for c in range(2):
                tr(ps0b[:, c * 128:(c + 1) * 128], qkg[:, 2 + c, tk], [qkg_r[2 + c]], [ps_r[0]])
            cp(tokKg[:, :], ps0b[:, 0:256], [ps_r[0]], [tokKg_r], eng="dve")

            TTv = TTall[:, :, :].rearrange("q (p s) c -> q s p c", s=2)
            order = [(p_, 0) for p_ in range(4)] + [(p_, 1) for p_ in range(4)]
            for idx, (p, s2) in enumerate(order):
                h = 2 * p + s2
                hp = slice(s2 * 64, (s2 + 1) * 64)
                SB = (0, 1, 6, 7)[idx % 4]
                bi = s2 * 2 + p // 2
                PB = 2 + bi
                c0 = (p % 2) * 256
                arv = AR[hp, p, :, tk]
                mm(ps[SB][:, 0:256].rearrange("p (a b) -> p a b", a=2), Bt[hp, p, tk], arv, True, True,
                   [Bt_r[p], AR_r[p]], [ps_r[SB]])
                mm(ps[SB][:, 256:512].rearrange("p (a b) -> p a b", a=2), Kt[hp, p, tk], arv, True, True,
                   [Kt_r[p], AR_r[p]], [ps_r[SB]])
                mm(ps[PB][:, c0:c0 + 128], AR[hp, p, 0, tk], Bt[hp, p, tk], True, True, [Bt_r[p], AR_r[p]], [ps_r[PB]])
                tt(Sball[:, h, :, :], ps[SB][:, :].rearrange("p (a b) -> p a b", a=4), cmb[:, 0:4, :], ALU.mult,
                   [ps_r[SB], cm_r], [Sball_r[h]])
                tt(Pb[bi][0][:, c0:c0 + 128], ps[PB][:, c0:c0 + 128], cmb[:, 4, :], ALU.mult, [ps_r[PB], cm_r], [Pb_r[bi][0]])
                cp(Pb[bi][0][:, c0 + 128:c0 + 256], Sball[:, h, 0, :], [Sball_r[h]], [Pb_r[bi][0]])
                tt(TTall[:, h, :], Sball[:, h, 0, :], ident, ALU.add, [Sball_r[h], cm_r], TTall_r, eng="pool")
            for lv in range(5):
                for bi in range(4):
                    cur, cur_r = Pb[bi][lv % 2], Pb_r[bi][lv % 2]
                    for c0 in (0, 256):
                        mm(ps[2 + bi][:, c0:c0 + 128], cur[:, c0 + 128:c0 + 256], cur[:, c0:c0 + 128], True, True,
                           [cur_r], [ps_r[2 + bi]])
                        if lv < 4:
                            mm(ps[2 + bi][:, c0 + 128:c0 + 256], cur[:, c0:c0 + 128], cur[:, c0 + 128:c0 + 256], True, True,
                               [cur_r], [ps_r[2 + bi]])
                for bi in range(4):
                    nxt, nxt_r = Pb[bi][(lv + 1) % 2], Pb_r[bi][(lv + 1) % 2]
                    ev_eng = "dve" if bi == 3 else "act"
                    if lv < 4:
                        cp(nxt[:, :], ps[2 + bi][:, :], [ps_r[2 + bi]], [nxt_r], eng=ev_eng)
                    else:
                        cp(nxt[:, :].rearrange("q (a b) -> q a b", a=2)[:, :, 0:128],
                           ps[2 + bi][:, :].rearrange("q (a b) -> q a b", a=2)[:, :, 0:128], [ps_r[2 + bi]], [nxt_r], eng=ev_eng)
                for s2 in range(2):
                    for p in range(4):
                        h = 2 * p + s2
                        bi = s2 * 2 + p // 2
                        c0 = (p % 2) * 256
                        nxt, nxt_r = Pb[bi][(lv + 1) % 2], Pb_r[bi][(lv + 1) % 2]
                        mm(ps[6 + s2][:, p * 128:(p + 1) * 128], nxt[:, c0:c0 + 128], TTall[:, h, :], True, True,
                           [nxt_r] + TTall_r, [ps_r[6 + s2]])
                for s2 in range(2):
                    tt(TTv[:, s2, :, :], TTv[:, s2, :, :], ps[6 + s2][:, :].rearrange("q (p c) -> q p c", p=4), ALU.add,
                       TTall_r + [ps_r[6 + s2]], TTall_r)

            for h in range(4):
                p, s2 = h // 2, h % 2
                hp = slice(s2 * 64, (s2 + 1) * 64)
                mm(ps[4 + s2][:, p * 128:(p + 1) * 128], qkg[hp, 2 + p, tk], qkg[hp, p, tk], True, True,
                   [qkg_r[p], qkg_r[2 + p]], [ps_r[4 + s2]])
            STv = STb[:, :].rearrange("p (a b c) -> p a b c", a=2, b=2)
            for s2 in range(2):
                tt(STv[:, :, s2, :], ps[4 + s2][:, 0:256].rearrange("p (a c) -> p a c", a=2), cmb[:, 8:10, :], ALU.mult,
                   [ps_r[4 + s2], cm_r], [STb_r])

            for c in range(2):
                cs = slice(c * 64, (c + 1) * 64)
                tkc = slice(s * 128 + c * 64, s * 128 + (c + 1) * 64)
                ci = s * 2 + c
                for h in range(8):
                    p, s2 = h // 2, h % 2
                    o = ps[6][cs, h * 64:(h + 1) * 64]
                    mm(o, AR[:, p, 0, tkc], Hbd[:, p, s2 * 64:(s2 + 1) * 64], True, False, [AR_r[p], Hbd_r], [ps_r[6]])
                    mm(o, Sball[:, h, 2, cs], tokV[:, p * 128 + s2 * 64:p * 128 + (s2 + 1) * 64], False, True,
                       [Sball_r[h], tokV_r], [ps_r[6]])
                for h in range(4):
                    p = h // 2
                    o = ps[5][:, h * 128 + c * 64:h * 128 + (c + 1) * 64]
                    mm(o, tokV[:, 512 + h * 128:512 + (h + 1) * 128], STb[:, h * 128 + c * 64:h * 128 + (c + 1) * 64],
                       True, False, [tokV_r, STb_r], [ps_r[5]])
                    mm(o, Sgbd[:, h, :], qkg[:, p, tkc], False, True, [Sgbd_r, qkg_r[p]], [ps_r[5]])
                cp(Wb[cs, :], ps[6][cs, :], [ps_r[6]], [Wb_r])
                for h in range(4):
                    p, s2 = h // 2, h % 2
                    mm(ps[4][s2 * 64:(s2 + 1) * 64, p * 128:(p + 1) * 128], tokKg[cs, h * 64:(h + 1) * 64],
                       tokV[cs, 512 + h * 128:512 + (h + 1) * 128], True, True, [tokKg_r, tokV_r], [ps_r[4]])
                for h in range(8):
                    mm(ps[7][cs, h * 64:(h + 1) * 64], TTall[:, h, cs], Wb[:, h * 64:(h + 1) * 64], True, True,
                       [TTall_r[0], Wb_r], [ps_r[7]])
                tt(tmpS[:, :], ps[4][:, 0:256], Sg[:, :, :].rearrange("p a b -> p (a b)"), ALU.add, [ps_r[4], Sg_r], [tmpS_r])
                cp(Ub[cs, :], ps[7][cs, :], [ps_r[7]], [Ub_r], eng="dve")
                tt(Sg[:, :, :], tmpS[:, :].rearrange("p (a b) -> p a b", a=2),
                   gamCg[:, :, ci:ci + 1].to_broadcast([128, 2, 128]), ALU.mult, [tmpS_r, gamCg_r], [Sg_r])
                Sgv = Sgbd[:, :, :].rearrange("p (a b) v -> p a b v", b=2)
                cp(Sgv[0:64, :, 0, :], Sg[0:64, :, :], [Sg_r], [Sgbd_r], eng="dve")
                cp(Sgv[64:128, :, 1, :], Sg[64:128, :, :], [Sg_r], [Sgbd_r])
                for h in range(8):
                    p, s2 = h // 2, h % 2
                    o = ps[2][s2 * 64:(s2 + 1) * 64, c * 256 + p * 64:c * 256 + (p + 1) * 64]
                    mm(o, Hbd[:, p, s2 * 64:(s2 + 1) * 64], AR[:, p, 1, tkc], True, False, [Hbd_r, AR_r[p]], [ps_r[2]])
                    mm(o, Ub[:, h * 64:(h + 1) * 64], Sball[:, h, 1, cs], False, False, [Ub_r, Sball_r[h]], [ps_r[2]])
                    mm(o, tokV[:, p * 128 + s2 * 64:p * 128 + (s2 + 1) * 64], Sball[:, h, 3, cs], False, True,
                       [tokV_r, Sball_r[h]], [ps_r[2]])
                for h in range(8):
                    p, s2 = h // 2, h % 2
                    o = ps[3][s2 * 64:(s2 + 1) * 64, p * 64:(p + 1) * 64]
                    cb = p * 128 + s2 * 64
                    mm(o, tokBK[cs, cb:cb + 64], Ub[cs, h * 64:(h + 1) * 64], True, False, [tokBK_r, Ub_r], [ps_r[3]])
                    mm(o, tokBK[cs, 512 + cb:512 + cb + 64], tokV[cs, cb:cb + 64], False, True, [tokBK_r, tokV_r], [ps_r[3]])
                Hs2 = Hs[:, :, :].rearrange("p a b -> p (a b)")
                tt(tmpH[:, :], ps[3][:, 0:256], Hs2, ALU.add, [ps_r[3], Hs_r], [tmpH_r])
                tt(Hs[:, :, :], tmpH[:, :].rearrange("p (a b) -> p a b", a=4),
                   gamCr[:, :, ci:ci + 1].to_broadcast([128, 4, 64]), ALU.mult, [tmpH_r, gamCr_r], [Hs_r])
                cp(Hbd[0:64, :, 0:64], Hs[0:64, :, :], [Hs_r], [Hbd_r], eng="dve")
                cp(Hbd[64:128, :, 64:128], Hs[64:128, :, :], [Hs_r], [Hbd_r])
                cp(aT[:, 0:8, :].bitcast(F32).rearrange("p a t -> p (a t)").rearrange("p (a t) -> p a t", a=4)[:, :, tkc],
                   ps[2][:, c * 256:(c + 1) * 256].rearrange("p (a b) -> p a b", a=4), [ps_r[2]], Yraw_r)
            Ov = aT[:, 8:16, :].bitcast(F32).rearrange("p a t -> p (a t)").rearrange("p (a t) -> p a t", a=4)
            cp(Ov[:, :, tk], ps[5][:, :].rearrange("p (a b) -> p a b", a=4), [ps_r[5]], Oraw_r)

        Yv4 = aT[:, 0:8, :].bitcast(F32).rearrange("p a t -> p (a t)").rearrange("p (a t) -> p a t", a=4)
        Ov4 = aT[:, 8:16, :].bitcast(F32).rearrange("p a t -> p (a t)").rearrange("p (a t) -> p a t", a=4)
        for p in range(4):
            y = Yv4[:, p, :]
            cp(sq1[:, :], y, Yraw_r, [sq1_r])
            bkm = nextbank()
            mm(ps[bkm][:, :], blkones, sq1[:, :], True, True, [cm_r, sq1_r], [ps_r[bkm]])
            ts(F[0][:, :], ps[bkm][:, :], 1.0 / 64.0, None, ALU.mult, None, [ps_r[bkm]], [FR[0]])
            act(sq1[:, :], y, AF.Square, Yraw_r, [sq1_r])
            bke = nextbank()
            mm(ps[bke][:, :], blkones, sq1[:, :], True, True, [cm_r, sq1_r], [ps_r[bke]])
            tt(F[1][:, :], F[0][:, :], F[0][:, :], ALU.mult, [FR[0]], [FR[1]])
            stt(F[2][:, :], ps[bke][:, :], 1.0 / 64.0, F[1][:, :], ALU.mult, ALU.subtract, [ps_r[bke], FR[1]], [FR[2]])
            ts(F[2][:, :], F[2][:, :], 0.0, None, ALU.max, None, [FR[2]], [FR[2]])
            act(F[3][:, :], F[2][:, :], AF.Ln, [FR[2], pc_r], [FR[3]], bias=pcol(PC_EPS + 1))
            act(F[1][:, :], F[3][:, :], AF.Exp, [FR[3]], [FR[1]], scale=-0.5)
            tt(F[2][:, :], y, F[0][:, :], ALU.subtract, Yraw_r + [FR[0]], [FR[2]])
            tt(F[2][:, :], F[2][:, :], F[1][:, :], ALU.mult, [FR[2], FR[1]], [FR[2]])
            ts(F[2][:, :], F[2][:, :], pcol(PC_LW + p), pcol(PC_LB + p), ALU.mult, ALU.add, [FR[2], pc_r], [FR[2]])
            tt(F[2][:, :], F[2][:, :], bonus[:, p, :], ALU.add, [FR[2], bonus_r[p]], [FR[2]])
            tt(mixedT[:, 4 + p, :], F[2][:, :], gater[:, p, :], ALU.mult, [FR[2], gater_r[p]], [mixedT_r[4 + p]])
        for h in range(4):
            o = Ov4[:, h, :]
            act(sq1[:, :], o, AF.Square, Oraw_r, [sq1_r])
            bks = nextbank()
            mm(ps[bks][:, :], ones, sq1[:, :], True, True, [cm_r, sq1_r], [ps_r[bks]])
            act(F[3][:, :], ps[bks][:, :], AF.Ln, [ps_r[bks], pc_r], [FR[3]], bias=pcol(PC_EPS), scale=1.0 / 128.0)
            act(F[1][:, :], F[3][:, :], AF.Exp, [FR[3]], [FR[1]], scale=-0.5)
            tt(F[2][:, :], o, F[1][:, :], ALU.mult, Oraw_r + [FR[1]], [FR[2]])
            stt(mixedT[:, h, :], F[2][:, :], pcol(PC_GN), gateg[:, h, :], ALU.mult, ALU.mult,
                [FR[2], pc_r, gateg_r[h]], [mixedT_r[h]])

        for oc in range(8):
            slot, slot_r = stream_A(sout_s[oc], 1024)
            sv = slot[:, 0:1024].rearrange("p (k c) -> p k c", k=8)
            bk = nextbank()
            for fc in range(8):
                mm(ps[bk][:, :], sv[:, fc, :], mixedT[:, fc, :], fc == 0, fc == 7, [slot_r, mixedT_r[fc]], [ps_r[bk]])
            tt(xb[:, oc, :], ps[bk][:, :], xb[:, oc, :], ALU.add, [ps_r[bk], xr[oc]], [xr[oc]])

    xvs = [a.rearrange("(c p) t -> p c t", p=128) for a in xT_ds]
    ovs = [a.rearrange("(c p) t -> p c t", p=128) for a in out_ds]
    out_ops = []
    for n in range(NT):
        b = n % 2
        xb, xr = xT[b], xT_r[b]
        t0 = n * TT
        xv = xvs[t0 // TH]
        ov = ovs[t0 // TH]
        tl = t0 % TH
        P.add("pool", lambda e, xb=xb, tl=tl, xv=xv: e.dma_start(out=xb[:, :, :], in_=xv[:, :, tl:tl + TT]),
              writes=xr, dkey=("x", b))
        if cfg.get("ffn1", True):
            rmsnorm_to_h(xb, xr, PC_N1, 2)
            ffn(0, xb, xr, 2)
        if cfg.get("mix", True):
            mixer(n, xb, xr)
            if cfg.get("dbg", False):
                P.add("pool", lambda e, t0=t0: e.dma_start(out=dbg_d[:, :, t0:t0 + TT], in_=mixedT[:, :, :]),
                      reads=mixedT_r, dkey=("dbg", 0))
        if cfg.get("ffn2", True):
            rmsnorm_to_h(xb, xr, PC_N2, 2)
            ffn(1, xb, xr, 2)
        final_norm(xb, xr)
        op = P.add("pool", lambda e, xb=xb, tl=tl, ov=ov: e.dma_start(out=ov[:, :, tl:tl + TT], in_=xb[:, :, :]),
                   reads=xr, dkey=("o", b))
        out_ops.append(op)
    P.add("pool", lambda e: e.nop(), extra=out_ops[-2:])

    P.emit(nc, es)
    es.close()
    return nc


def _prep_shared(inp):
    f32 = np.float32
    m = {}
    for i, tag in ((1, "ffn1"), (2, "ffn2")):
        g = _kblocks(np.asarray(inp[f"{tag}_w_gate"][0], f32), 128)
        u = _kblocks(np.asarray(inp[f"{tag}_w_up"][0], f32), 128)
        m[f"wgu{i}"] = np.ascontiguousarray(np.stack([g, u], axis=2)).reshape(NFC, 128, 2048)
        m[f"wd{i}"] = _kblocks(np.asarray(inp[f"{tag}_w_down"][0], f32), 128).reshape(8, 128, DFF)
    win = np.asarray(inp["w_in"][0], f32)
    m["wing"] = _kblocks(win[:, 0:1536], 128).reshape(12, 128, 1024)
    m["winga"] = _kblocks(win[:, 1536:1552], 16).reshape(128, 128)
    m["winr"] = _kblocks(win[:, 1552:3344], 128).reshape(14, 128, 1024)
    mu = np.asarray(inp["rwkv_mu"][0], f32).reshape(14, 1, 128)
    m["mur"] = np.ascontiguousarray(np.broadcast_to(mu, (14, 128, 128)))
    m["wout"] = _kblocks(np.asarray(inp["w_out"][0], f32), 128).reshape(8, 128, 1024)
    m["aw2"] = np.ascontiguousarray(np.asarray(inp["gla_alpha_w2"][0], f32))
    m["w2"] = np.ascontiguousarray(np.asarray(inp["rwkv_w2"][0], f32))
    m["a2"] = np.ascontiguousarray(np.asarray(inp["rwkv_a2"][0], f32))
    m["g2"] = np.ascontiguousarray(np.asarray(inp["rwkv_g2"][0], f32))
    pc = np.zeros((128, PC_TOT), f32)
    pc[:, PC_N1:PC_N1 + 8] = _cols(np.asarray(inp["ffn1_norm"][0], f32), 8)
    pc[:, PC_NM:PC_NM + 8] = _cols(np.asarray(inp["mix_norm"][0], f32), 8)
    pc[:, PC_N2:PC_N2 + 8] = _cols(np.asarray(inp["ffn2_norm"][0], f32), 8)
    pc[:, PC_NF:PC_NF + 8] = _cols(np.asarray(inp["final_norm"], f32), 8)
    pc[:, PC_AB:PC_AB + 2] = _cols(np.asarray(inp["gla_alpha_b"][0], f32), 2)
    pc[:, PC_GN] = np.asarray(inp["gla_norm"][0], f32)
    for col, key in ((PC_W0, "rwkv_w0"), (PC_A0, "rwkv_a0"), (PC_KK, "rwkv_k_k"), (PC_KA, "rwkv_k_a"),
                     (PC_LW, "rwkv_ln_w"), (PC_LB, "rwkv_ln_b")):
        pc[:, col:col + 4] = _cols(np.asarray(inp[key][0], f32), 4)
    pc[:, PC_RK:PC_RK + 4] = _cols(np.asarray(inp["rwkv_r_k"][0], f32).reshape(512), 4)
    pc[:, PC_EPS] = EPS
    pc[:, PC_EPS + 1] = GN_EPS
    pc[:, PC_EPS + 2] = 1e-24
    pc[:, PC_EPS + 3] = 1.0
    m["pc"] = pc
    c = _host_consts()
    m["cm"] = c["cm"].reshape(128, 12 * 128)
    m["rst"] = c["rst"]
    return m


_CFG = {"ffn1": True, "mix": True, "ffn2": True}


def kernel(**inputs):
    x = np.asarray(inputs["x"], np.float32)
    B, T, _ = x.shape
    shared = _prep_shared(inputs)
    nc = build_program(T, _CFG)
    in_maps = []
    for b in range(B):
        m = dict(shared)
        xs = 2 if (T // TT) % 2 == 0 else 1
        th = T // xs
        for i in range(xs):
            m[f"xT{i}"] = np.ascontiguousarray(x[b, i * th:(i + 1) * th].T)
        in_maps.append(m)
    res = run_bass_kernel_spmd(nc, in_maps, core_ids=list(range(B)))
    xs = 2 if (T // TT) % 2 == 0 else 1
    out = np.stack([np.concatenate([r[f"outT{i}"].T for i in range(xs)], axis=0) for r in res.results], axis=0)
    return out.astype(np.float32)
```
